# Optimizing a Trainium2 kernel written in Bass

```python
import math
import jax, jax.numpy as jnp
from jax import lax
import numpy as np

D_MODEL = 4096
BATCH = 2
SEQ = 8192
DEPTH = 2

F32 = jnp.float32
GRID_W = 64
CTX_LEN = 256
ROPE_BASE = 10000.0
NORM_EPS = 1e-6
MASK_VALUE = -1e30

LRU_WIDTH = 1024
LRU_BLOCKS = 8
LRU_BLOCK_DIM = LRU_WIDTH // LRU_BLOCKS
CONV_WIDTH = 4
LRU_C = 8.0
HG_HEADS = 8
HG_DK = 128
HG_DV = 128
HG_KEY = HG_HEADS * HG_DK
HG_VAL = HG_HEADS * HG_DV
HG_CHUNK = 64
MLA_HEADS = 8
MLA_NOPE = 128
MLA_ROPE = 64
MLA_V = 128
MLA_Q_RANK = 1024
MLA_KV_RANK = 512
Q_BLOCK = 128
WIN_HEADS = 16
WIN_KV_HEADS = 2
WIN_HD = 64
WINDOW = 128
WIN_BLOCK = 128
D_MIX = LRU_WIDTH + HG_VAL + MLA_HEADS * MLA_V + WIN_HEADS * WIN_HD
N_EXPERTS = 16
EXPERT_FF = 1024
EC_CAPACITY = 2

IN_SPLITS = (LRU_WIDTH, LRU_WIDTH,
             HG_KEY, HG_KEY, HG_KEY, HG_VAL, HG_VAL,
             MLA_Q_RANK, MLA_KV_RANK, MLA_ROPE,
             WIN_HEADS * WIN_HD, WIN_KV_HEADS * WIN_HD, WIN_KV_HEADS * WIN_HD)
D_IN = sum(IN_SPLITS)

kernel_name = "hybrid_parallel_heads_ec_moe_diffusion"


def rms_norm(x, g):
    xf = x.astype(F32)
    y = xf * lax.rsqrt(jnp.mean(xf * xf, axis=-1, keepdims=True) + NORM_EPS)
    return (y * g.astype(F32)).astype(x.dtype)


def modulate(h, shift, scale):
    return h * (1.0 + scale) + shift


def _split_cols(p):
    return jnp.split(p, np.cumsum(IN_SPLITS)[:-1].tolist(), axis=-1)


def _flip(t, rev):
    return jnp.flip(t, axis=1) if rev else t


def _grid_positions(n_tokens):
    rows = n_tokens // GRID_W
    r, cidx = jnp.meshgrid(jnp.arange(rows), jnp.arange(GRID_W), indexing="ij")
    return r.reshape(-1), cidx.reshape(-1)


def _rope_1d(x, pos):
    n = x.shape[-1] // 2
    inv = ROPE_BASE ** (-jnp.arange(n, dtype=F32) / n)
    ang = pos.astype(F32)[:, None] * inv
    cos = jnp.cos(ang)[:, None, :].astype(x.dtype)
    sin = jnp.sin(ang)[:, None, :].astype(x.dtype)
    x1, x2 = x[..., :n], x[..., n:]
    return jnp.concatenate([x1 * cos - x2 * sin, x1 * sin + x2 * cos], axis=-1)


def axial_rope(x, row, col):
    h = x.shape[-1] // 2
    return jnp.concatenate([_rope_1d(x[..., :h], row), _rope_1d(x[..., h:], col)], axis=-1)


def _centered_dwconv(x, w, b):
    k = w.shape[0]
    y = lax.conv_general_dilated(x, w[:, None, :], window_strides=(1,),
                                 padding=[(k // 2, k - 1 - k // 2)],
                                 dimension_numbers=("NWC", "WIO", "NWC"),
                                 feature_group_count=x.shape[-1])
    return y + b


def _linear_scan(a, b, h0):
    b = b.at[:, 0].add(a[:, 0] * h0)
    def comb(l, r):
        return l[0] * r[0], r[0] * l[1] + r[1]
    _, h = lax.associative_scan(comb, (a, b), axis=1)
    return h


def _rglru_coeffs(u, wr, br, wi, bi, lam):
    ub = u.reshape(u.shape[:-1] + (LRU_BLOCKS, LRU_BLOCK_DIM))
    r = jax.nn.sigmoid(jnp.einsum("btni,nij->btnj", ub, wr).reshape(u.shape) + br)
    i = jax.nn.sigmoid(jnp.einsum("btni,nij->btnj", ub, wi).reshape(u.shape) + bi)
    log_a = -LRU_C * r.astype(F32) * jax.nn.softplus(-lam.astype(F32))
    a = jnp.exp(log_a)
    b = jnp.sqrt(-jnp.expm1(2.0 * log_a)) * (i * u).astype(F32)
    return a, b


def rglru_mixer(pc, pl, conv_w, conv_b, wr, br, wi, bi, lam, need_ctx):
    (xc_br, gc), (xl_br, gl) = pc, pl
    uc = _centered_dwconv(xc_br, conv_w, conv_b)
    ul = _centered_dwconv(xl_br, conv_w, conv_b)
    bsz = ul.shape[0]
    hl, hc = 0.0, 0.0
    for d in range(2):
        rev = d == 1
        ac, bc = _rglru_coeffs(_flip(uc, rev), wr[d], br[d], wi[d], bi[d], lam[d])
        hc_d = _linear_scan(ac, bc, jnp.zeros((bsz, LRU_WIDTH), F32))
        al, bl = _rglru_coeffs(_flip(ul, rev), wr[d], br[d], wi[d], bi[d], lam[d])
        hl_d = _linear_scan(al, bl, hc_d[:, -1])
        hl = hl + _flip(hl_d, rev)
        if need_ctx:
            hc = hc + _flip(hc_d, rev)
    yl = hl.astype(gl.dtype) * jax.nn.gelu(gl)
    yc = hc.astype(gc.dtype) * jax.nn.gelu(gc) if need_ctx else None
    return yc, yl


def _hgrn2_chunk_scan(q, k, logf, v, s0):
    bsz, t, h, _ = q.shape
    n = t // HG_CHUNK
    def to_chunks(z):
        return jnp.moveaxis(z.reshape(bsz, n, HG_CHUNK, h, z.shape[-1]), 1, 0)
    causal = jnp.tril(jnp.ones((HG_CHUNK, HG_CHUNK), dtype=bool))[None, :, :, None, None]
    def step(s, inp):
        qc, kc, lfc, vc = inp
        g = jnp.cumsum(lfc, axis=1)
        o_inter = jnp.einsum("bchk,bhkv->bchv", qc * jnp.exp(g), s)
        diff = g[:, :, None] - g[:, None, :]
        decay = jnp.where(causal, jnp.exp(jnp.minimum(diff, 0.0)), 0.0)
        att = jnp.einsum("bthk,bshk,btshk->bhts", qc, kc, decay)
        o_intra = jnp.einsum("bhts,bshv->bthv", att, vc)
        g_last = g[:, -1]
        s_new = jnp.exp(g_last)[..., None] * s + jnp.einsum(
            "bshk,bshv->bhkv", kc * jnp.exp(g_last[:, None] - g), vc)
        return s_new, o_inter + o_intra
    s_fin, o = lax.scan(step, s0, (to_chunks(q), to_chunks(k), to_chunks(logf), to_chunks(v)))
    return jnp.moveaxis(o, 0, 1).reshape(bsz, t, h, v.shape[-1]), s_fin


def hgrn2_mixer(pc, pl, lb, norm_g, need_ctx):
    def heads(z):
        return z.reshape(z.shape[:2] + (HG_HEADS, -1)).astype(F32)
    def gates(fraw, lbd):
        sig = jax.nn.sigmoid(fraw.astype(F32))
        f = lbd + (1.0 - lbd) * sig
        return heads(1.0 - f), heads(jnp.log(f))
    qc, fcf, fcb, ic, gc = pc
    ql, flf, flb, il, gl = pl
    bsz = ql.shape[0]
    ol, oc = 0.0, 0.0
    for d, (fc_raw, fl_raw) in enumerate(((fcf, flf), (fcb, flb))):
        rev = d == 1
        kc_, lfc = gates(fc_raw, lb[d])
        kl_, lfl = gates(fl_raw, lb[d])
        s0 = jnp.zeros((bsz, HG_HEADS, HG_DK, HG_DV), F32)
        o_c, s_c = _hgrn2_chunk_scan(_flip(heads(qc), rev), _flip(kc_, rev), _flip(lfc, rev),
                                     _flip(heads(ic), rev), s0)
        o_l, _ = _hgrn2_chunk_scan(_flip(heads(ql), rev), _flip(kl_, rev), _flip(lfl, rev),
                                   _flip(heads(il), rev), s_c)
        ol = ol + _flip(o_l, rev)
        if need_ctx:
            oc = oc + _flip(o_c, rev)
    def readout(o, g):
        y = rms_norm(o, norm_g.reshape(HG_HEADS, HG_DV)).reshape(g.shape)
        return y.astype(g.dtype) * jax.nn.silu(g)
    yl = readout(ol, gl)
    yc = readout(oc, gc) if need_ctx else None
    return yc, yl


def _block_attention(q, k, v, scale):
    bsz, t, h, dq = q.shape
    n = t // Q_BLOCK
    qb = jnp.moveaxis(q.reshape(bsz, n, Q_BLOCK, h, dq), 1, 0)
    def one(qblk):
        s = jnp.einsum("bqhd,bkhd->bhqk", qblk, k).astype(F32) * scale
        p = jax.nn.softmax(s, axis=-1).astype(v.dtype)
        return jnp.einsum("bhqk,bkhd->bqhd", p, v)
    o = lax.map(one, qb)
    return jnp.moveaxis(o, 0, 1).reshape(bsz, t, h, v.shape[-1])


def mla_mixer(pc, pl, q_g, kv_g, w_uq, w_ukv, row, col, need_ctx):
    def queries(cq, rope):
        bsz, t, _ = cq.shape
        q = (rms_norm(cq, q_g) @ w_uq).reshape(bsz, t, MLA_HEADS, MLA_NOPE + MLA_ROPE)
        if rope:
            q = jnp.concatenate([q[..., :MLA_NOPE], axial_rope(q[..., MLA_NOPE:], row, col)], axis=-1)
        return q
    def keys_values(ckv, kr, rope):
        bsz, t, _ = ckv.shape
        kv = (rms_norm(ckv, kv_g) @ w_ukv).reshape(bsz, t, MLA_HEADS, MLA_NOPE + MLA_V)
        k_rope = kr[:, :, None, :]
        if rope:
            k_rope = axial_rope(k_rope, row, col)
        k = jnp.concatenate([kv[..., :MLA_NOPE],
                             jnp.broadcast_to(k_rope, (bsz, t, MLA_HEADS, MLA_ROPE))], axis=-1)
        return k, kv[..., MLA_NOPE:]
    (cq_c, ckv_c, kr_c), (cq_l, ckv_l, kr_l) = pc, pl
    bsz, seq_l, _ = cq_l.shape
    scale = (MLA_NOPE + MLA_ROPE) ** -0.5
    k_c, v_c = keys_values(ckv_c, kr_c, False)
    k_l, v_l = keys_values(ckv_l, kr_l, True)
    y_l = _block_attention(queries(cq_l, True), jnp.concatenate([k_l, k_c], axis=1),
                           jnp.concatenate([v_l, v_c], axis=1), scale).reshape(bsz, seq_l, -1)
    y_c = None
    if need_ctx:
        y_c = _block_attention(queries(cq_c, False), k_c, v_c, scale).reshape(bsz, cq_c.shape[1], -1)
    return y_c, y_l


def window_mixer(pc, pl, sink, row, col, need_ctx):
    (qc, kc, vc), (ql, kl, vl) = pc, pl
    bsz, seq_l, _ = ql.shape
    seq_c = kc.shape[1]
    grp = WIN_HEADS // WIN_KV_HEADS
    ql = axial_rope(ql.reshape(bsz, seq_l, WIN_HEADS, WIN_HD), row, col).reshape(
        bsz, seq_l, WIN_KV_HEADS, grp, WIN_HD)
    kl = axial_rope(kl.reshape(bsz, seq_l, WIN_KV_HEADS, WIN_HD), row, col)
    vl = vl.reshape(bsz, seq_l, WIN_KV_HEADS, WIN_HD)
    kc = kc.reshape(bsz, seq_c, WIN_KV_HEADS, WIN_HD)
    vc = vc.reshape(bsz, seq_c, WIN_KV_HEADS, WIN_HD)
    scale = WIN_HD ** -0.5
    sink_f = sink.astype(F32).reshape(WIN_KV_HEADS, grp)[None, :, :, None, None]

    def sink_softmax(scores):
        s = jnp.concatenate(scores + [jnp.broadcast_to(sink_f, scores[0].shape[:-1] + (1,))], axis=-1)
        return jax.nn.softmax(s, axis=-1)[..., :-1]

    span = WIN_BLOCK + 2 * WINDOW
    kpad = jnp.pad(kl, ((0, 0), (WINDOW, WINDOW), (0, 0), (0, 0)))
    vpad = jnp.pad(vl, ((0, 0), (WINDOW, WINDOW), (0, 0), (0, 0)))
    nb = seq_l // WIN_BLOCK
    qb = jnp.moveaxis(ql.reshape(bsz, nb, WIN_BLOCK, WIN_KV_HEADS, grp, WIN_HD), 1, 0)
    offs = jnp.arange(span) - WINDOW
    band = jnp.abs(offs[None, :] - jnp.arange(WIN_BLOCK)[:, None]) <= WINDOW

    def one(args):
        bidx, qblk = args
        start = bidx * WIN_BLOCK
        kw = lax.dynamic_slice_in_dim(kpad, start, span, axis=1)
        vw = lax.dynamic_slice_in_dim(vpad, start, span, axis=1)
        kpos = start + offs
        ok = band & ((kpos >= 0) & (kpos < seq_l))[None, :]
        s_w = jnp.where(ok, jnp.einsum("bqngd,bknd->bngqk", qblk, kw).astype(F32) * scale, MASK_VALUE)
        s_c = jnp.einsum("bqngd,bknd->bngqk", qblk, kc).astype(F32) * scale
        p = sink_softmax([s_w, s_c]).astype(vw.dtype)
        return (jnp.einsum("bngqk,bknd->bqngd", p[..., :span], vw)
                + jnp.einsum("bngqk,bknd->bqngd", p[..., span:], vc))

    o = lax.map(one, (jnp.arange(nb), qb))
    y_l = jnp.moveaxis(o, 0, 1).reshape(bsz, seq_l, WIN_HEADS * WIN_HD)
    y_c = None
    if need_ctx:
        qcc = qc.reshape(bsz, seq_c, WIN_KV_HEADS, grp, WIN_HD)
        s = jnp.einsum("bqngd,bknd->bngqk", qcc, kc).astype(F32) * scale
        p = sink_softmax([s]).astype(vc.dtype)
        y_c = jnp.einsum("bngqk,bknd->bqngd", p, vc).reshape(bsz, seq_c, WIN_HEADS * WIN_HD)
    return y_c, y_l


def expert_choice_ffn(h, w_router, w_gate, w_up, w_down):
    bsz, n, d = h.shape
    cap = max(1, EC_CAPACITY * n // N_EXPERTS)
    aff = jax.nn.softmax((h @ w_router).astype(F32), axis=-1)
    g, idx = lax.top_k(jnp.swapaxes(aff, 1, 2), cap)
    xg = jax.vmap(lambda t, i: t[i])(h, idx)
    hid = jax.nn.silu(jnp.einsum("becd,edf->becf", xg, w_gate)) * jnp.einsum("becd,edf->becf", xg, w_up)
    y = jnp.einsum("becf,efd->becd", hid, w_down) * g[..., None].astype(h.dtype)
    return jax.vmap(lambda yy, i: jax.ops.segment_sum(yy.reshape(-1, d), i.reshape(-1), num_segments=n))(y, idx)


def _trunk_layer(xc, xl, c, c_ctx, row, col, lb, need_ctx,
                 w_ada, b_ada, g1, g2, w_in,
                 conv_w, conv_b, wr, br, wi, bi, lam,
                 hg_g, q_g, kv_g, w_uq, w_ukv, sink,
                 w_out, w_router, w_gate, w_up, w_down):
    sh1l, sc1l, ga1l, sh2l, sc2l, ga2l = jnp.split((jax.nn.silu(c) @ w_ada + b_ada)[:, None, :], 6, axis=-1)
    sh1c, sc1c, ga1c, sh2c, sc2c, ga2c = jnp.split(jax.nn.silu(c_ctx) @ w_ada + b_ada, 6, axis=-1)
    pl = _split_cols(modulate(rms_norm(xl, g1), sh1l, sc1l) @ w_in)
    pc = _split_cols(modulate(rms_norm(xc, g1), sh1c, sc1c) @ w_in)
    ya_c, ya_l = rglru_mixer(pc[0:2], pl[0:2], conv_w, conv_b, wr, br, wi, bi, lam, need_ctx)
    yb_c, yb_l = hgrn2_mixer(pc[2:7], pl[2:7], lb, hg_g, need_ctx)
    yc_c, yc_l = mla_mixer(pc[7:10], pl[7:10], q_g, kv_g, w_uq, w_ukv, row, col, need_ctx)
    yd_c, yd_l = window_mixer(pc[10:13], pl[10:13], sink, row, col, need_ctx)
    xl = xl + ga1l * (jnp.concatenate([ya_l, yb_l, yc_l, yd_l], axis=-1) @ w_out)
    xl = xl + ga2l * expert_choice_ffn(modulate(rms_norm(xl, g2), sh2l, sc2l), w_router, w_gate, w_up, w_down)
    if need_ctx:
        xc = xc + ga1c * (jnp.concatenate([ya_c, yb_c, yc_c, yd_c], axis=-1) @ w_out)
        xc = xc + ga2c * expert_choice_ffn(modulate(rms_norm(xc, g2), sh2c, sc2c), w_router, w_gate, w_up, w_down)
    return xc, xl


def setup_inputs(seed: int = 0) -> dict:
    key = jax.random.key(seed)
    ks = iter(jax.random.split(key, 40))
    d = D_MODEL
    def nrm(shape, scale):
        return jax.random.normal(next(ks), shape, F32) * scale
    def gain(shape):
        return 1.0 + nrm(shape, 0.02)
    x = nrm((BATCH, SEQ, d), 1.0)
    c = nrm((BATCH, d), 1.0)
    ctx = nrm((BATCH, CTX_LEN, d), 1.0)
    c_ctx = nrm((d,), 1.0)
    w_ada = nrm((DEPTH, d, 6 * d), 0.5 * d ** -0.5)
    b_ada = nrm((DEPTH, 6 * d), 0.02)
    norm1_g = gain((DEPTH, d))
    norm2_g = gain((DEPTH, d))
    w_in = nrm((DEPTH, d, D_IN), d ** -0.5)
    lru_conv_w = nrm((DEPTH, CONV_WIDTH, LRU_WIDTH), CONV_WIDTH ** -0.5)
    lru_conv_b = nrm((DEPTH, LRU_WIDTH), 0.02)
    lru_wr = nrm((DEPTH, 2, LRU_BLOCKS, LRU_BLOCK_DIM, LRU_BLOCK_DIM), LRU_BLOCK_DIM ** -0.5)
    lru_br = nrm((DEPTH, 2, LRU_WIDTH), 0.02)
    lru_wi = nrm((DEPTH, 2, LRU_BLOCKS, LRU_BLOCK_DIM, LRU_BLOCK_DIM), LRU_BLOCK_DIM ** -0.5)
    lru_bi = nrm((DEPTH, 2, LRU_WIDTH), 0.02)
    u = jax.random.uniform(next(ks), (DEPTH, 2, LRU_WIDTH), F32, 0.9, 0.999)
    a0 = u ** (1.0 / LRU_C)
    lru_lambda = jnp.log(a0) - jnp.log1p(-a0)
    hg_lb_logits = nrm((2, DEPTH, HG_KEY), 0.5)
    hg_norm_g = gain((DEPTH, HG_VAL))
    mla_q_norm_g = gain((DEPTH, MLA_Q_RANK))
    mla_kv_norm_g = gain((DEPTH, MLA_KV_RANK))
    mla_w_uq = nrm((DEPTH, MLA_Q_RANK, MLA_HEADS * (MLA_NOPE + MLA_ROPE)), MLA_Q_RANK ** -0.5)
    mla_w_ukv = nrm((DEPTH, MLA_KV_RANK, MLA_HEADS * (MLA_NOPE + MLA_V)), MLA_KV_RANK ** -0.5)
    win_sink = nrm((DEPTH, WIN_HEADS), 0.5)
    w_out = nrm((DEPTH, D_MIX, d), D_MIX ** -0.5)
    w_router = nrm((DEPTH, d, N_EXPERTS), d ** -0.5)
    w_gate = nrm((DEPTH, N_EXPERTS, d, EXPERT_FF), d ** -0.5)
    w_up = nrm((DEPTH, N_EXPERTS, d, EXPERT_FF), d ** -0.5)
    w_down = nrm((DEPTH, N_EXPERTS, EXPERT_FF, d), EXPERT_FF ** -0.5)
    final_norm_g = gain((d,))
    return {"x": x, "c": c, "ctx": ctx, "c_ctx": c_ctx, "w_ada": w_ada, "b_ada": b_ada,
            "norm1_g": norm1_g, "norm2_g": norm2_g, "w_in": w_in,
            "lru_conv_w": lru_conv_w, "lru_conv_b": lru_conv_b, "lru_wr": lru_wr, "lru_br": lru_br,
            "lru_wi": lru_wi, "lru_bi": lru_bi, "lru_lambda": lru_lambda,
            "hg_lb_logits": hg_lb_logits, "hg_norm_g": hg_norm_g,
            "mla_q_norm_g": mla_q_norm_g, "mla_kv_norm_g": mla_kv_norm_g,
            "mla_w_uq": mla_w_uq, "mla_w_ukv": mla_w_ukv, "win_sink": win_sink,
            "w_out": w_out, "w_router": w_router, "w_gate": w_gate, "w_up": w_up, "w_down": w_down,
            "final_norm_g": final_norm_g}


def reference(x, c, ctx, c_ctx, w_ada, b_ada, norm1_g, norm2_g, w_in,
              lru_conv_w, lru_conv_b, lru_wr, lru_br, lru_wi, lru_bi, lru_lambda,
              hg_lb_logits, hg_norm_g, mla_q_norm_g, mla_kv_norm_g, mla_w_uq, mla_w_ukv, win_sink,
              w_out, w_router, w_gate, w_up, w_down, final_norm_g):
    row, col = _grid_positions(x.shape[1])
    lb_p = jax.nn.softmax(hg_lb_logits.astype(F32), axis=1)
    lb_all = jnp.cumsum(lb_p, axis=1) - lb_p[:, :1]
    xc, xl = ctx, x
    for li in range(DEPTH):
        xc, xl = _trunk_layer(xc, xl, c, c_ctx, row, col, lb_all[:, li], li < DEPTH - 1,
                              w_ada[li], b_ada[li], norm1_g[li], norm2_g[li], w_in[li],
                              lru_conv_w[li], lru_conv_b[li], lru_wr[li], lru_br[li], lru_wi[li], lru_bi[li],
                              lru_lambda[li], hg_norm_g[li], mla_q_norm_g[li], mla_kv_norm_g[li],
                              mla_w_uq[li], mla_w_ukv[li], win_sink[li],
                              w_out[li], w_router[li], w_gate[li], w_up[li], w_down[li])
    return rms_norm(xl, final_norm_g)
```

```python
import contextlib
import numpy as np
import concourse.bass as bass
import concourse.mybir as mybir

F32 = mybir.dt.float32
BF16 = mybir.dt.bfloat16
I32 = mybir.dt.int32
U32 = mybir.dt.uint32
AF = mybir.ActivationFunctionType
ALU = mybir.AluOpType
AX = mybir.AxisListType

NSLOT = 10
QUEUES = ("sp", "pool")
ENGS = ("pe", "dve", "act", "pool", "sp")


class Buf:
    def __init__(self, t, name=""):
        self.t = t
        self.name = name
        self.w = None
        self.r = []

    def __getitem__(self, idx):
        return self.t[idx]

    def ap(self):
        return self.t[:]


class SplitBuf(Buf):
    def __init__(self, parts, name=""):
        Buf.__init__(self, None, name)
        self.parts = parts

    def __getitem__(self, idx):
        rs, cs_ = idx
        for (r0, r1, ap) in self.parts:
            if r0 <= rs.start < r1:
                assert rs.stop <= r1
                return ap[rs.start - r0:rs.stop - r0, cs_]
        raise IndexError(idx)

    def ap(self):
        assert len(self.parts) == 1
        return self.parts[0][2]


class K:
    def __init__(self, nc):
        self.nc = nc
        self.es = contextlib.ExitStack()
        self.eng = {"pe": nc.tensor, "dve": nc.vector, "act": nc.scalar, "pool": nc.gpsimd, "sp": nc.sync}
        self.sem = {}
        self.cnt = {}
        for e in ENGS:
            self.sem[e] = self.es.enter_context(nc.semaphore("s_" + e))
            self.cnt[e] = 0
        for q in QUEUES:
            for s in range(NSLOT):
                n = "d_%s%d" % (q, s)
                self.sem[n] = self.es.enter_context(nc.semaphore(n))
                self.cnt[n] = 0
        self.slot = {q: 0 for q in QUEUES}
        self.slot_tok = {}
        self.epoch = 0
        self.seen = {e: {} for e in ENGS}
        self.dry = False
        self.vars = {}
        self.nvar = 0
        self.n_ins = 0

    def sbuf(self, name, shape, dt):
        self.nalloc = getattr(self, "nalloc", 0) + 1
        name = "sb%d_%s" % (self.nalloc, name)
        return Buf(self.es.enter_context(self.nc.sbuf_tensor(name, list(shape), dt)), name)

    def psum(self, name, shape, dt=F32):
        return Buf(self.es.enter_context(self.nc.psum_tensor(name, list(shape), dt)), name)

    def dram(self, name, shape, dt, kind="Internal", shared=False):
        if kind == "Internal":
            t = self.nc.dram_tensor(name, list(shape), dt, addr_space="Shared" if shared else "Local")
        else:
            t = self.nc.dram_tensor(name, list(shape), dt, kind=kind)
        return Buf(t.ap(), name)

    def _val(self, lin):
        return lin

    def _wait(self, e, tok):
        if tok is None:
            return
        sname, lin, ep = tok
        if ep != self.epoch:
            return
        prev = self.seen[e].get(sname)
        if prev is not None and prev >= lin:
            return
        self.seen[e][sname] = lin
        if not self.dry:
            self.eng[e].wait_ge(self.sem[sname], self._val(lin))

    def _deps(self, e, reads, writes, pe_acc=False):
        for b in reads:
            self._wait(e, b.w)
        for b in writes:
            if not (pe_acc and b.w is not None and b.w[0] == "pe"):
                self._wait(e, b.w)
            for t in b.r:
                self._wait(e, t)

    def _mark(self, tok, reads, writes):
        for b in reads:
            b.r.append(tok)
            if len(b.r) > 24:
                b.r = b.r[-24:] if False else b.r
        for b in writes:
            b.w = tok
            b.r = []

    def op(self, e, fn, reads=(), writes=(), pe_acc=False):
        self._deps(e, reads, writes, pe_acc)
        self.cnt[e] += 1
        if not self.dry:
            fn().then_inc(self.sem[e], 1)
        self.n_ins += 1
        tok = (e, self.cnt[e], self.epoch)
        self._mark(tok, reads, writes)
        return tok

    def dma(self, q, fn, reads=(), writes=()):
        s = self.slot[q]
        self.slot[q] = (s + 1) % NSLOT
        sname = "d_%s%d" % (q, s)
        self._wait(q, self.slot_tok.get(sname))
        self._deps(q, reads, writes)
        self.cnt[sname] += 16
        if not self.dry:
            fn(self.eng[q]).then_inc(self.sem[sname], 16)
        self.n_ins += 1
        tok = (sname, self.cnt[sname], self.epoch)
        self.slot_tok[sname] = tok
        self._mark(tok, reads, writes)
        return tok

    def barrier(self):
        if not self.dry:
            for e in ENGS:
                for sname, lin in self.cnt.items():
                    if sname == e:
                        continue
                    self.eng[e].wait_ge(self.sem[sname], self._val(lin))
        self.epoch += 1
        self.seen = {e: {} for e in ENGS}
        for q in QUEUES:
            self.slot[q] = 0

    def all_core_barrier(self):
        self.barrier()
        if not self.dry:
            self.nc.all_core_barrier()
        self.barrier()

    def close(self):
        self.barrier()
        self.es.close()


    @contextlib.contextmanager
    def scope(self):
        old = self.es
        self.es = contextlib.ExitStack()
        try:
            yield
        finally:
            self.barrier()
            self.es.close()
            self.es = old


class Cfg:
    def __init__(s, **kw):
        s.__dict__.update(kw)
        s.HK = s.HH * 128
        s.splits = [s.LW, s.LW, s.HK, s.HK, s.HK, s.HK, s.HK, s.QR, s.KVR, 64, s.WH * 64, s.WKV * 64, s.WKV * 64]
        s.off = [0]
        for w in s.splits:
            s.off.append(s.off[-1] + w)
        s.DIN = s.off[-1]
        (s.o_lx, s.o_lg, s.o_hq, s.o_hff, s.o_hfb, s.o_hi, s.o_hg, s.o_cq, s.o_ckv, s.o_kr, s.o_wq, s.o_wk, s.o_wv) = s.off[:13]
        s.DMIX = s.LW + s.HK + s.MH * 128 + s.WH * 64
        s.KC = s.D // 128
        s.TT = s.T + s.Tc
        s.cap = max(1, 2 * s.T // s.NE)
        s.capc = max(1, 2 * s.Tc // s.NE)
        s.blocks = [(0, s.Tc, True)] + [(s.Tc + i * 512, 512, False) for i in range(s.T // 512)]
        s.ntiles = []
        c0 = 0
        while c0 < s.DIN:
            w = 128
            if c0 == s.o_kr:
                w = 64
            s.ntiles.append((c0, min(w, s.DIN - c0)))
            c0 += w


FULL = dict(D=4096, T=8192, Tc=256, L=2, GW=64, LW=1024, HH=8, MH=8, QR=1024, KVR=512, WH=16, WKV=2, NE=16, FF=1024)
EPS = 1e-6


def _dbg(k, cfg, name, buf_ap_fn, shape, dt=F32):
    if not cfg.debug:
        return
    o = k.dram("dbg_" + name, shape, dt, kind="ExternalOutput")
    k.barrier()
    k.dma("sp", lambda e: e.dma_start(out=o.ap(), in_=buf_ap_fn()), writes=[o])
    k.barrier()


def _dump(k, cfg, name, buf, ap_fn, shape, dt=F32):
    if not getattr(cfg, "dump", False):
        return
    o = k.dram("dmp_" + name, shape, dt, kind="ExternalOutput")
    k.dma("sp", lambda e: e.dma_start(out=o.ap(), in_=ap_fn()), reads=[buf], writes=[o])


def build(cfg):
    nc = bass.Bass("TRN2", target_bir_lowering=False, num_devices=2)
    k = K(nc)
    k.es.enter_context(nc.allow_non_contiguous_dma(reason="small strided parameter / layout DMAs"))
    c = cfg
    D, T, Tc, TT, L, KC, DIN = c.D, c.T, c.Tc, c.TT, c.L, c.KC, c.DIN
    V, S, A = nc.vector, nc.scalar, nc.tensor

    def inp(name, shape, dt=F32):
        return k.dram(name, shape, dt, kind="ExternalInput")

    I = {}
    I["x"] = inp("x", [T, D]); I["ctx"] = inp("ctx", [Tc, D]); I["cv"] = inp("cv", [128, 3, KC])
    I["w_ada"] = inp("w_ada", [D, 6 * D]); I["b_ada"] = inp("b_ada", [L, 6 * D])
    I["g1"] = inp("g1", [L, D]); I["g2"] = inp("g2", [L, D]); I["gf"] = inp("gf", [1, D])
    I["w_in"] = inp("w_in", [D, DIN]); I["w_out"] = inp("w_out", [c.DMIX, D])
    I["w_gate"] = inp("w_gate", [c.NE * D, c.FF]); I["w_up"] = inp("w_up", [c.NE * D, c.FF]); I["w_down"] = inp("w_down", [c.NE * c.FF, D])
    I["w_router"] = inp("w_router", [L, D, c.NE])
    I["w_uq"] = inp("w_uq", [L, c.QR, c.MH * 192]); I["w_ukv"] = inp("w_ukv", [L, c.KVR, c.MH * 256])
    I["conv_w"] = inp("conv_w", [L, 4, c.LW]); I["conv_b"] = inp("conv_b", [L, c.LW])
    I["lru_wr"] = inp("lru_wr", [L, 2, c.LW // 128, 128, 128]); I["lru_wi"] = inp("lru_wi", [L, 2, c.LW // 128, 128, 128])
    I["lru_br"] = inp("lru_br", [L, 2, c.LW]); I["lru_bi"] = inp("lru_bi", [L, 2, c.LW]); I["lru_lam"] = inp("lru_lam", [L, 2, c.LW])
    I["hg_lb"] = inp("hg_lb", [2, L, c.HK]); I["hg_g"] = inp("hg_g", [L, c.HK])
    I["q_g"] = inp("q_g", [L, c.QR]); I["kv_g"] = inp("kv_g", [L, c.KVR]); I["sink"] = inp("sink", [L, c.WH])
    I["coreidx"] = inp("coreidx", [128, 16], I32)
    I["ident_f"] = inp("ident_f", [128, 128]); I["rope_cs"] = inp("rope_cs", [2, 128, T]); I["rope_rt"] = inp("rope_rt", [128, 128])
    I["wmask"] = inp("wmask", [128, 384]); I["tri"] = inp("tri", [2, 64, 512])
    OUT = k.dram("out", [T, D], F32, kind="ExternalOutput")

    WINB = k.dram("WINB", [L * D, DIN], BF16, shared=True)
    WOUTB = k.dram("WOUTB", [L * c.DMIX, D], BF16, shared=True)
    NH = c.NE // 2
    WGB = [k.dram("WGB%d" % h_, [L * NH * D, c.FF], BF16, shared=True) for h_ in range(2)]
    WUB = [k.dram("WUB%d" % h_, [L * NH * D, c.FF], BF16, shared=True) for h_ in range(2)]
    WDB = [k.dram("WDB%d" % h_, [L * NH * c.FF, D], BF16, shared=True) for h_ in range(2)]
    ADA = k.dram("ADA", [L * 3, 6 * D], F32, shared=True)
    MOD = k.dram("MOD", [L * 2 * 6, D], F32)
    X = k.dram("X", [TT, D], F32)
    if DIN * TT * 4 > 200 * 2 ** 20:
        PT = SplitBuf([(0, c.o_hi, k.dram("PTa", [c.o_hi, TT], F32).t), (c.o_hi, DIN, k.dram("PTb", [DIN - c.o_hi, TT], F32).t)], "PT")
    else:
        PT = SplitBuf([(0, DIN, k.dram("PT", [DIN, TT], F32).t)], "PT")
    YT = k.dram("YT", [c.DMIX, TT], BF16)

    ident_f = k.sbuf("ident_f", [128, 128], F32)
    ident_b = k.sbuf("ident_b", [128, 128], BF16)
    cidx = k.sbuf("cidx", [128, 16], I32)
    k.dma("sp", lambda e: e.dma_start(out=ident_f[:, :], in_=I["ident_f"][:, :]), reads=[I["ident_f"]], writes=[ident_f])
    k.op("dve", lambda: V.tensor_copy(out=ident_b[:, :], in_=ident_f[:, :]), reads=[ident_f], writes=[ident_b])
    k.dma("sp", lambda e: e.dma_start(out=cidx[:, :], in_=I["coreidx"][:, :]), reads=[I["coreidx"]], writes=[cidx])
    PS = [k.psum("ps%d" % i, [128, 512]) for i in range(8)]
    psi = [0]

    def nps():
        psi[0] = (psi[0] + 1) % 6
        return PS[2 + psi[0]]

    poi = [0]

    def npo():
        poi[0] = (poi[0] + 1) % 2
        return PS[poi[0]]

    k.dma("sp", lambda e: e.dma_start(out=X[0:Tc, :], in_=I["ctx"][:, :]), reads=[I["ctx"]], writes=[X])
    for r0 in range(0, T, 512):
        k.dma("sp", lambda e: e.dma_start(out=X[Tc + r0:Tc + r0 + 512, :], in_=I["x"][r0:r0 + 512, :]), reads=[I["x"]], writes=[X])

    def stage(src, dst, rows, cols, widx, src0=0):
        with k.scope():
            CW = cols
            fs = [k.sbuf("stg_f%d" % i, [128, CW], F32) for i in range(2)]
            bs = [k.sbuf("stg_b%d" % i, [128, CW], BF16) for i in range(2)]
            n = 0
            for r0 in range(0, rows, 128):
                for c0 in range(0, cols, CW):
                    cw = min(CW, cols - c0)
                    f, b = fs[n % 2], bs[n % 2]
                    k.dma("sp", lambda e: e.dma_start(out=f[:, :cw], in_=src[src0 + r0:src0 + r0 + 128, c0:c0 + cw]), reads=[src], writes=[f])
                    if n % 2 == 0:
                        k.op("dve", lambda: V.tensor_copy(out=b[:, :cw], in_=f[:, :cw]), reads=[f], writes=[b])
                    else:
                        k.op("act", lambda: S.copy(out=b[:, :cw], in_=f[:, :cw]), reads=[f], writes=[b])
                    k.dma("pool", lambda e: e.indirect_dma_start(
                        out=dst[:, 0:cw], out_offset=bass.IndirectOffsetOnAxis(ap=cidx[:, widx:widx + 1], axis=0),
                        in_=b[:, :cw], in_offset=None, element_offset=r0 * cols + c0), reads=[b, cidx], writes=[])
                    n += 1

    if c.stop_after == "alloc":
        k.barrier()
        for r0 in range(0, T, 512):
            k.dma("sp", lambda e: e.dma_start(out=OUT[r0:r0 + 512, :], in_=X[Tc + r0:Tc + r0 + 512, :]), reads=[X], writes=[OUT])
        return nc, k, I, {}
    stage(I["w_in"], WINB, D, DIN, 0)
    stage(I["w_out"], WOUTB, c.DMIX, D, 1)
    for h_ in range(2):
        stage(I["w_gate"], WGB[h_], NH * D, c.FF, 2, src0=h_ * NH * D)
        stage(I["w_up"], WUB[h_], NH * D, c.FF, 2, src0=h_ * NH * D)
        stage(I["w_down"], WDB[h_], NH * c.FF, D, 3, src0=h_ * NH * c.FF)

    with k.scope():
        cv = k.sbuf("cv", [128, 3, KC], F32)
        cs = k.sbuf("cs", [128, 3, KC], F32)
        k.dma("sp", lambda e: e.dma_start(out=cv[:, :, :], in_=I["cv"][:, :, :]), reads=[I["cv"]], writes=[cv])
        k.op("act", lambda: S.activation(out=cs[:, :, :], in_=cv[:, :, :], func=AF.Silu), reads=[cv], writes=[cs])
        NG = 2048 if (6 * D) % 2048 == 0 else 1536
        wts = [k.sbuf("adaw%d" % i, [128, NG], F32) for i in range(3)]
        ao = k.sbuf("adao", [3, 6 * D], F32)
        n = 0
        for g in range(6 * D // NG):
            pss = [nps() for _ in range(NG // 512)]
            for kc in range(KC):
                wt = wts[n % 3]; n += 1
                k.dma("sp", lambda e: e.dma_start(out=wt[:, :], in_=I["w_ada"][kc * 128:(kc + 1) * 128, g * NG:(g + 1) * NG]), reads=[I["w_ada"]], writes=[wt])
                for j in range(NG // 512):
                    k.op("pe", lambda: A.matmul(pss[j][0:3, :], lhsT=cs[:, :, kc], rhs=wt[:, j * 512:(j + 1) * 512], start=(kc == 0), stop=(kc == KC - 1)),
                         reads=[cs, wt], writes=[pss[j]], pe_acc=(kc > 0))
            for j in range(NG // 512):
                k.op("act", lambda: S.copy(out=ao[:, g * NG + j * 512:g * NG + (j + 1) * 512], in_=pss[j][0:3, :]), reads=[pss[j]], writes=[ao])
        ADA2 = ADA[:, :].rearrange("r (j d) -> (r j) d", j=6)
        aoj = [k.sbuf("adaoj%d" % i, [3, D], F32) for i in range(2)]
        for j in range(6):
            t_ = aoj[j % 2]
            k.op("act", lambda: S.copy(out=t_[:, :], in_=ao[:, j * D:(j + 1) * D]), reads=[ao], writes=[t_])
            k.dma("pool", lambda e: e.indirect_dma_start(out=ADA2, out_offset=bass.IndirectOffsetOnAxis(ap=cidx[0:3, 8 + j:9 + j], axis=0),
                                                         in_=t_[:, :], in_offset=None), reads=[t_, cidx], writes=[])
    k.all_core_barrier()

    with k.scope():
        pid = nc.partition_id()
        ADA4 = ADA[:, :].rearrange("(l v) (j d) -> v l j d", v=3, j=6)
        a_t = k.sbuf("ma", [6, D], F32); b_t = k.sbuf("mb", [6, D], F32); g_t = k.sbuf("mg", [6, D], F32); m_t = k.sbuf("mm", [6, D], F32)
        for l in range(L):
            for v in range(2):
                if v == 0:
                    k.dma("sp", lambda e: e.dma_start(out=a_t[:, :], in_=ADA4[pid, l, :, :]), reads=[ADA], writes=[a_t])
                else:
                    k.dma("sp", lambda e: e.dma_start(out=a_t[:, :], in_=ADA4[2, l, :, :]), reads=[ADA], writes=[a_t])
                k.dma("sp", lambda e: e.dma_start(out=b_t[:, :], in_=I["b_ada"][l, :].rearrange("(j d) -> j d", j=6)), reads=[I["b_ada"]], writes=[b_t])
                k.op("dve", lambda: V.memset(g_t[:, :], 1.0), writes=[g_t])
                k.dma("sp", lambda e: e.dma_start(out=g_t[1:2, :], in_=I["g1"][l:l + 1, :]), reads=[I["g1"]], writes=[g_t])
                k.dma("sp", lambda e: e.dma_start(out=g_t[4:5, :], in_=I["g2"][l:l + 1, :]), reads=[I["g2"]], writes=[g_t])
                k.op("dve", lambda: V.tensor_tensor(out=a_t[:, :], in0=a_t[:, :], in1=b_t[:, :], op=ALU.add), reads=[a_t, b_t], writes=[a_t])
                k.op("dve", lambda: V.scalar_tensor_tensor(out=m_t[:, :], in0=a_t[:, :], scalar=1.0, in1=g_t[:, :], op0=ALU.add, op1=ALU.mult),
                     reads=[a_t, g_t], writes=[m_t])
                r0 = (l * 2 + v) * 6
                k.dma("sp", lambda e: e.dma_start(out=MOD[r0:r0 + 6, :], in_=a_t[:, :]), reads=[a_t], writes=[MOD])
                k.dma("sp", lambda e: e.dma_start(out=MOD[r0 + 1:r0 + 2, :], in_=m_t[1:2, :]), reads=[m_t], writes=[MOD])
                k.dma("sp", lambda e: e.dma_start(out=MOD[r0 + 4:r0 + 5, :], in_=m_t[4:5, :]), reads=[m_t], writes=[MOD])
    _dbg(k, c, "MOD", lambda: MOD.ap(), [L * 12, D])
    _dbg(k, c, "ADA", lambda: ADA.ap(), [L * 3, 6 * D])

    def bc_load(dst, row):
        k.dma("sp", lambda e: e.dma_start(out=dst[:, :], in_=MOD[row:row + 1, :].partition_broadcast(128)), reads=[MOD], writes=[dst])

    def rstd_of(ss, n, tmp):
        k.op("dve", lambda: V.tensor_scalar(out=ss[:, :], in0=ss[:, :], scalar1=1.0 / n, scalar2=EPS, op0=ALU.mult, op1=ALU.add), reads=[ss], writes=[ss])
        k.op("act", lambda: S.activation(out=tmp[:, :], in_=ss[:, :], func=AF.Sqrt), reads=[ss], writes=[tmp])
        k.op("dve", lambda: V.reciprocal(out=ss[:, :], in_=tmp[:, :]), reads=[tmp], writes=[ss])

    def norm_mod_T(xt, hb, junk, Abc, Sbc, ss, tmp, hT, ti, ntok):
        k.op("act", lambda: S.activation(out=junk[:, :], in_=xt[:, :], func=AF.Square, accum_out=ss[:, 0:1]), reads=[xt], writes=[junk, ss])
        rstd_of(ss, D, tmp)
        k.op("dve", lambda: V.scalar_tensor_tensor(out=xt[:, :], in0=xt[:, :], scalar=ss[:, 0:1], in1=Abc[:, :], op0=ALU.mult, op1=ALU.mult),
             reads=[xt, ss, Abc], writes=[xt])
        k.op("dve", lambda: V.tensor_tensor(out=hb[:, :], in0=xt[:, :], in1=Sbc[:, :], op=ALU.add), reads=[xt, Sbc], writes=[hb])
        transpose_into(hb, hT, ti, KC)

    def transpose_into(hb, hT, ti, nkc):
        for k0 in range(0, nkc, 8):
            n8 = min(8, nkc - k0)
            p = nps()
            pb = p[:, :].bitcast(BF16)
            for j in range(n8):
                k.op("pe", lambda: A.transpose(out=pb[:, j * 128:(j + 1) * 128], in_=hb[:, (k0 + j) * 128:(k0 + j + 1) * 128], identity=ident_b[:, :]),
                     reads=[hb, ident_b], writes=[p])
            eng = "act" if (k0 // 8) % 2 else "dve"
            src = lambda: pb[:, 0:n8 * 128].rearrange("p (a b) -> p a b", a=n8)
            dst = lambda: hT[:, k0:k0 + n8, ti * 128:(ti + 1) * 128]
            if eng == "act":
                k.op("act", lambda: S.copy(out=dst(), in_=src()), reads=[p], writes=[hT])
            else:
                k.op("dve", lambda: V.tensor_copy(out=dst(), in_=src()), reads=[p], writes=[hT])

    def phase_in(l):
        with k.scope():
            Abc = k.sbuf("Abc", [128, D], F32); Sbc = k.sbuf("Sbc", [128, D], F32)
            xts = [k.sbuf("xt%d" % i, [128, D], F32) for i in range(2)]
            hb = k.sbuf("hb", [128, D], BF16); junk = k.sbuf("junk", [128, D], BF16)
            ss = k.sbuf("ss", [128, 1], F32); tmp = k.sbuf("tmp", [128, 1], F32)
            hT = k.sbuf("hT", [128, KC, 512], BF16)
            GW_ = 384
            wts = [k.sbuf("wt%d" % i, [128, KC, GW_], BF16) for i in range(2)]
            ots = [k.sbuf("ot%d" % i, [128, 512], F32) for i in range(4)]
            groups = []
            for (c0, w) in c.ntiles:
                if groups and groups[-1][1] + w <= GW_:
                    groups[-1][1] += w; groups[-1][2].append((c0, w))
                else:
                    groups.append([c0, w, [(c0, w)]])
            cur_v = None
            nw = 0; no = 0; nx = 0
            for (tok0, ntok, is_ctx) in c.blocks:
                v = 1 if is_ctx else 0
                if v != cur_v:
                    bc_load(Abc, (l * 2 + v) * 6 + 1); bc_load(Sbc, (l * 2 + v) * 6 + 0); cur_v = v
                for ti in range(ntok // 128):
                    xt = xts[nx % 2]; nx += 1
                    k.dma("sp", lambda e: e.dma_start(out=xt[:, :], in_=X[tok0 + ti * 128:tok0 + (ti + 1) * 128, :]), reads=[X], writes=[xt])
                    norm_mod_T(xt, hb, junk, Abc, Sbc, ss, tmp, hT, ti, ntok)
                for (g0, gw, tiles) in groups:
                    wt = wts[nw % 2]; nw += 1
                    for kq in range(0, KC, 8):
                        k8 = min(8, KC - kq)
                        k.dma("sp", lambda e: e.dma_start(out=wt[:, kq:kq + k8, 0:gw], in_=WINB[l * D + kq * 128:l * D + (kq + k8) * 128, g0:g0 + gw].rearrange("(kc p) n -> p kc n", p=128)),
                              reads=[WINB], writes=[wt])
                    for (c0, w) in tiles:
                        lo = c0 - g0
                        p = nps()
                        for kc in range(KC):
                            k.op("pe", lambda: A.matmul(p[0:w, 0:ntok], lhsT=wt[:, kc, lo:lo + w], rhs=hT[:, kc, 0:ntok], start=(kc == 0), stop=(kc == KC - 1)),
                                 reads=[wt, hT], writes=[p], pe_acc=(kc > 0))
                        ot = ots[no % 4]; no += 1
                        if no % 2:
                            k.op("act", lambda: S.copy(out=ot[0:w, 0:ntok], in_=p[0:w, 0:ntok]), reads=[p], writes=[ot])
                        else:
                            k.op("dve", lambda: V.tensor_copy(out=ot[0:w, 0:ntok], in_=p[0:w, 0:ntok]), reads=[p], writes=[ot])
                        k.dma("sp", lambda e: e.dma_start(out=PT[c0:c0 + w, tok0:tok0 + ntok], in_=ot[0:w, 0:ntok]), reads=[ot], writes=[PT])


    def colload(dst, src_ap_fn, srcbuf):
        k.dma("sp", lambda e: e.dma_start(out=dst, in_=src_ap_fn()), reads=[srcbuf], writes=[])

    SEGS = [(0, Tc)] + [(Tc + i * min(2048, T), min(2048, T)) for i in range(T // min(2048, T))]

    def gelu_tanh(dst, x, t1, n):
        pass

    def phase_lru(l):
        NCH = c.LW // 128
        with k.scope():
            par = k.sbuf("lpar", [128, 16, NCH], F32)
            for kk in range(4):
                k.dma("sp", lambda e: e.dma_start(out=par[:, kk, :], in_=I["conv_w"][l, kk, :].rearrange("(c p) -> p c", p=128)), reads=[I["conv_w"]], writes=[par])
            k.dma("sp", lambda e: e.dma_start(out=par[:, 4, :], in_=I["conv_b"][l, :].rearrange("(c p) -> p c", p=128)), reads=[I["conv_b"]], writes=[par])
            for d in range(2):
                k.dma("sp", lambda e: e.dma_start(out=par[:, 5 + d, :], in_=I["lru_lam"][l, d, :].rearrange("(c p) -> p c", p=128)), reads=[I["lru_lam"]], writes=[par])
                k.dma("sp", lambda e: e.dma_start(out=par[:, 7 + d, :], in_=I["lru_br"][l, d, :].rearrange("(c p) -> p c", p=128)), reads=[I["lru_br"]], writes=[par])
                k.dma("sp", lambda e: e.dma_start(out=par[:, 9 + d, :], in_=I["lru_bi"][l, d, :].rearrange("(c p) -> p c", p=128)), reads=[I["lru_bi"]], writes=[par])
            k.op("act", lambda: S.activation(out=par[:, 11:13, :], in_=par[:, 5:7, :], func=AF.Exp, scale=-1.0), reads=[par], writes=[par])
            k.op("dve", lambda: V.tensor_scalar(out=par[:, 11:13, :], in0=par[:, 11:13, :], scalar1=1.0, scalar2=None, op0=ALU.add), reads=[par], writes=[par])
            k.op("act", lambda: S.activation(out=par[:, 11:13, :], in_=par[:, 11:13, :], func=AF.Ln), reads=[par], writes=[par])
            k.op("dve", lambda: V.tensor_scalar(out=par[:, 13:15, :], in0=par[:, 11:13, :], scalar1=-16.0, scalar2=None, op0=ALU.mult), reads=[par], writes=[par])
            k.op("dve", lambda: V.tensor_scalar(out=par[:, 11:13, :], in0=par[:, 11:13, :], scalar1=-8.0, scalar2=None, op0=ALU.mult), reads=[par], writes=[par])
            XB = k.sbuf("lX", [128, TT], F32); U = k.sbuf("lU", [128, TT], F32)
            SG = max(sz for _, sz in SEGS)
            ub = k.sbuf("lub", [128, SG], BF16); r_ = k.sbuf("lr", [128, SG], F32); i_ = k.sbuf("li", [128, SG], F32)
            t_ = k.sbuf("lt", [128, SG], F32); h_ = k.sbuf("lh", [128, SG], F32); yb = k.sbuf("lyb", [128, SG], BF16)
            carry = k.sbuf("lcarry", [128, 1], F32)
            wf = k.sbuf("lwf", [128, 4, 128], F32); wb = k.sbuf("lwb", [128, 4, 128], BF16)
            for ch in range(NCH):
                pc = lambda j: par[:, j, ch:ch + 1]
                k.dma("sp", lambda e: e.dma_start(out=XB[:, :], in_=PT[c.o_lx + ch * 128:c.o_lx + (ch + 1) * 128, :]), reads=[PT], writes=[XB])
                for d in range(2):
                    k.dma("sp", lambda e: e.dma_start(out=wf[:, d, :], in_=I["lru_wr"][l, d, ch, :, :]), reads=[I["lru_wr"]], writes=[wf])
                    k.dma("sp", lambda e: e.dma_start(out=wf[:, 2 + d, :], in_=I["lru_wi"][l, d, ch, :, :]), reads=[I["lru_wi"]], writes=[wf])
                k.op("dve", lambda: V.tensor_copy(out=wb[:, :, :], in_=wf[:, :, :]), reads=[wf], writes=[wb])
                k.op("act", lambda: S.activation(out=U[:, :], in_=XB[:, :], func=AF.Identity, bias=pc(4), scale=pc(2)), reads=[XB, par], writes=[U])
                for (lo, hi) in ((0, Tc), (Tc, TT)):
                    for kk, sh in ((0, -2), (1, -1), (3, 1)):
                        a0, a1 = max(lo, lo - sh), min(hi, hi - sh)
                        k.op("dve", lambda: V.scalar_tensor_tensor(out=U[:, a0:a1], in0=XB[:, a0 + sh:a1 + sh], scalar=pc(kk), in1=U[:, a0:a1], op0=ALU.mult, op1=ALU.add),
                             reads=[XB, U, par], writes=[U])
                for d in range(2):
                    order = SEGS if d == 0 else [SEGS[0]] + SEGS[1:][::-1]
                    for si, (s0, sz) in enumerate(order):
                        k.op("dve", lambda: V.tensor_copy(out=ub[:, 0:sz], in_=U[:, s0:s0 + sz]), reads=[U], writes=[ub])
                        for p0 in range(0, sz, 512):
                            pw = min(512, sz - p0)
                            pr, pi = nps(), nps()
                            k.op("pe", lambda: A.matmul(pr[:, 0:pw], lhsT=wb[:, d, :], rhs=ub[:, p0:p0 + pw], start=True, stop=True), reads=[wb, ub], writes=[pr])
                            k.op("pe", lambda: A.matmul(pi[:, 0:pw], lhsT=wb[:, 2 + d, :], rhs=ub[:, p0:p0 + pw], start=True, stop=True), reads=[wb, ub], writes=[pi])
                            k.op("act", lambda: S.activation(out=r_[:, p0:p0 + pw], in_=pr[:, 0:pw], func=AF.Sigmoid, bias=pc(7 + d)), reads=[pr, par], writes=[r_])
                            k.op("act", lambda: S.activation(out=i_[:, p0:p0 + pw], in_=pi[:, 0:pw], func=AF.Sigmoid, bias=pc(9 + d)), reads=[pi, par], writes=[i_])
                        k.op("act", lambda: S.activation(out=t_[:, 0:sz], in_=r_[:, 0:sz], func=AF.Exp, scale=pc(13 + d)), reads=[r_, par], writes=[t_])
                        k.op("act", lambda: S.activation(out=r_[:, 0:sz], in_=r_[:, 0:sz], func=AF.Exp, scale=pc(11 + d)), reads=[r_, par], writes=[r_])
                        k.op("dve", lambda: V.tensor_scalar(out=t_[:, 0:sz], in0=t_[:, 0:sz], scalar1=-1.0, scalar2=1.0, op0=ALU.mult, op1=ALU.add), reads=[t_], writes=[t_])
                        k.op("act", lambda: S.activation(out=t_[:, 0:sz], in_=t_[:, 0:sz], func=AF.Sqrt), reads=[t_], writes=[t_])
                        k.op("dve", lambda: V.tensor_tensor(out=i_[:, 0:sz], in0=i_[:, 0:sz], in1=U[:, s0:s0 + sz], op=ALU.mult), reads=[i_, U], writes=[i_])
                        k.op("dve", lambda: V.tensor_tensor(out=i_[:, 0:sz], in0=i_[:, 0:sz], in1=t_[:, 0:sz], op=ALU.mult), reads=[i_, t_], writes=[i_])
                        init = 0.0 if si == 0 else carry[:, 0:1]
                        if d == 0:
                            k.op("dve", lambda: V.tensor_tensor_scan(out=h_[:, 0:sz], data0=r_[:, 0:sz], data1=i_[:, 0:sz], initial=init, op0=ALU.mult, op1=ALU.add),
                                 reads=[r_, i_, carry], writes=[h_])
                            k.op("dve", lambda: V.tensor_copy(out=carry[:, :], in_=h_[:, sz - 1:sz]), reads=[h_], writes=[carry])
                            k.op("act", lambda: S.copy(out=XB[:, s0:s0 + sz], in_=h_[:, 0:sz]), reads=[h_], writes=[XB])
                        else:
                            k.op("dve", lambda: V.tensor_tensor_scan(out=h_[:, sz - 1::-1] if False else h_[:, 0:sz][:, ::-1], data0=r_[:, 0:sz][:, ::-1], data1=i_[:, 0:sz][:, ::-1],
                                                                   initial=init, op0=ALU.mult, op1=ALU.add), reads=[r_, i_, carry], writes=[h_])
                            k.op("dve", lambda: V.tensor_copy(out=carry[:, :], in_=h_[:, 0:1]), reads=[h_], writes=[carry])
                            k.op("dve", lambda: V.tensor_tensor(out=XB[:, s0:s0 + sz], in0=XB[:, s0:s0 + sz], in1=h_[:, 0:sz], op=ALU.add), reads=[h_, XB], writes=[XB])
                for (s0, sz) in SEGS:
                    k.dma("sp", lambda e: e.dma_start(out=r_[:, 0:sz], in_=PT[c.o_lg + ch * 128:c.o_lg + (ch + 1) * 128, s0:s0 + sz]), reads=[PT], writes=[r_])
                    k.op("act", lambda: S.activation(out=t_[:, 0:sz], in_=r_[:, 0:sz], func=AF.Square), reads=[r_], writes=[t_])
                    k.op("dve", lambda: V.tensor_scalar(out=t_[:, 0:sz], in0=t_[:, 0:sz], scalar1=0.044715, scalar2=1.0, op0=ALU.mult, op1=ALU.add), reads=[t_], writes=[t_])
                    k.op("dve", lambda: V.tensor_tensor(out=t_[:, 0:sz], in0=t_[:, 0:sz], in1=r_[:, 0:sz], op=ALU.mult), reads=[t_, r_], writes=[t_])
                    k.op("act", lambda: S.activation(out=t_[:, 0:sz], in_=t_[:, 0:sz], func=AF.Sigmoid, scale=1.5957691216057308), reads=[t_], writes=[t_])
                    k.op("dve", lambda: V.tensor_tensor(out=t_[:, 0:sz], in0=t_[:, 0:sz], in1=r_[:, 0:sz], op=ALU.mult), reads=[t_, r_], writes=[t_])
                    k.op("dve", lambda: V.tensor_tensor(out=yb[:, 0:sz], in0=t_[:, 0:sz], in1=XB[:, s0:s0 + sz], op=ALU.mult), reads=[t_, XB], writes=[yb])
                    k.dma("sp", lambda e: e.dma_start(out=YT[ch * 128:(ch + 1) * 128, s0:s0 + sz], in_=yb[:, 0:sz]), reads=[yb], writes=[YT])


    ones_f = k.sbuf("ones_f", [128, 128], F32)
    k.op("dve", lambda: V.memset(ones_f[:, :], 1.0), writes=[ones_f])

    def phase_hg(l):
        with k.scope():
            SG = max(sz for _, sz in SEGS)
            NTs = SG // 128
            tri = k.sbuf("htri", [128, 2, 128], F32)
            k.op("dve", lambda: V.memset(tri[:, :, :], 0.0), writes=[tri])
            CH = 32
            NB = 128 // CH
            for d in range(2):
                for hb_ in range(NB):
                    k.dma("sp", lambda e: e.dma_start(out=tri[hb_ * CH:(hb_ + 1) * CH, d, hb_ * CH:(hb_ + 1) * CH], in_=I["tri"][d, 0:CH, 0:CH]), reads=[I["tri"]], writes=[tri])
            M = k.sbuf("hM", [128, 2, SG], F32)
            k.op("dve", lambda: V.memset(M[:, :, :], 1.0), writes=[M])
            k.op("dve", lambda: V.memset(M[:, 0, :].rearrange("p (c t) -> p c t", t=CH)[:, :, 0:1], 0.0), writes=[M])
            k.op("dve", lambda: V.memset(M[:, 1, :].rearrange("p (c t) -> p c t", t=CH)[:, :, CH - 1:CH], 0.0), writes=[M])
            lbt = k.sbuf("hlb", [128, 2, L, c.HH], F32); lbc = k.sbuf("hlbc", [128, 2, 2, c.HH], F32)
            gcol = k.sbuf("hgcol", [128, c.HH], F32)
            k.dma("sp", lambda e: e.dma_start(out=gcol[:, :], in_=I["hg_g"][l, :].rearrange("(h p) -> p h", p=128)), reads=[I["hg_g"]], writes=[gcol])
            for d in range(2):
                for ll in range(L):
                    k.dma("sp", lambda e: e.dma_start(out=lbt[:, d, ll, :], in_=I["hg_lb"][d, ll, :].rearrange("(h p) -> p h", p=128)), reads=[I["hg_lb"]], writes=[lbt])
            if l == 0:
                k.op("dve", lambda: V.memset(lbc[:, :, 0, :], 0.0), writes=[lbc])
            else:
                k.op("dve", lambda: V.tensor_tensor(out=lbc[:, :, 0, :], in0=lbt[:, :, 1, :], in1=lbt[:, :, 0, :], op=ALU.subtract), reads=[lbt], writes=[lbc])
                k.op("act", lambda: S.activation(out=lbc[:, :, 0, :], in_=lbc[:, :, 0, :], func=AF.Sigmoid), reads=[lbc], writes=[lbc])
            k.op("dve", lambda: V.tensor_scalar(out=lbc[:, :, 1, :], in0=lbc[:, :, 0, :], scalar1=-1.0, scalar2=1.0, op0=ALU.mult, op1=ALU.add), reads=[lbc], writes=[lbc])
            OS = k.sbuf("hOS", [128, TT], F32)
            q_ = k.sbuf("hq", [128, SG], F32); fr = k.sbuf("hfr", [128, SG], F32); g_ = k.sbuf("hg_", [128, SG], F32); kk_ = k.sbuf("hkk", [128, SG], F32)
            vT = k.sbuf("hvT", [128, SG], F32)
            qt = k.sbuf("hqt", [128, SG], BF16); kt = k.sbuf("hkt", [128, SG], BF16); kh = k.sbuf("hkh", [128, SG], BF16); vb = k.sbuf("hvb", [128, SG], BF16)
            vtok = k.sbuf("hvtok", [128, NTs, 128], BF16); khc = k.sbuf("hkhc", [CH, NTs * NB, 128], BF16); vc = k.sbuf("hvc", [CH, NTs * NB, 128], BF16)
            egl = k.sbuf("hegl", [128, SG // CH], F32)
            Sf = k.sbuf("hSf", [128, 128], F32); Sb = k.sbuf("hSb", [128, 128], BF16)
            ams = [k.sbuf("ham%d" % i, [128, 128], BF16) for i in range(2)]
            sq = k.sbuf("hsq", [128, 512], F32); rs = k.sbuf("hrs", [128, 512], F32); yb = k.sbuf("hyb", [128, 512], BF16)
            for h in range(c.HH):
                for d in range(2):
                    order = SEGS if d == 0 else [SEGS[0]] + SEGS[1:][::-1]
                    k.op("dve", lambda: V.memset(Sf[:, :], 0.0), writes=[Sf])
                    k.op("dve", lambda: V.memset(Sb[:, :], 0.0), writes=[Sb])
                    fo = c.o_hff if d == 0 else c.o_hfb
                    for (s0, sz) in order:
                        nch = sz // CH
                        k.dma("sp", lambda e: e.dma_start(out=q_[:, 0:sz], in_=PT[c.o_hq + h * 128:c.o_hq + (h + 1) * 128, s0:s0 + sz]), reads=[PT], writes=[q_])
                        k.dma("sp", lambda e: e.dma_start(out=fr[:, 0:sz], in_=PT[fo + h * 128:fo + (h + 1) * 128, s0:s0 + sz]), reads=[PT], writes=[fr])
                        k.dma("sp", lambda e: e.dma_start(out=vT[:, 0:sz], in_=PT[c.o_hi + h * 128:c.o_hi + (h + 1) * 128, s0:s0 + sz]), reads=[PT], writes=[vT])
                        k.op("act", lambda: S.activation(out=fr[:, 0:sz], in_=fr[:, 0:sz], func=AF.Sigmoid), reads=[fr], writes=[fr])
                        k.op("dve", lambda: V.tensor_scalar(out=fr[:, 0:sz], in0=fr[:, 0:sz], scalar1=lbc[:, d, 1, h:h + 1], scalar2=lbc[:, d, 0, h:h + 1], op0=ALU.mult, op1=ALU.add),
                             reads=[fr, lbc], writes=[fr])
                        k.op("dve", lambda: V.tensor_scalar(out=kk_[:, 0:sz], in0=fr[:, 0:sz], scalar1=-1.0, scalar2=1.0, op0=ALU.mult, op1=ALU.add), reads=[fr], writes=[kk_])
                        k.op("act", lambda: S.activation(out=fr[:, 0:sz], in_=fr[:, 0:sz], func=AF.Ln), reads=[fr], writes=[fr])
                        if d == 0:
                            k.op("dve", lambda: V.tensor_tensor_scan(out=g_[:, 0:sz], data0=M[:, 0, 0:sz], data1=fr[:, 0:sz], initial=0.0, op0=ALU.mult, op1=ALU.add),
                                 reads=[M, fr], writes=[g_])
                        else:
                            k.op("dve", lambda: V.tensor_tensor_scan(out=g_[:, 0:sz][:, ::-1], data0=M[:, 1, 0:sz][:, ::-1], data1=fr[:, 0:sz][:, ::-1], initial=0.0,
                                                                   op0=ALU.mult, op1=ALU.add), reads=[M, fr], writes=[g_])
                        gl = lambda: g_[:, 0:sz].rearrange("p (c t) -> p c t", t=CH)[:, :, (CH - 1 if d == 0 else 0)]
                        k.op("act", lambda: S.activation(out=egl[:, 0:nch], in_=gl(), func=AF.Exp), reads=[g_], writes=[egl])
                        k.op("act", lambda: S.activation(out=fr[:, 0:sz], in_=g_[:, 0:sz], func=AF.Exp), reads=[g_], writes=[fr])
                        k.op("dve", lambda: V.tensor_tensor(out=qt[:, 0:sz], in0=q_[:, 0:sz], in1=fr[:, 0:sz], op=ALU.mult), reads=[q_, fr], writes=[qt])
                        k.op("act", lambda: S.activation(out=fr[:, 0:sz], in_=g_[:, 0:sz], func=AF.Exp, scale=-1.0), reads=[g_], writes=[fr])
                        k.op("dve", lambda: V.tensor_tensor(out=kk_[:, 0:sz], in0=kk_[:, 0:sz], in1=fr[:, 0:sz], op=ALU.mult), reads=[kk_, fr], writes=[kk_])
                        k.op("act", lambda: S.copy(out=kt[:, 0:sz], in_=kk_[:, 0:sz]), reads=[kk_], writes=[kt])
                        k.op("dve", lambda: V.tensor_tensor(out=kh[:, 0:sz].rearrange("p (c t) -> p c t", t=CH), in0=kk_[:, 0:sz].rearrange("p (c t) -> p c t", t=CH),
                                                            in1=egl[:, 0:nch].unsqueeze(2).to_broadcast([128, nch, CH]), op=ALU.mult), reads=[kk_, egl], writes=[kh])
                        k.op("act", lambda: S.copy(out=vb[:, 0:sz], in_=vT[:, 0:sz]), reads=[vT], writes=[vb])
                        if h == 0 and d == 0 and s0 == 0:
                            _dump(k, c, "g", g_, lambda: g_[:, 0:sz], [128, sz]); _dump(k, c, "qt", qt, lambda: qt[:, 0:sz], [128, sz], BF16)
                            _dump(k, c, "kt", kt, lambda: kt[:, 0:sz], [128, sz], BF16); _dump(k, c, "kh", kh, lambda: kh[:, 0:sz], [128, sz], BF16)
                            _dump(k, c, "egl", egl, lambda: egl[:, 0:nch], [128, nch])
                        if c.hgcut == 1:
                            return
                        nt = sz // 128
                        for src, dst in ((vb, vc), (kh, khc)):
                            for t0 in range(0, nt * NB, 8):
                                n8 = min(8, nt * NB - t0)
                                p = nps(); pb = p[:, :].bitcast(BF16)
                                for j in range(n8):
                                    k.op("pe", lambda: A.transpose(out=pb[0:CH, j * 128:(j + 1) * 128], in_=src[:, (t0 + j) * CH:(t0 + j + 1) * CH], identity=ident_b[:, :]),
                                         reads=[src, ident_b], writes=[p])
                                k.op("act", lambda: S.copy(out=dst[:, t0:t0 + n8, :], in_=pb[0:CH, 0:n8 * 128].rearrange("p (a b) -> p a b", a=n8)), reads=[p], writes=[dst])
                        for src, dst in ((vb, vtok),):
                            for t0 in range(0, nt, 8):
                                n8 = min(8, nt - t0)
                                p = nps(); pb = p[:, :].bitcast(BF16)
                                for j in range(n8):
                                    k.op("pe", lambda: A.transpose(out=pb[:, j * 128:(j + 1) * 128], in_=src[:, (t0 + j) * 128:(t0 + j + 1) * 128], identity=ident_b[:, :]),
                                         reads=[src, ident_b], writes=[p])
                                k.op("dve", lambda: V.tensor_copy(out=dst[:, t0:t0 + n8, :], in_=pb[:, 0:n8 * 128].rearrange("p (a b) -> p a b", a=n8)), reads=[p], writes=[dst])
                        if c.hgcut == 2:
                            return
                        blocks = list(range(0, sz, 512))
                        if d == 1:
                            blocks = blocks[::-1]
                        na = 0
                        for b0 in blocks:
                            bw = min(512, sz - b0)
                            po = npo()
                            tiles = list(range(b0 // 128, (b0 + bw) // 128))
                            if d == 1:
                                tiles = tiles[::-1]
                            for ti in tiles:
                                lo = ti * 128 - b0
                                pa = nps()
                                k.op("pe", lambda: A.matmul(pa[:, 0:128], lhsT=kt[:, ti * 128:(ti + 1) * 128], rhs=qt[:, ti * 128:(ti + 1) * 128], start=True, stop=True),
                                     reads=[kt, qt], writes=[pa])
                                am = ams[na % 2]; na += 1
                                k.op("dve", lambda: V.tensor_tensor(out=am[:, :], in0=pa[:, 0:128], in1=tri[:, d, :], op=ALU.mult), reads=[pa, tri], writes=[am])
                                k.op("pe", lambda: A.matmul(po[:, lo:lo + 128], lhsT=vtok[:, ti, :], rhs=am[:, :], start=True, stop=False), reads=[vtok, am], writes=[po])
                                if c.hgcut == 3:
                                    return
                                chs = list(range(NB)) if d == 0 else list(range(NB))[::-1]
                                for ci, cc in enumerate(chs):
                                    cidx_ = ti * NB + cc
                                    k.op("pe", lambda: A.matmul(po[:, lo + cc * CH:lo + (cc + 1) * CH], lhsT=Sb[:, :], rhs=qt[:, cidx_ * CH:(cidx_ + 1) * CH], start=False, stop=True),
                                         reads=[Sb, qt], writes=[po], pe_acc=True)
                                    if c.hgcut == 4:
                                        return
                                    pst = nps()
                                    k.op("pe", lambda: A.matmul(pst[:, 0:128], lhsT=khc[:, cidx_, :], rhs=vc[:, cidx_, :], start=True, stop=True),
                                         reads=[khc, vc], writes=[pst])
                                    k.op("dve", lambda: V.scalar_tensor_tensor(out=Sf[:, :], in0=Sf[:, :], scalar=egl[:, cidx_:cidx_ + 1], in1=pst[:, 0:128], op0=ALU.mult, op1=ALU.add),
                                         reads=[Sf, egl, pst], writes=[Sf])
                                    k.op("act", lambda: S.copy(out=Sb[:, :], in_=Sf[:, :]), reads=[Sf], writes=[Sb])
                            if d == 0:
                                k.op("dve", lambda: V.tensor_copy(out=OS[:, s0 + b0:s0 + b0 + bw], in_=po[:, 0:bw]), reads=[po], writes=[OS])
                            else:
                                k.op("dve", lambda: V.tensor_tensor(out=OS[:, s0 + b0:s0 + b0 + bw], in0=OS[:, s0 + b0:s0 + b0 + bw], in1=po[:, 0:bw], op=ALU.add), reads=[po, OS], writes=[OS])
                if h == 0:
                    _dump(k, c, "OS", OS, lambda: OS[:, :], [128, TT])
                for b0 in range(0, TT, 512):
                    bw = min(512, TT - b0)
                    k.op("act", lambda: S.activation(out=sq[:, 0:bw], in_=OS[:, b0:b0 + bw], func=AF.Square), reads=[OS], writes=[sq])
                    p = nps()
                    k.op("pe", lambda: A.matmul(p[:, 0:bw], lhsT=ones_f[:, :], rhs=sq[:, 0:bw], start=True, stop=True), reads=[ones_f, sq], writes=[p])
                    k.op("dve", lambda: V.tensor_scalar(out=rs[:, 0:bw], in0=p[:, 0:bw], scalar1=1.0 / 128, scalar2=EPS, op0=ALU.mult, op1=ALU.add), reads=[p], writes=[rs])
                    k.op("act", lambda: S.activation(out=rs[:, 0:bw], in_=rs[:, 0:bw], func=AF.Sqrt), reads=[rs], writes=[rs])
                    k.op("dve", lambda: V.reciprocal(out=rs[:, 0:bw], in_=rs[:, 0:bw]), reads=[rs], writes=[rs])
                    k.op("dve", lambda: V.scalar_tensor_tensor(out=rs[:, 0:bw], in0=rs[:, 0:bw], scalar=gcol[:, h:h + 1], in1=OS[:, b0:b0 + bw], op0=ALU.mult, op1=ALU.mult),
                         reads=[rs, gcol, OS], writes=[rs])
                    k.dma("sp", lambda e: e.dma_start(out=sq[:, 0:bw], in_=PT[c.o_hg + h * 128:c.o_hg + (h + 1) * 128, b0:b0 + bw]), reads=[PT], writes=[sq])
                    k.op("act", lambda: S.activation(out=sq[:, 0:bw], in_=sq[:, 0:bw], func=AF.Silu), reads=[sq], writes=[sq])
                    k.op("dve", lambda: V.tensor_tensor(out=yb[:, 0:bw], in0=rs[:, 0:bw], in1=sq[:, 0:bw], op=ALU.mult), reads=[rs, sq], writes=[yb])
                    k.dma("sp", lambda e: e.dma_start(out=YT[c.LW + h * 128:c.LW + (h + 1) * 128, b0:b0 + bw], in_=yb[:, 0:bw]), reads=[yb], writes=[YT])


    QT = k.dram("QT", [c.MH * 192, TT], BF16)
    KT = k.dram("KT", [c.MH * 128, TT], BF16)
    KRT = k.dram("KRT", [64, TT], BF16)
    VV = k.dram("VV", [TT, c.MH * 128], BF16)
    rt_f = k.sbuf("rt_f", [128, 128], F32)
    k.dma("sp", lambda e: e.dma_start(out=rt_f[:, :], in_=I["rope_rt"][:, :]), reads=[I["rope_rt"]], writes=[rt_f])

    def rope_apply(p, nrow, ntok, cs, xs, t1, outb):
        k.op("act", lambda: S.copy(out=xs[0:nrow, 0:ntok], in_=p[0:nrow, 0:ntok]), reads=[p], writes=[xs])
        p2 = nps()
        k.op("pe", lambda: A.matmul(p2[0:nrow, 0:ntok], lhsT=rt_f[0:nrow, 0:nrow], rhs=xs[0:nrow, 0:ntok], start=True, stop=True), reads=[rt_f, xs], writes=[p2])
        k.op("dve", lambda: V.tensor_tensor(out=t1[0:nrow, 0:ntok], in0=xs[0:nrow, 0:ntok], in1=cs[0:nrow, 0, 0:ntok], op=ALU.mult), reads=[xs, cs], writes=[t1])
        k.op("dve", lambda: V.tensor_tensor(out=xs[0:nrow, 0:ntok], in0=p2[0:nrow, 0:ntok], in1=cs[0:nrow, 1, 0:ntok], op=ALU.mult), reads=[p2, cs], writes=[xs])
        k.op("dve", lambda: V.tensor_tensor(out=outb[0:nrow, 0:ntok], in0=t1[0:nrow, 0:ntok], in1=xs[0:nrow, 0:ntok], op=ALU.add), reads=[t1, xs], writes=[outb])

    def phase_mla_proj(l):
        QC, VC = c.QR // 128, c.KVR // 128
        with k.scope():
            wuq = k.sbuf("wuq", [128, QC, c.MH * 192], BF16); wukv = k.sbuf("wukv", [128, VC, c.MH * 256], BF16)
            wtmp = k.sbuf("wtmp", [128, max(c.MH * 192, c.MH * 256)], F32)
            for kc in range(QC):
                k.dma("sp", lambda e: e.dma_start(out=wtmp[:, 0:c.MH * 192], in_=I["w_uq"][l, kc * 128:(kc + 1) * 128, :]), reads=[I["w_uq"]], writes=[wtmp])
                k.op("dve", lambda: V.tensor_copy(out=wuq[:, kc, :], in_=wtmp[:, 0:c.MH * 192]), reads=[wtmp], writes=[wuq])
            for kc in range(VC):
                k.dma("sp", lambda e: e.dma_start(out=wtmp[:, 0:c.MH * 256], in_=I["w_ukv"][l, kc * 128:(kc + 1) * 128, :]), reads=[I["w_ukv"]], writes=[wtmp])
                k.op("dve", lambda: V.tensor_copy(out=wukv[:, kc, :], in_=wtmp[:, 0:c.MH * 256]), reads=[wtmp], writes=[wukv])
            gq = k.sbuf("gq", [128, QC], F32); gkv = k.sbuf("gkv", [128, VC], F32)
            k.dma("sp", lambda e: e.dma_start(out=gq[:, :], in_=I["q_g"][l, :].rearrange("(c p) -> p c", p=128)), reads=[I["q_g"]], writes=[gq])
            k.dma("sp", lambda e: e.dma_start(out=gkv[:, :], in_=I["kv_g"][l, :].rearrange("(c p) -> p c", p=128)), reads=[I["kv_g"]], writes=[gkv])
            xin = k.sbuf("mxin", [128, QC, 512], F32); xn = k.sbuf("mxn", [128, QC, 512], BF16)
            sq = k.sbuf("msq", [128, 512], F32); rs = k.sbuf("mrs", [128, 512], F32)
            cs = k.sbuf("mcs", [128, 2, 512], F32); xs = k.sbuf("mxs", [128, 512], F32); t1 = k.sbuf("mt1", [128, 512], F32)
            obs = [k.sbuf("mob%d" % i, [128, 512], BF16) for i in range(3)]
            no = [0]

            def normed(row0, nchunk, gcol, ntok, tok0):
                k.dma("sp", lambda e: e.dma_start(out=xin[:, 0:nchunk, 0:ntok], in_=PT[row0:row0 + nchunk * 128, tok0:tok0 + ntok].rearrange("(c p) t -> p c t", p=128)),
                      reads=[PT], writes=[xin])
                p = nps()
                for cc in range(nchunk):
                    k.op("act", lambda: S.activation(out=sq[:, 0:ntok], in_=xin[:, cc, 0:ntok], func=AF.Square), reads=[xin], writes=[sq])
                    k.op("pe", lambda: A.matmul(p[:, 0:ntok], lhsT=ones_f[:, :], rhs=sq[:, 0:ntok], start=(cc == 0), stop=(cc == nchunk - 1)), reads=[ones_f, sq], writes=[p], pe_acc=(cc > 0))
                k.op("dve", lambda: V.tensor_scalar(out=rs[:, 0:ntok], in0=p[:, 0:ntok], scalar1=1.0 / (nchunk * 128), scalar2=EPS, op0=ALU.mult, op1=ALU.add), reads=[p], writes=[rs])
                k.op("act", lambda: S.activation(out=rs[:, 0:ntok], in_=rs[:, 0:ntok], func=AF.Sqrt), reads=[rs], writes=[rs])
                k.op("dve", lambda: V.reciprocal(out=rs[:, 0:ntok], in_=rs[:, 0:ntok]), reads=[rs], writes=[rs])
                for cc in range(nchunk):
                    k.op("dve", lambda: V.scalar_tensor_tensor(out=xn[:, cc, 0:ntok], in0=xin[:, cc, 0:ntok], scalar=gcol[:, cc:cc + 1], in1=rs[:, 0:ntok], op0=ALU.mult, op1=ALU.mult),
                         reads=[xin, gcol, rs], writes=[xn])

            def nob():
                no[0] += 1
                return obs[no[0] % 3]

            for (tok0, ntok, is_ctx) in c.blocks:
                if not is_ctx:
                    k.dma("sp", lambda e: e.dma_start(out=cs[:, :, 0:ntok], in_=I["rope_cs"][:, :, tok0 - Tc:tok0 - Tc + ntok].rearrange("a p t -> p a t")), reads=[I["rope_cs"]], writes=[cs])
                normed(c.o_cq, QC, gq, ntok, tok0)
                for h in range(c.MH):
                    for (c0, w, rope) in ((h * 192, 128, False), (h * 192 + 128, 64, True)):
                        p = nps()
                        for kc in range(QC):
                            k.op("pe", lambda: A.matmul(p[0:w, 0:ntok], lhsT=wuq[:, kc, c0:c0 + w], rhs=xn[:, kc, 0:ntok], start=(kc == 0), stop=(kc == QC - 1)),
                                 reads=[wuq, xn], writes=[p], pe_acc=(kc > 0))
                        ob = nob()
                        if rope and not is_ctx:
                            rope_apply(p, w, ntok, cs, xs, t1, ob)
                        else:
                            k.op("act", lambda: S.copy(out=ob[0:w, 0:ntok], in_=p[0:w, 0:ntok]), reads=[p], writes=[ob])
                        k.dma("sp", lambda e: e.dma_start(out=QT[c0:c0 + w, tok0:tok0 + ntok], in_=ob[0:w, 0:ntok]), reads=[ob], writes=[QT])
                normed(c.o_ckv, VC, gkv, ntok, tok0)
                for h in range(c.MH):
                    p = nps()
                    for kc in range(VC):
                        k.op("pe", lambda: A.matmul(p[:, 0:ntok], lhsT=wukv[:, kc, h * 256:h * 256 + 128], rhs=xn[:, kc, 0:ntok], start=(kc == 0), stop=(kc == VC - 1)),
                             reads=[wukv, xn], writes=[p], pe_acc=(kc > 0))
                    ob = nob()
                    k.op("act", lambda: S.copy(out=ob[:, 0:ntok], in_=p[:, 0:ntok]), reads=[p], writes=[ob])
                    k.dma("sp", lambda e: e.dma_start(out=KT[h * 128:(h + 1) * 128, tok0:tok0 + ntok], in_=ob[:, 0:ntok]), reads=[ob], writes=[KT])
                for ti in range(ntok // 128):
                    for h0 in range(0, c.MH, 4):
                        nh = min(4, c.MH - h0)
                        p = nps()
                        for hh in range(nh):
                            for kc in range(VC):
                                k.op("pe", lambda: A.matmul(p[:, hh * 128:(hh + 1) * 128], lhsT=xn[:, kc, ti * 128:(ti + 1) * 128], rhs=wukv[:, kc, (h0 + hh) * 256 + 128:(h0 + hh) * 256 + 256],
                                                            start=(kc == 0), stop=(kc == VC - 1)), reads=[wukv, xn], writes=[p], pe_acc=(kc > 0 or hh > 0))
                        ob = nob()
                        k.op("dve", lambda: V.tensor_copy(out=ob[:, 0:nh * 128], in_=p[:, 0:nh * 128]), reads=[p], writes=[ob])
                        k.dma("sp", lambda e: e.dma_start(out=VV[tok0 + ti * 128:tok0 + (ti + 1) * 128, h0 * 128:(h0 + nh) * 128], in_=ob[:, 0:nh * 128]), reads=[ob], writes=[VV])
                k.dma("sp", lambda e: e.dma_start(out=xs[0:64, 0:ntok], in_=PT[c.o_kr:c.o_kr + 64, tok0:tok0 + ntok]), reads=[PT], writes=[xs])
                ob = nob()
                if is_ctx:
                    k.op("act", lambda: S.copy(out=ob[0:64, 0:ntok], in_=xs[0:64, 0:ntok]), reads=[xs], writes=[ob])
                else:
                    p2 = nps()
                    k.op("pe", lambda: A.matmul(p2[0:64, 0:ntok], lhsT=rt_f[0:64, 0:64], rhs=xs[0:64, 0:ntok], start=True, stop=True), reads=[rt_f, xs], writes=[p2])
                    k.op("dve", lambda: V.tensor_tensor(out=t1[0:64, 0:ntok], in0=xs[0:64, 0:ntok], in1=cs[0:64, 0, 0:ntok], op=ALU.mult), reads=[xs, cs], writes=[t1])
                    k.op("dve", lambda: V.tensor_tensor(out=sq[0:64, 0:ntok], in0=p2[0:64, 0:ntok], in1=cs[0:64, 1, 0:ntok], op=ALU.mult), reads=[p2, cs], writes=[sq])
                    k.op("dve", lambda: V.tensor_tensor(out=ob[0:64, 0:ntok], in0=t1[0:64, 0:ntok], in1=sq[0:64, 0:ntok], op=ALU.add), reads=[t1, sq], writes=[ob])
                k.dma("sp", lambda e: e.dma_start(out=KRT[:, tok0:tok0 + ntok], in_=ob[0:64, 0:ntok]), reads=[ob], writes=[KRT])

    def attention_core(q_tiles, nq0, q0_fn, q1_fn, key_parts, v_fn, scale, out_fn, mask_fn=None, sink_col=None, dv=128, st=None):
        pass

    def phase_mla_attn(l, need_ctx):
        NT = TT // 128
        scale = 192.0 ** -0.5
        with k.scope():
            kt0 = k.sbuf("akt0", [128, TT], BF16); kt1 = k.sbuf("akt1", [64, TT], BF16); vtok = k.sbuf("avtok", [128, NT, 128], BF16)
            Ssb = k.sbuf("aS", [128, TT], F32); Pb = k.sbuf("aP", [128, TT], BF16); PTr = k.sbuf("aPT", [128, NT, 128], BF16)
            q0s = [k.sbuf("aq0%d" % i, [128, 128], BF16) for i in range(2)]; q1s = [k.sbuf("aq1%d" % i, [64, 128], BF16) for i in range(2)]
            mx = k.sbuf("amx", [128, 1], F32); rsum = k.sbuf("ars", [128, 1], F32); ob = k.sbuf("aob", [128, 128], BF16); oT = k.sbuf("aoT", [128, 512], BF16)
            k.dma("sp", lambda e: e.dma_start(out=kt1[:, :], in_=KRT[:, :]), reads=[KRT], writes=[kt1])
            nq = 0
            for h in range(c.MH):
                k.dma("sp", lambda e: e.dma_start(out=kt0[:, :], in_=KT[h * 128:(h + 1) * 128, :]), reads=[KT], writes=[kt0])
                for tq in range(0, NT, 8):
                    tn = min(8, NT - tq)
                    k.dma("sp", lambda e: e.dma_start(out=vtok[:, tq:tq + tn, :], in_=VV[tq * 128:(tq + tn) * 128, h * 128:(h + 1) * 128].rearrange("(t p) v -> p t v", p=128)), reads=[VV], writes=[vtok])
                qtiles = ([(i * 128, 0, Tc) for i in range(Tc // 128)] if need_ctx else []) + [(Tc + i * 128, 0, TT) for i in range(T // 128)]
                for qi, (qs, k0, k1) in enumerate(qtiles):
                    q0, q1 = q0s[nq % 2], q1s[nq % 2]; nq += 1
                    k.dma("sp", lambda e: e.dma_start(out=q0[:, :], in_=QT[h * 192:h * 192 + 128, qs:qs + 128]), reads=[QT], writes=[q0])
                    k.dma("sp", lambda e: e.dma_start(out=q1[:, :], in_=QT[h * 192 + 128:h * 192 + 192, qs:qs + 128]), reads=[QT], writes=[q1])
                    nk = k1 - k0
                    for kb in range(k0, k1, 512):
                        kw = min(512, k1 - kb)
                        p = nps()
                        k.op("pe", lambda: A.matmul(p[:, 0:kw], lhsT=q0[:, :], rhs=kt0[:, kb:kb + kw], start=True, stop=False), reads=[q0, kt0], writes=[p])
                        k.op("pe", lambda: A.matmul(p[:, 0:kw], lhsT=q1[:, :], rhs=kt1[:, kb:kb + kw], start=False, stop=True), reads=[q1, kt1], writes=[p], pe_acc=True)
                        if (kb // 512) % 2:
                            k.op("act", lambda: S.activation(out=Ssb[:, kb:kb + kw], in_=p[:, 0:kw], func=AF.Copy, scale=scale), reads=[p], writes=[Ssb])
                        else:
                            k.op("dve", lambda: V.tensor_scalar(out=Ssb[:, kb:kb + kw], in0=p[:, 0:kw], scalar1=scale, scalar2=None, op0=ALU.mult), reads=[p], writes=[Ssb])
                    k.op("dve", lambda: V.tensor_reduce(out=mx[:, :], in_=Ssb[:, k0:k1], axis=AX.X, op=ALU.max), reads=[Ssb], writes=[mx])
                    k.op("dve", lambda: V.tensor_scalar(out=mx[:, :], in0=mx[:, :], scalar1=-1.0, scalar2=None, op0=ALU.mult), reads=[mx], writes=[mx])
                    k.op("act", lambda: S.activation(out=Pb[:, k0:k1], in_=Ssb[:, k0:k1], func=AF.Exp, bias=mx[:, 0:1], accum_out=rsum[:, 0:1]), reads=[Ssb, mx], writes=[Pb, rsum])
                    kts = list(range(k0 // 128, k1 // 128))
                    for t0 in range(0, len(kts), 8):
                        n8 = min(8, len(kts) - t0)
                        p = nps(); pb = p[:, :].bitcast(BF16)
                        for j in range(n8):
                            kt_ = kts[t0 + j]
                            k.op("pe", lambda: A.transpose(out=pb[:, j * 128:(j + 1) * 128], in_=Pb[:, kt_ * 128:(kt_ + 1) * 128], identity=ident_b[:, :]), reads=[Pb, ident_b], writes=[p])
                        tt0 = kts[t0]
                        if (t0 // 8) % 2:
                            k.op("act", lambda: S.copy(out=PTr[:, tt0:tt0 + n8, :], in_=pb[:, 0:n8 * 128].rearrange("p (a b) -> p a b", a=n8)), reads=[p], writes=[PTr])
                        else:
                            k.op("dve", lambda: V.tensor_copy(out=PTr[:, tt0:tt0 + n8, :], in_=pb[:, 0:n8 * 128].rearrange("p (a b) -> p a b", a=n8)), reads=[p], writes=[PTr])
                    po = npo()
                    for j, kt_ in enumerate(kts):
                        k.op("pe", lambda: A.matmul(po[:, 0:128], lhsT=PTr[:, kt_, :], rhs=vtok[:, kt_, :], start=(j == 0), stop=(j == len(kts) - 1)), reads=[PTr, vtok], writes=[po], pe_acc=(j > 0))
                    k.op("dve", lambda: V.reciprocal(out=rsum[:, :], in_=rsum[:, :]), reads=[rsum], writes=[rsum])
                    k.op("dve", lambda: V.tensor_scalar(out=ob[:, :], in0=po[:, 0:128], scalar1=rsum[:, 0:1], scalar2=None, op0=ALU.mult), reads=[po, rsum], writes=[ob])
                    p = nps(); pb = p[:, :].bitcast(BF16)
                    k.op("pe", lambda: A.transpose(out=pb[:, 0:128], in_=ob[:, :], identity=ident_b[:, :]), reads=[ob, ident_b], writes=[p])
                    k.op("act", lambda: S.copy(out=oT[:, 0:128], in_=pb[:, 0:128]), reads=[p], writes=[oT])
                    r0 = c.LW + c.HK + h * 128
                    k.dma("sp", lambda e: e.dma_start(out=YT[r0:r0 + 128, qs:qs + 128], in_=oT[:, 0:128]), reads=[oT], writes=[YT])

    def phase_mla(l, need_ctx=True):
        phase_mla_proj(l)
        phase_mla_attn(l, need_ctx)


    def phase_win(l, need_ctx=True):
        NT = TT // 128
        scale = 64.0 ** -0.5
        G = c.WH // c.WKV
        with k.scope():
            wm = k.sbuf("wwm", [128, 384], F32)
            k.dma("sp", lambda e: e.dma_start(out=wm[:, :], in_=I["wmask"][:, :]), reads=[I["wmask"]], writes=[wm])
            sinkc = k.sbuf("wsink", [128, c.WH], F32)
            k.dma("sp", lambda e: e.dma_start(out=sinkc[:, :], in_=I["sink"][l:l + 1, :].partition_broadcast(128)), reads=[I["sink"]], writes=[sinkc])
            cs = k.sbuf("wcs", [128, 2, 512], F32); xs = k.sbuf("wxs", [128, 512], F32); t1 = k.sbuf("wt1", [128, 512], F32); t2 = k.sbuf("wt2", [128, 512], F32)
            xin = k.sbuf("wxin", [64, 512], F32)
            kb = k.sbuf("wkb", [64, TT], BF16); vb = k.sbuf("wvb", [64, TT], BF16); vtok = k.sbuf("wvtok", [128, NT, 64], BF16); qb = k.sbuf("wqb", [64, TT], BF16)
            Ssb = k.sbuf("wS", [128, 384 + Tc], F32); Pb = k.sbuf("wP", [128, 384 + Tc], BF16); PTr = k.sbuf("wPT", [128, (384 + Tc) // 128, 128], BF16)
            mx = k.sbuf("wmx", [128, 1], F32); rsum = k.sbuf("wrs", [128, 1], F32); es = k.sbuf("wes", [128, 1], F32)
            ob = k.sbuf("wob", [128, 64], BF16); oT = k.sbuf("woT", [64, 128], BF16)

            def load_rope(row0, dst, do_rope=True):
                for (tok0, ntok, is_ctx) in c.blocks:
                    k.dma("sp", lambda e: e.dma_start(out=xin[:, 0:ntok], in_=PT[row0:row0 + 64, tok0:tok0 + ntok]), reads=[PT], writes=[xin])
                    if is_ctx or not do_rope:
                        k.op("act", lambda: S.copy(out=dst[:, tok0:tok0 + ntok], in_=xin[:, 0:ntok]), reads=[xin], writes=[dst])
                    else:
                        k.dma("sp", lambda e: e.dma_start(out=cs[:, :, 0:ntok], in_=I["rope_cs"][:, :, tok0 - Tc:tok0 - Tc + ntok].rearrange("a p t -> p a t")), reads=[I["rope_cs"]], writes=[cs])
                        p2 = nps()
                        k.op("pe", lambda: A.matmul(p2[0:64, 0:ntok], lhsT=rt_f[0:64, 0:64], rhs=xin[:, 0:ntok], start=True, stop=True), reads=[rt_f, xin], writes=[p2])
                        k.op("dve", lambda: V.tensor_tensor(out=t1[0:64, 0:ntok], in0=xin[:, 0:ntok], in1=cs[0:64, 0, 0:ntok], op=ALU.mult), reads=[xin, cs], writes=[t1])
                        k.op("dve", lambda: V.tensor_tensor(out=t2[0:64, 0:ntok], in0=p2[0:64, 0:ntok], in1=cs[0:64, 1, 0:ntok], op=ALU.mult), reads=[p2, cs], writes=[t2])
                        k.op("dve", lambda: V.tensor_tensor(out=dst[:, tok0:tok0 + ntok], in0=t1[0:64, 0:ntok], in1=t2[0:64, 0:ntok], op=ALU.add), reads=[t1, t2], writes=[dst])

            for kv in range(c.WKV):
                load_rope(c.o_wk + kv * 64, kb)
                load_rope(c.o_wv + kv * 64, vb, do_rope=False)
                for t0 in range(0, NT, 8):
                    n8 = min(8, NT - t0)
                    p = nps(); pb = p[:, :].bitcast(BF16)
                    for j in range(n8):
                        k.op("pe", lambda: A.transpose(out=pb[:, j * 64:(j + 1) * 64], in_=vb[:, (t0 + j) * 128:(t0 + j + 1) * 128], identity=ident_b[0:64, 0:64]), reads=[vb, ident_b], writes=[p])
                    k.op("dve", lambda: V.tensor_copy(out=vtok[:, t0:t0 + n8, :], in_=pb[:, 0:n8 * 64].rearrange("p (a b) -> p a b", a=n8)), reads=[p], writes=[vtok])
                for g in range(G):
                    h = kv * G + g
                    load_rope(c.o_wq + h * 64, qb)
                    qtiles = []
                    if need_ctx:
                        qtiles += [(i * 128, [(0, Tc, None)]) for i in range(Tc // 128)]
                    for b in range(T // 128):
                        lo, hi = max(0, b * 128 - 128), min(T, b * 128 + 256)
                        qtiles.append((Tc + b * 128, [(Tc + lo, hi - lo, lo - (b * 128 - 128)), (0, Tc, None)]))
                    for (qs, parts) in qtiles:
                        off = 0
                        for (ks, kl, moff) in parts:
                            p = nps()
                            k.op("pe", lambda: A.matmul(p[:, 0:kl], lhsT=qb[:, qs:qs + 128], rhs=kb[:, ks:ks + kl], start=True, stop=True), reads=[qb, kb], writes=[p])
                            if moff is not None:
                                k.op("dve", lambda: V.scalar_tensor_tensor(out=Ssb[:, off:off + kl], in0=p[:, 0:kl], scalar=scale, in1=wm[:, moff:moff + kl], op0=ALU.mult, op1=ALU.add),
                                     reads=[p, wm], writes=[Ssb])
                            else:
                                k.op("act", lambda: S.activation(out=Ssb[:, off:off + kl], in_=p[:, 0:kl], func=AF.Copy, scale=scale), reads=[p], writes=[Ssb])
                            off += kl
                        nk = off
                        k.op("dve", lambda: V.tensor_reduce(out=mx[:, :], in_=Ssb[:, 0:nk], axis=AX.X, op=ALU.max), reads=[Ssb], writes=[mx])
                        k.op("dve", lambda: V.tensor_tensor(out=mx[:, :], in0=mx[:, :], in1=sinkc[:, h:h + 1], op=ALU.max), reads=[mx, sinkc], writes=[mx])
                        k.op("dve", lambda: V.tensor_scalar(out=mx[:, :], in0=mx[:, :], scalar1=-1.0, scalar2=None, op0=ALU.mult), reads=[mx], writes=[mx])
                        k.op("act", lambda: S.activation(out=Pb[:, 0:nk], in_=Ssb[:, 0:nk], func=AF.Exp, bias=mx[:, 0:1], accum_out=rsum[:, 0:1]), reads=[Ssb, mx], writes=[Pb, rsum])
                        k.op("act", lambda: S.activation(out=es[:, :], in_=sinkc[:, h:h + 1], func=AF.Exp, bias=mx[:, 0:1]), reads=[sinkc, mx], writes=[es])
                        k.op("dve", lambda: V.tensor_tensor(out=rsum[:, :], in0=rsum[:, :], in1=es[:, :], op=ALU.add), reads=[rsum, es], writes=[rsum])
                        k.op("dve", lambda: V.reciprocal(out=rsum[:, :], in_=rsum[:, :]), reads=[rsum], writes=[rsum])
                        nkt = nk // 128
                        p = nps(); pb = p[:, :].bitcast(BF16)
                        for j in range(nkt):
                            k.op("pe", lambda: A.transpose(out=pb[:, j * 128:(j + 1) * 128], in_=Pb[:, j * 128:(j + 1) * 128], identity=ident_b[:, :]), reads=[Pb, ident_b], writes=[p])
                        k.op("act", lambda: S.copy(out=PTr[:, 0:nkt, :], in_=pb[:, 0:nkt * 128].rearrange("p (a b) -> p a b", a=nkt)), reads=[p], writes=[PTr])
                        po = npo()
                        vt_ids = []
                        for (ks, kl, moff) in parts:
                            vt_ids += list(range(ks // 128, (ks + kl) // 128))
                        for j, vt in enumerate(vt_ids):
                            k.op("pe", lambda: A.matmul(po[:, 0:64], lhsT=PTr[:, j, :], rhs=vtok[:, vt, :], start=(j == 0), stop=(j == nkt - 1)), reads=[PTr, vtok], writes=[po], pe_acc=(j > 0))
                        k.op("dve", lambda: V.tensor_scalar(out=ob[:, :], in0=po[:, 0:64], scalar1=rsum[:, 0:1], scalar2=None, op0=ALU.mult), reads=[po, rsum], writes=[ob])
                        p = nps(); pb = p[:, :].bitcast(BF16)
                        k.op("pe", lambda: A.transpose(out=pb[0:64, 0:128], in_=ob[:, :], identity=ident_b[:, :]), reads=[ob, ident_b], writes=[p])
                        k.op("act", lambda: S.copy(out=oT[:, :], in_=pb[0:64, 0:128]), reads=[p], writes=[oT])
                        r0 = c.LW + c.HK + c.MH * 128 + h * 64
                        k.dma("sp", lambda e: e.dma_start(out=YT[r0:r0 + 64, qs:qs + 128], in_=oT[:, :]), reads=[oT], writes=[YT])


    H2 = k.dram("H2", [TT, D], BF16)
    AFFT = k.dram("AFFT", [c.NE, TT], F32)
    MO = k.dram("MO", [TT, D], F32)

    def phase_out(l, need_ctx):
        DC = c.DMIX // 128
        with k.scope():
            ga = k.sbuf("oga", [128, D], F32); A2 = k.sbuf("oA2", [128, D], F32); S2 = k.sbuf("oS2", [128, D], F32)
            xts = [k.sbuf("oxt%d" % i, [128, D], F32) for i in range(2)]
            yT = k.sbuf("oyT", [128, DC, 256], BF16)
            wos = [k.sbuf("owo%d" % i, [128, DC, 256], BF16) for i in range(2)]
            tmp = k.sbuf("otmp", [128, 256], F32)
            hb = k.sbuf("ohb", [128, D], BF16); hT = k.sbuf("ohT", [128, KC, 128], F32)
            ss = k.sbuf("oss", [128, 1], F32); st = k.sbuf("ost", [128, 1], F32)
            wr = k.sbuf("owr", [128, KC, c.NE], F32)
            for kq in range(0, KC, 8):
                k8 = min(8, KC - kq)
                k.dma("sp", lambda e: e.dma_start(out=wr[:, kq:kq + k8, :], in_=I["w_router"][l, kq * 128:(kq + k8) * 128, :].rearrange("(kc p) n -> p kc n", p=128)), reads=[I["w_router"]], writes=[wr])
            lg = k.sbuf("olg", [128, c.NE], F32); lgT = k.sbuf("olgT", [c.NE, 128], F32)
            cur_v = None
            nwo = 0
            blocks = []
            for (tok0, ntok, is_ctx) in c.blocks:
                if is_ctx and not need_ctx:
                    continue
                for b0 in range(0, ntok, 256):
                    blocks.append((tok0 + b0, min(256, ntok - b0), is_ctx))
            for (tok0, ntok, is_ctx) in blocks:
                v = 1 if is_ctx else 0
                if v != cur_v:
                    bc_load(ga, (l * 2 + v) * 6 + 2); bc_load(A2, (l * 2 + v) * 6 + 4); bc_load(S2, (l * 2 + v) * 6 + 3); cur_v = v
                nt = ntok // 128
                for kq in range(0, DC, 8):
                    k8 = min(8, DC - kq)
                    k.dma("sp", lambda e: e.dma_start(out=yT[:, kq:kq + k8, 0:ntok], in_=YT[kq * 128:(kq + k8) * 128, tok0:tok0 + ntok].rearrange("(kc p) t -> p kc t", p=128)), reads=[YT], writes=[yT])
                for ti in range(nt):
                    k.dma("sp", lambda e: e.dma_start(out=xts[ti][:, :], in_=X[tok0 + ti * 128:tok0 + (ti + 1) * 128, :]), reads=[X], writes=[xts[ti]])
                for n0 in range(0, D, 256):
                    wo = wos[nwo % 2]; nwo += 1
                    for kq in range(0, DC, 8):
                        k8 = min(8, DC - kq)
                        k.dma("sp", lambda e: e.dma_start(out=wo[:, kq:kq + k8, :], in_=WOUTB[l * c.DMIX + kq * 128:l * c.DMIX + (kq + k8) * 128, n0:n0 + 256].rearrange("(kc p) n -> p kc n", p=128)), reads=[WOUTB], writes=[wo])
                    for ti in range(nt):
                        p = nps()
                        for kc in range(DC):
                            k.op("pe", lambda: A.matmul(p[:, 0:256], lhsT=yT[:, kc, ti * 128:(ti + 1) * 128], rhs=wo[:, kc, :], start=(kc == 0), stop=(kc == DC - 1)),
                                 reads=[yT, wo], writes=[p], pe_acc=(kc > 0))
                        k.op("dve", lambda: V.tensor_tensor(out=tmp[:, :], in0=p[:, 0:256], in1=ga[:, n0:n0 + 256], op=ALU.mult), reads=[p, ga], writes=[tmp])
                        k.op("pool", lambda: nc.gpsimd.tensor_tensor(out=xts[ti][:, n0:n0 + 256], in0=xts[ti][:, n0:n0 + 256], in1=tmp[:, :], op=ALU.add), reads=[tmp, xts[ti]], writes=[xts[ti]])
                for ti in range(nt):
                    xt = xts[ti]
                    r0 = tok0 + ti * 128
                    k.dma("sp", lambda e: e.dma_start(out=X[r0:r0 + 128, :], in_=xt[:, :]), reads=[xt], writes=[X])
                    k.op("act", lambda: S.activation(out=hb[:, :], in_=xt[:, :], func=AF.Square, accum_out=ss[:, 0:1]), reads=[xt], writes=[hb, ss])
                    rstd_of(ss, D, st)
                    k.op("dve", lambda: V.scalar_tensor_tensor(out=xt[:, :], in0=xt[:, :], scalar=ss[:, 0:1], in1=A2[:, :], op0=ALU.mult, op1=ALU.mult), reads=[xt, ss, A2], writes=[xt])
                    k.op("dve", lambda: V.tensor_tensor(out=xt[:, :], in0=xt[:, :], in1=S2[:, :], op=ALU.add), reads=[xt, S2], writes=[xt])
                    k.op("act", lambda: S.copy(out=hb[:, :], in_=xt[:, :]), reads=[xt], writes=[hb])
                    k.dma("sp", lambda e: e.dma_start(out=H2[r0:r0 + 128, :], in_=hb[:, :]), reads=[hb], writes=[H2])
                    for k0 in range(0, KC, 4):
                        n4 = min(4, KC - k0)
                        p = nps()
                        for j in range(n4):
                            k.op("pe", lambda: A.transpose(out=p[:, j * 128:(j + 1) * 128], in_=xt[:, (k0 + j) * 128:(k0 + j + 1) * 128], identity=ident_f[:, :]), reads=[xt, ident_f], writes=[p])
                        k.op("act", lambda: S.copy(out=hT[:, k0:k0 + n4, :], in_=p[:, 0:n4 * 128].rearrange("p (a b) -> p a b", a=n4)), reads=[p], writes=[hT])
                    p = nps()
                    for kc in range(KC):
                        k.op("pe", lambda: A.matmul(p[:, 0:c.NE], lhsT=hT[:, kc, :], rhs=wr[:, kc, :], start=(kc == 0), stop=(kc == KC - 1)), reads=[hT, wr], writes=[p], pe_acc=(kc > 0))
                    k.op("dve", lambda: V.tensor_reduce(out=ss[:, :], in_=p[:, 0:c.NE], axis=AX.X, op=ALU.max), reads=[p], writes=[ss])
                    k.op("dve", lambda: V.tensor_scalar(out=ss[:, :], in0=ss[:, :], scalar1=-1.0, scalar2=None, op0=ALU.mult), reads=[ss], writes=[ss])
                    k.op("act", lambda: S.activation(out=lg[:, :], in_=p[:, 0:c.NE], func=AF.Exp, bias=ss[:, 0:1], accum_out=st[:, 0:1]), reads=[p, ss], writes=[lg, st])
                    k.op("dve", lambda: V.reciprocal(out=st[:, :], in_=st[:, :]), reads=[st], writes=[st])
                    k.op("dve", lambda: V.tensor_scalar(out=lg[:, :], in0=lg[:, :], scalar1=st[:, 0:1], scalar2=None, op0=ALU.mult), reads=[lg, st], writes=[lg])
                    p = nps()
                    k.op("pe", lambda: A.transpose(out=p[0:c.NE, 0:128], in_=lg[:, :], identity=ident_f[:, :]), reads=[lg, ident_f], writes=[p])
                    k.op("act", lambda: S.copy(out=lgT[:, :], in_=p[0:c.NE, 0:128]), reads=[p], writes=[lgT])
                    k.dma("sp", lambda e: e.dma_start(out=AFFT[:, r0:r0 + 128], in_=lgT[:, :]), reads=[lgT], writes=[AFFT])

    def regions(need_ctx):
        r = []
        if need_ctx:
            r.append((0, 0, Tc, c.capc))
        r.append((1, Tc, T, c.cap))
        return r

    def phase_moe(l, need_ctx):
        FC = c.FF // 128
        with k.scope():
            z = k.sbuf("mz", [128, D], F32)
            k.op("dve", lambda: V.memset(z[:, :], 0.0), writes=[z])
            for t0 in range(0 if need_ctx else Tc, TT, 128):
                k.dma("sp", lambda e: e.dma_start(out=MO[t0:t0 + 128, :], in_=z[:, :]), reads=[z], writes=[MO])
        for (ri, t0, n, cap) in regions(need_ctx):
            ns = min(128, cap)
            NG_ = cap // ns
            with k.scope():
                idxT = k.sbuf("eidx", [128, c.NE * NG_], I32); gT = k.sbuf("egT", [128, c.NE * NG_], F32); idxS = k.sbuf("eidxS", [128, c.NE * NG_], I32)
                nsp = max(1, D // getattr(c, "msplit", 2048))
                Dh = D // nsp
                MO2 = MO[:, :].rearrange("t (s d) -> (t s) d", s=nsp)
                with k.scope():
                    wk = k.sbuf("twk", [c.NE, n], F32); vals = k.sbuf("tvals", [c.NE, cap], F32); idx = k.sbuf("tidx", [c.NE, cap], U32); idxf = k.sbuf("tidxf", [c.NE, cap], F32)
                    k.dma("sp", lambda e: e.dma_start(out=wk[:, :], in_=AFFT[:, t0:t0 + n]), reads=[AFFT], writes=[wk])
                    for r in range((cap + 7) // 8):
                        k.op("dve", lambda: V.max(out=vals[:, r * 8:(r + 1) * 8], in_=wk[:, :]), reads=[wk], writes=[vals])
                        k.op("dve", lambda: V.max_index(out=idx[:, r * 8:(r + 1) * 8], in_max=vals[:, r * 8:(r + 1) * 8], in_values=wk[:, :]), reads=[wk, vals], writes=[idx])
                        k.op("dve", lambda: V.match_replace(out=wk[:, :], in_to_replace=vals[:, r * 8:(r + 1) * 8], in_values=wk[:, :], imm_value=-1.0), reads=[wk, vals], writes=[wk])
                    k.op("dve", lambda: V.tensor_copy(out=idxf[:, :], in_=idx[:, :]), reads=[idx], writes=[idxf])
                    idxf2 = k.sbuf("tidxf2", [c.NE, cap], F32)
                    k.op("dve", lambda: V.tensor_scalar(out=idxf2[:, :], in0=idxf[:, :], scalar1=float(nsp), scalar2=None, op0=ALU.mult), reads=[idxf], writes=[idxf2])
                    for j in range(NG_):
                        for (src, dst) in ((idxf, idxT), (vals, gT), (idxf2, idxS)):
                            p = nps()
                            k.op("pe", lambda: A.transpose(out=p[0:ns, 0:c.NE], in_=src[:, j * ns:(j + 1) * ns], identity=ident_f[0:c.NE, 0:c.NE]), reads=[src, ident_f], writes=[p])
                            k.op("dve", lambda: V.tensor_copy(out=dst[0:ns, :].rearrange("p (e j) -> p e j", e=c.NE)[:, :, j], in_=p[0:ns, 0:c.NE]), reads=[p], writes=[dst])
                hid = k.sbuf("ehid", [128, FC, cap], BF16)
                for e_ in range(c.NE):
                    with k.scope():
                        xgT = k.sbuf("exgT", [128, KC, cap], BF16)
                        xgs = [k.sbuf("exg%d" % i, [128, D], BF16) for i in range(2)]
                        for j in range(NG_):
                            xg = xgs[j % 2]
                            col = e_ * NG_ + j
                            k.dma("pool", lambda e: e.indirect_dma_start(out=xg[0:ns, :], out_offset=None, in_=H2[:, :], in_offset=bass.IndirectOffsetOnAxis(ap=idxT[0:ns, col:col + 1], axis=0),
                                                                         element_offset=t0 * D), reads=[H2, idxT], writes=[xg])
                            for k0 in range(0, KC, 8):
                                n8 = min(8, KC - k0)
                                p = nps(); pb = p[:, :].bitcast(BF16)
                                for jj in range(n8):
                                    k.op("pe", lambda: A.transpose(out=pb[:, jj * 128:jj * 128 + ns], in_=xg[0:ns, (k0 + jj) * 128:(k0 + jj + 1) * 128], identity=ident_b[0:ns, 0:ns]),
                                         reads=[xg, ident_b], writes=[p])
                                k.op("act" if (k0 // 8) % 2 else "dve",
                                     (lambda: S.copy(out=xgT[:, k0:k0 + n8, j * ns:(j + 1) * ns], in_=pb[:, 0:n8 * 128].rearrange("p (a b) -> p a b", a=n8)[:, :, 0:ns])) if (k0 // 8) % 2 else
                                     (lambda: V.tensor_copy(out=xgT[:, k0:k0 + n8, j * ns:(j + 1) * ns], in_=pb[:, 0:n8 * 128].rearrange("p (a b) -> p a b", a=n8)[:, :, 0:ns])),
                                     reads=[p], writes=[xgT])
                        wgs = [k.sbuf("ewg%d" % i, [128, KC, 256], BF16) for i in range(2)]
                        wus = [k.sbuf("ewu%d" % i, [128, KC, 256], BF16) for i in range(2)]
                        tg = k.sbuf("etg", [128, 512], F32)
                        WGh, WUh, WDh = WGB[e_ // NH], WUB[e_ // NH], WDB[e_ // NH]
                        wrow = (l * NH + e_ % NH) * D
                        nw_ = 0
                        for f0 in range(0, c.FF, 256):
                            fw = min(256, c.FF - f0)
                            wg, wu = wgs[nw_ % 2], wus[nw_ % 2]; nw_ += 1
                            for kq in range(0, KC, 8):
                                k8 = min(8, KC - kq)
                                k.dma("sp", lambda e: e.dma_start(out=wg[:, kq:kq + k8, 0:fw], in_=WGh[wrow + kq * 128:wrow + (kq + k8) * 128, f0:f0 + fw].rearrange("(kc p) n -> p kc n", p=128)), reads=[WGh], writes=[wg])
                                k.dma("sp", lambda e: e.dma_start(out=wu[:, kq:kq + k8, 0:fw], in_=WUh[wrow + kq * 128:wrow + (kq + k8) * 128, f0:f0 + fw].rearrange("(kc p) n -> p kc n", p=128)), reads=[WUh], writes=[wu])
                            for fi in range(fw // 128):
                                fc = (f0 // 128) + fi
                                for s0 in range(0, cap, 512):
                                    sw = min(512, cap - s0)
                                    pg, pu = nps(), nps()
                                    for kc in range(KC):
                                        k.op("pe", lambda: A.matmul(pg[:, 0:sw], lhsT=wg[:, kc, fi * 128:(fi + 1) * 128], rhs=xgT[:, kc, s0:s0 + sw], start=(kc == 0), stop=(kc == KC - 1)),
                                             reads=[wg, xgT], writes=[pg], pe_acc=(kc > 0))
                                    for kc in range(KC):
                                        k.op("pe", lambda: A.matmul(pu[:, 0:sw], lhsT=wu[:, kc, fi * 128:(fi + 1) * 128], rhs=xgT[:, kc, s0:s0 + sw], start=(kc == 0), stop=(kc == KC - 1)),
                                             reads=[wu, xgT], writes=[pu], pe_acc=(kc > 0))
                                    k.op("act", lambda: S.activation(out=tg[:, 0:sw], in_=pg[:, 0:sw], func=AF.Silu), reads=[pg], writes=[tg])
                                    k.op("dve", lambda: V.tensor_tensor(out=hid[:, fc, s0:s0 + sw], in0=tg[:, 0:sw], in1=pu[:, 0:sw], op=ALU.mult), reads=[tg, pu], writes=[hid])
                    with k.scope():
                        wd = k.sbuf("ewd", [128, FC, D], BF16)
                        yos = [[k.sbuf("eyo%d_%d" % (i, h_), [128, Dh], F32) for h_ in range(nsp)] for i in range(2)]
                        WDh = WDB[e_ // NH]
                        drow = (l * NH + e_ % NH) * c.FF
                        k.dma("sp", lambda e: e.dma_start(out=wd[:, :, :], in_=WDh[drow:drow + c.FF, :].rearrange("(fc p) n -> p fc n", p=128)), reads=[WDh], writes=[wd])
                        for j in range(NG_):
                            yo = yos[j % 2]
                            col = e_ * NG_ + j
                            NST = min(512, Dh)
                            for n0 in range(0, D, NST):
                                nw2 = min(NST, D - n0)
                                p = nps()
                                for fc in range(FC):
                                    k.op("pe", lambda: A.matmul(p[0:ns, 0:nw2], lhsT=hid[:, fc, j * ns:(j + 1) * ns], rhs=wd[:, fc, n0:n0 + nw2], start=(fc == 0), stop=(fc == FC - 1)),
                                         reads=[hid, wd], writes=[p], pe_acc=(fc > 0))
                                yh = yo[n0 // Dh]
                                m0 = n0 % Dh
                                if (n0 // NST) % 2:
                                    k.op("act", lambda: S.activation(out=yh[0:ns, m0:m0 + nw2], in_=p[0:ns, 0:nw2], func=AF.Copy, scale=gT[0:ns, col:col + 1]), reads=[p, gT], writes=[yh])
                                else:
                                    k.op("dve", lambda: V.tensor_scalar(out=yh[0:ns, m0:m0 + nw2], in0=p[0:ns, 0:nw2], scalar1=gT[0:ns, col:col + 1], scalar2=None, op0=ALU.mult), reads=[p, gT], writes=[yh])
                            for h_ in range(nsp):
                                yh = yo[h_]
                                k.dma("pool", lambda e: e.indirect_dma_start(out=MO2, out_offset=bass.IndirectOffsetOnAxis(ap=idxS[0:ns, col:col + 1], axis=0), in_=yh[0:ns, :], in_offset=None,
                                                                             element_offset=t0 * D + h_ * Dh, compute_op=ALU.add), reads=[yh, idxS], writes=[MO])

    def phase_final(l, need_ctx, last):
        with k.scope():
            ga = k.sbuf("fga", [128, D], F32); gfb = k.sbuf("fgf", [128, D], F32)
            xts = [k.sbuf("fxt%d" % i, [128, D], F32) for i in range(2)]; mts = [k.sbuf("fmt%d" % i, [128, D], F32) for i in range(2)]
            junk = k.sbuf("fjunk", [128, D], BF16); ss = k.sbuf("fss", [128, 1], F32); st = k.sbuf("fst", [128, 1], F32)
            if last:
                k.dma("sp", lambda e: e.dma_start(out=gfb[:, :], in_=I["gf"][0:1, :].partition_broadcast(128)), reads=[I["gf"]], writes=[gfb])
            cur_v = None
            n = 0
            for t0 in range(0 if need_ctx else Tc, TT, 128):
                v = 1 if t0 < Tc else 0
                if v != cur_v:
                    bc_load(ga, (l * 2 + v) * 6 + 5); cur_v = v
                xt, mt = xts[n % 2], mts[n % 2]; n += 1
                k.dma("sp", lambda e: e.dma_start(out=xt[:, :], in_=X[t0:t0 + 128, :]), reads=[X], writes=[xt])
                k.dma("sp", lambda e: e.dma_start(out=mt[:, :], in_=MO[t0:t0 + 128, :]), reads=[MO], writes=[mt])
                k.op("dve", lambda: V.tensor_tensor(out=mt[:, :], in0=mt[:, :], in1=ga[:, :], op=ALU.mult), reads=[mt, ga], writes=[mt])
                k.op("pool", lambda: nc.gpsimd.tensor_tensor(out=xt[:, :], in0=xt[:, :], in1=mt[:, :], op=ALU.add), reads=[xt, mt], writes=[xt])
                if not last:
                    k.dma("sp", lambda e: e.dma_start(out=X[t0:t0 + 128, :], in_=xt[:, :]), reads=[xt], writes=[X])
                else:
                    k.op("act", lambda: S.activation(out=junk[:, :], in_=xt[:, :], func=AF.Square, accum_out=ss[:, 0:1]), reads=[xt], writes=[junk, ss])
                    rstd_of(ss, D, st)
                    k.op("dve", lambda: V.scalar_tensor_tensor(out=xt[:, :], in0=xt[:, :], scalar=ss[:, 0:1], in1=gfb[:, :], op0=ALU.mult, op1=ALU.mult), reads=[xt, ss, gfb], writes=[xt])
                    k.dma("sp", lambda e: e.dma_start(out=OUT[t0 - Tc:t0 - Tc + 128, :], in_=xt[:, :]), reads=[xt], writes=[OUT])

    def layer(l):
        need_ctx = l < L - 1
        phase_in(l)
        phase_lru(l)
        phase_hg(l)
        phase_mla(l, need_ctx)
        phase_win(l, need_ctx)
        phase_out(l, need_ctx)
        phase_moe(l, need_ctx)
        phase_final(l, need_ctx, l == L - 1)

    B = dict(OUT=OUT, WINB=WINB, WOUTB=WOUTB, MOD=MOD, X=X, PT=PT, YT=YT, ident_f=ident_f, ident_b=ident_b, nps=nps, PS=PS,
             phase_in=phase_in)
    if c.stop_after in ("alloc", "stage", "in", "lru", "hg", "mlap", "mla", "win", "out", "moe"):
        seq = ["alloc", "stage", "in", "lru", "hg", "mlap", "mla", "win", "out", "moe"]
        n_ = seq.index(c.stop_after)
        if n_ >= 2: phase_in(0)
        if n_ >= 3: phase_lru(0)
        if n_ >= 4: phase_hg(0)
        if n_ >= 5: phase_mla_proj(0)
        if n_ >= 6: phase_mla_attn(0, True)
        if n_ >= 7: phase_win(0, True)
        if n_ >= 8: phase_out(0, True)
        if n_ >= 9: phase_moe(0, True)
        k.barrier()
        for r0 in range(0, T, 512):
            k.dma("sp", lambda e: e.dma_start(out=OUT[r0:r0 + 512, :], in_=X[Tc + r0:Tc + r0 + 512, :]), reads=[X], writes=[OUT])
        return nc, k, I, B
    if c.stop_after == "in0":
        phase_in(0)
        _dbg(k, c, "PT", lambda: PT.ap(), [DIN, TT])
        return nc, k, I, B
    if c.stop_after == "":
        for rep in range(getattr(c, "repeat", 1)):
            for l in range(L):
                if rep > 0 and l == L - 1:
                    continue
                layer(l)
        return nc, k, I, B
    if c.stop_after.startswith("mix"):
        PTin = inp("PT_in", [DIN, TT])
        k.dma("sp", lambda e: e.dma_start(out=PT.ap(), in_=PTin.ap()), reads=[PTin], writes=[PT])
        k.barrier()
        for ph in c.stop_after.split("_")[1:]:
            dict(lru=phase_lru, hg=phase_hg, mla=phase_mla, win=phase_win)[ph](0)
        _dbg(k, c, "YT", lambda: YT.ap(), [c.DMIX, TT], BF16)
        return nc, k, I, B
    return nc, k, I, B


def prep(cfg, inp):
    c = cfg
    f32 = np.float32
    T, D, KC = c.T, c.D, c.KC
    vecs = np.stack([inp["c"][0], inp["c"][1], inp["c_ctx"]]).astype(f32)
    cv = np.ascontiguousarray(vecs.reshape(3, KC, 128).transpose(2, 0, 1))
    pos = np.arange(T)
    row, col = pos // c.GW, pos % c.GW
    inv = (10000.0 ** (-np.arange(16, dtype=f32) / 16)).astype(f32)
    d = np.arange(64)
    p = np.where(d[:, None] < 32, row[None, :], col[None, :]).astype(f32)
    ang = p * inv[d % 16][:, None]
    cs = np.stack([np.cos(ang), np.sin(ang)]).astype(f32)
    rope_cs = np.ascontiguousarray(np.concatenate([cs, cs], axis=1))
    R32 = np.zeros((32, 32), f32)
    for i in range(16):
        R32[i, i + 16] = -1.0
        R32[i + 16, i] = 1.0
    R128 = np.kron(np.eye(4, dtype=f32), R32)
    qi = np.arange(128)[:, None]
    offs = np.arange(384)[None, :] - 128
    wmask = np.where(np.abs(offs - qi) <= 128, 0.0, -1e30).astype(f32)
    s_, t_ = np.arange(64)[:, None], np.arange(64)[None, :]
    tri = np.stack([np.tile((s_ <= t_).astype(f32), (1, 8)), np.tile((s_ >= t_).astype(f32), (1, 8))])
    maps = []
    for core in range(2):
        l = core
        pidx = np.arange(128)
        coreidx = np.zeros((128, 16), np.int32)
        for j in range(6):
            coreidx[:, 8 + j] = (pidx + 3 * core) * 6 + j
        coreidx[:, 0] = pidx + core * D
        coreidx[:, 1] = pidx + core * c.DMIX
        coreidx[:, 2] = pidx + core * (c.NE // 2) * D
        coreidx[:, 3] = pidx + core * (c.NE // 2) * c.FF
        coreidx[:, 4] = pidx + 3 * core
        m = dict(
            x=inp["x"][core], ctx=inp["ctx"][core], cv=cv,
            w_ada=inp["w_ada"][l], b_ada=inp["b_ada"], g1=inp["norm1_g"], g2=inp["norm2_g"], gf=inp["final_norm_g"].reshape(1, D),
            w_in=inp["w_in"][l], w_out=inp["w_out"][l],
            w_gate=inp["w_gate"][l].reshape(c.NE * D, c.FF), w_up=inp["w_up"][l].reshape(c.NE * D, c.FF),
            w_down=inp["w_down"][l].reshape(c.NE * c.FF, D), w_router=inp["w_router"],
            w_uq=inp["mla_w_uq"], w_ukv=inp["mla_w_ukv"], conv_w=inp["lru_conv_w"], conv_b=inp["lru_conv_b"],
            lru_wr=inp["lru_wr"], lru_wi=inp["lru_wi"], lru_br=inp["lru_br"], lru_bi=inp["lru_bi"], lru_lam=inp["lru_lambda"],
            hg_lb=inp["hg_lb_logits"], hg_g=inp["hg_norm_g"], q_g=inp["mla_q_norm_g"], kv_g=inp["mla_kv_norm_g"], sink=inp["win_sink"],
            coreidx=coreidx, ident_f=np.eye(128, dtype=f32), rope_cs=rope_cs, rope_rt=np.ascontiguousarray(R128.T), wmask=wmask, tri=tri,
        )
        maps.append({k_: np.ascontiguousarray(v_) for k_, v_ in m.items()})
    return maps


def kernel(**inputs):
    from concourse.bass_utils import run_bass_kernel_spmd
    cfg = Cfg(**FULL, debug=False, stop_after="", hgcut=0)
    inp = {k_: np.asarray(v_) for k_, v_ in inputs.items()}
    nc, k, I, B = build(cfg)
    k.close()
    maps = prep(cfg, inp)
    res = run_bass_kernel_spmd(nc, maps, core_ids=[0, 1])
    return np.stack([np.asarray(res.results[c_]["out"]) for c_ in range(2)]).astype(np.float32)
```

```python
import contextlib
import numpy as np
import concourse.bass as bass
import concourse.mybir as mybir

F32 = mybir.dt.float32
BF16 = mybir.dt.bfloat16
I32 = mybir.dt.int32
U32 = mybir.dt.uint32
AF = mybir.ActivationFunctionType
ALU = mybir.AluOpType
AX = mybir.AxisListType

NSLOT = 6
QUEUES = ("sp", "pool")
ENGS = ("pe", "dve", "act", "pool", "sp")


class Buf:
    def __init__(self, t, name=""):
        self.t = t
        self.name = name
        self.w = None
        self.r = []

    def __getitem__(self, idx):
        return self.t[idx]

    def ap(self):
        return self.t[:]


class SplitBuf(Buf):
    def __init__(self, parts, name=""):
        Buf.__init__(self, None, name)
        self.parts = parts

    def __getitem__(self, idx):
        rs, cs_ = idx
        for (r0, r1, ap) in self.parts:
            if r0 <= rs.start < r1:
                assert rs.stop <= r1
                return ap[rs.start - r0:rs.stop - r0, cs_]
        raise IndexError(idx)

    def ap(self):
        assert len(self.parts) == 1
        return self.parts[0][2]


class K:
    def __init__(self, nc):
        self.nc = nc
        self.es = contextlib.ExitStack()
        self.eng = {"pe": nc.tensor, "dve": nc.vector, "act": nc.scalar, "pool": nc.gpsimd, "sp": nc.sync}
        self.sem = {}
        self.cnt = {}
        for e in ENGS:
            self.sem[e] = self.es.enter_context(nc.semaphore("s_" + e))
            self.cnt[e] = 0
        for q in QUEUES:
            for s in range(NSLOT):
                n = "d_%s%d" % (q, s)
                self.sem[n] = self.es.enter_context(nc.semaphore(n))
                self.cnt[n] = 0
        self.slot = {q: 0 for q in QUEUES}
        self.slot_tok = {}
        self.epoch = 0
        self.seen = {e: {} for e in ENGS}
        self.dry = False
        self.vars = {}
        self.nvar = 0
        self.n_ins = 0

    def sbuf(self, name, shape, dt):
        self.nalloc = getattr(self, "nalloc", 0) + 1
        name = "sb%d_%s" % (self.nalloc, name)
        return Buf(self.es.enter_context(self.nc.sbuf_tensor(name, list(shape), dt)), name)

    def psum(self, name, shape, dt=F32):
        return Buf(self.es.enter_context(self.nc.psum_tensor(name, list(shape), dt)), name)

    def dram(self, name, shape, dt, kind="Internal", shared=False):
        if kind == "Internal":
            t = self.nc.dram_tensor(name, list(shape), dt, addr_space="Shared" if shared else "Local")
        else:
            t = self.nc.dram_tensor(name, list(shape), dt, kind=kind)
        return Buf(t.ap(), name)

    def _val(self, lin):
        return lin

    def _wait(self, e, tok):
        if tok is None:
            return
        sname, lin, ep = tok
        if ep != self.epoch:
            return
        prev = self.seen[e].get(sname)
        if prev is not None and prev >= lin:
            return
        self.seen[e][sname] = lin
        if not self.dry:
            self.eng[e].wait_ge(self.sem[sname], self._val(lin))

    def _deps(self, e, reads, writes, pe_acc=False):
        for b in reads:
            self._wait(e, b.w)
        for b in writes:
            if not (pe_acc and b.w is not None and b.w[0] == "pe") and not (b.w is not None and b.w[0] == e):
                self._wait(e, b.w)
            for t in b.r:
                if t[0] != e:
                    self._wait(e, t)

    def _mark(self, tok, reads, writes):
        for b in reads:
            b.r.append(tok)
            if len(b.r) > 24:
                b.r = b.r[-24:] if False else b.r
        for b in writes:
            b.w = tok
            b.r = []

    def op(self, e, fn, reads=(), writes=(), pe_acc=False):
        self._deps(e, reads, writes, pe_acc)
        self.cnt[e] += 1
        if not self.dry:
            fn().then_inc(self.sem[e], 1)
        self.n_ins += 1
        tok = (e, self.cnt[e], self.epoch)
        self._mark(tok, reads, writes)
        return tok

    def dma(self, q, fn, reads=(), writes=()):
        s = self.slot[q]
        self.slot[q] = (s + 1) % NSLOT
        sname = "d_%s%d" % (q, s)
        self._wait(q, self.slot_tok.get(sname))
        self._deps(q, reads, writes)
        self.cnt[sname] += 16
        if not self.dry:
            fn(self.eng[q]).then_inc(self.sem[sname], 16)
        self.n_ins += 1
        tok = (sname, self.cnt[sname], self.epoch)
        self.slot_tok[sname] = tok
        self._mark(tok, reads, writes)
        return tok

    def barrier(self):
        if not self.dry:
            for e in ENGS:
                for sname, lin in self.cnt.items():
                    if sname == e:
                        continue
                    self.eng[e].wait_ge(self.sem[sname], self._val(lin))
        self.epoch += 1
        self.seen = {e: {} for e in ENGS}
        for q in QUEUES:
            self.slot[q] = 0

    def all_core_barrier(self):
        self.barrier()
        if not self.dry:
            self.nc.all_core_barrier()
        self.barrier()

    def close(self):
        self.barrier()
        self.es.close()


    @contextlib.contextmanager
    def scope(self):
        old = self.es
        self.es = contextlib.ExitStack()
        try:
            yield
        finally:
            self.barrier()
            self.es.close()
            self.es = old


class Cfg:
    def __init__(s, **kw):
        s.__dict__.update(kw)
        s.HK = s.HH * 128
        s.splits = [s.LW, s.LW, s.HK, s.HK, s.HK, s.HK, s.HK, s.QR, s.KVR, 64, s.WH * 64, s.WKV * 64, s.WKV * 64]
        s.off = [0]
        for w in s.splits:
            s.off.append(s.off[-1] + w)
        s.DIN = s.off[-1]
        (s.o_lx, s.o_lg, s.o_hq, s.o_hff, s.o_hfb, s.o_hi, s.o_hg, s.o_cq, s.o_ckv, s.o_kr, s.o_wq, s.o_wk, s.o_wv) = s.off[:13]
        s.DMIX = s.LW + s.HK + s.MH * 128 + s.WH * 64
        s.KC = s.D // 128
        s.TT = s.T + s.Tc
        s.cap = max(1, 2 * s.T // s.NE)
        s.capc = max(1, 2 * s.Tc // s.NE)
        s.blocks = [(0, s.Tc, True)] + [(s.Tc + i * 512, 512, False) for i in range(s.T // 512)]
        s.ntiles = []
        c0 = 0
        while c0 < s.DIN:
            w = 128
            if c0 == s.o_kr:
                w = 64
            s.ntiles.append((c0, min(w, s.DIN - c0)))
            c0 += w


FULL = dict(D=4096, T=8192, Tc=256, L=2, GW=64, LW=1024, HH=8, MH=8, QR=1024, KVR=512, WH=16, WKV=2, NE=16, FF=1024)
EPS = 1e-6


def _dbg(k, cfg, name, buf_ap_fn, shape, dt=F32):
    if not cfg.debug:
        return
    o = k.dram("dbg_" + name, shape, dt, kind="ExternalOutput")
    k.barrier()
    k.dma("sp", lambda e: e.dma_start(out=o.ap(), in_=buf_ap_fn()), writes=[o])
    k.barrier()


def _dump(k, cfg, name, buf, ap_fn, shape, dt=F32):
    if not getattr(cfg, "dump", False):
        return
    o = k.dram("dmp_" + name, shape, dt, kind="ExternalOutput")
    k.dma("sp", lambda e: e.dma_start(out=o.ap(), in_=ap_fn()), reads=[buf], writes=[o])


def build(cfg):
    nc = bass.Bass("TRN2", target_bir_lowering=False, num_devices=2)
    k = K(nc)
    k.es.enter_context(nc.allow_non_contiguous_dma(reason="small strided parameter / layout DMAs"))
    c = cfg
    D, T, Tc, TT, L, KC, DIN = c.D, c.T, c.Tc, c.TT, c.L, c.KC, c.DIN
    V, S, A = nc.vector, nc.scalar, nc.tensor

    def inp(name, shape, dt=F32):
        return k.dram(name, shape, dt, kind="ExternalInput")

    I = {}
    I["x"] = inp("x", [T, D]); I["ctx"] = inp("ctx", [Tc, D]); I["cv"] = inp("cv", [128, 3, KC])
    I["w_ada"] = inp("w_ada", [D, 6 * D]); I["b_ada"] = inp("b_ada", [L, 6 * D])
    I["g1"] = inp("g1", [L, D]); I["g2"] = inp("g2", [L, D]); I["gf"] = inp("gf", [1, D])
    I["w_in"] = inp("w_in", [D, DIN]); I["w_out"] = inp("w_out", [c.DMIX, D])
    I["w_gate"] = inp("w_gate", [c.NE * D, c.FF]); I["w_up"] = inp("w_up", [c.NE * D, c.FF]); I["w_down"] = inp("w_down", [c.NE * c.FF, D])
    I["w_router"] = inp("w_router", [L, D, c.NE])
    I["w_uq"] = inp("w_uq", [L, c.QR, c.MH * 192]); I["w_ukv"] = inp("w_ukv", [L, c.KVR, c.MH * 256])
    I["conv_w"] = inp("conv_w", [L, 4, c.LW]); I["conv_b"] = inp("conv_b", [L, c.LW])
    I["lru_wr"] = inp("lru_wr", [L, 2, c.LW // 128, 128, 128]); I["lru_wi"] = inp("lru_wi", [L, 2, c.LW // 128, 128, 128])
    I["lru_br"] = inp("lru_br", [L, 2, c.LW]); I["lru_bi"] = inp("lru_bi", [L, 2, c.LW]); I["lru_lam"] = inp("lru_lam", [L, 2, c.LW])
    I["hg_lb"] = inp("hg_lb", [2, L, c.HK]); I["hg_g"] = inp("hg_g", [L, c.HK])
    I["q_g"] = inp("q_g", [L, c.QR]); I["kv_g"] = inp("kv_g", [L, c.KVR]); I["sink"] = inp("sink", [L, c.WH])
    I["coreidx"] = inp("coreidx", [128, 16], I32)
    I["ident_f"] = inp("ident_f", [128, 128]); I["rope_cs"] = inp("rope_cs", [2, 128, T]); I["rope_rt"] = inp("rope_rt", [128, 128])
    I["wmask"] = inp("wmask", [128, 384]); I["tri"] = inp("tri", [2, 64, 512])
    OUT = k.dram("out", [T, D], F32, kind="ExternalOutput")

    WINB = k.dram("WINB", [L * D, DIN], BF16, shared=True)
    WOUTB = k.dram("WOUTB", [L * c.DMIX, D], BF16, shared=True)
    NH = c.NE // 2
    WGB = [k.dram("WGB%d" % h_, [L * NH * D, c.FF], BF16, shared=True) for h_ in range(2)]
    WUB = [k.dram("WUB%d" % h_, [L * NH * D, c.FF], BF16, shared=True) for h_ in range(2)]
    WDB = [k.dram("WDB%d" % h_, [L * NH * c.FF, D], BF16, shared=True) for h_ in range(2)]
    ADA = k.dram("ADA", [L * 3, 6 * D], F32, shared=True)
    MOD = k.dram("MOD", [L * 2 * 6, D], F32)
    X = k.dram("X", [TT, D], F32)
    if DIN * TT * 4 > 200 * 2 ** 20:
        PT = SplitBuf([(0, c.o_hi, k.dram("PTa", [c.o_hi, TT], F32).t), (c.o_hi, DIN, k.dram("PTb", [DIN - c.o_hi, TT], F32).t)], "PT")
    else:
        PT = SplitBuf([(0, DIN, k.dram("PT", [DIN, TT], F32).t)], "PT")
    YT = k.dram("YT", [c.DMIX, TT], BF16)

    ident_f = k.sbuf("ident_f", [128, 128], F32)
    ident_b = k.sbuf("ident_b", [128, 128], BF16)
    cidx = k.sbuf("cidx", [128, 16], I32)
    k.dma("sp", lambda e: e.dma_start(out=ident_f[:, :], in_=I["ident_f"][:, :]), reads=[I["ident_f"]], writes=[ident_f])
    k.op("dve", lambda: V.tensor_copy(out=ident_b[:, :], in_=ident_f[:, :]), reads=[ident_f], writes=[ident_b])
    k.dma("sp", lambda e: e.dma_start(out=cidx[:, :], in_=I["coreidx"][:, :]), reads=[I["coreidx"]], writes=[cidx])
    PS = [k.psum("ps%d" % i, [128, 512]) for i in range(8)]
    psi = [0]

    def nps():
        psi[0] = (psi[0] + 1) % 6
        return PS[2 + psi[0]]

    poi = [0]

    def npo():
        poi[0] = (poi[0] + 1) % 2
        return PS[poi[0]]

    k.dma("sp", lambda e: e.dma_start(out=X[0:Tc, :], in_=I["ctx"][:, :]), reads=[I["ctx"]], writes=[X])
    for r0 in range(0, T, 512):
        k.dma("sp", lambda e: e.dma_start(out=X[Tc + r0:Tc + r0 + 512, :], in_=I["x"][r0:r0 + 512, :]), reads=[I["x"]], writes=[X])

    def stage(src, dst, rows, cols, widx, src0=0):
        with k.scope():
            CW = cols
            fs = [k.sbuf("stg_f%d" % i, [128, CW], F32) for i in range(2)]
            bs = [k.sbuf("stg_b%d" % i, [128, CW], BF16) for i in range(2)]
            n = 0
            for r0 in range(0, rows, 128):
                for c0 in range(0, cols, CW):
                    cw = min(CW, cols - c0)
                    f, b = fs[n % 2], bs[n % 2]
                    k.dma("sp", lambda e: e.dma_start(out=f[:, :cw], in_=src[src0 + r0:src0 + r0 + 128, c0:c0 + cw]), reads=[src], writes=[f])
                    if n % 2 == 0:
                        k.op("dve", lambda: V.tensor_copy(out=b[:, :cw], in_=f[:, :cw]), reads=[f], writes=[b])
                    else:
                        k.op("act", lambda: S.copy(out=b[:, :cw], in_=f[:, :cw]), reads=[f], writes=[b])
                    k.dma("pool", lambda e: e.indirect_dma_start(
                        out=dst[:, 0:cw], out_offset=bass.IndirectOffsetOnAxis(ap=cidx[:, widx:widx + 1], axis=0),
                        in_=b[:, :cw], in_offset=None, element_offset=r0 * cols + c0), reads=[b, cidx], writes=[])
                    n += 1

    if c.stop_after == "alloc":
        k.barrier()
        for r0 in range(0, T, 512):
            k.dma("sp", lambda e: e.dma_start(out=OUT[r0:r0 + 512, :], in_=X[Tc + r0:Tc + r0 + 512, :]), reads=[X], writes=[OUT])
        return nc, k, I, {}
    stage(I["w_in"], WINB, D, DIN, 0)
    stage(I["w_out"], WOUTB, c.DMIX, D, 1)
    for h_ in range(2):
        stage(I["w_gate"], WGB[h_], NH * D, c.FF, 2, src0=h_ * NH * D)
        stage(I["w_up"], WUB[h_], NH * D, c.FF, 2, src0=h_ * NH * D)
        stage(I["w_down"], WDB[h_], NH * c.FF, D, 3, src0=h_ * NH * c.FF)

    with k.scope():
        cv = k.sbuf("cv", [128, 3, KC], F32)
        cs = k.sbuf("cs", [128, 3, KC], F32)
        k.dma("sp", lambda e: e.dma_start(out=cv[:, :, :], in_=I["cv"][:, :, :]), reads=[I["cv"]], writes=[cv])
        k.op("act", lambda: S.activation(out=cs[:, :, :], in_=cv[:, :, :], func=AF.Silu), reads=[cv], writes=[cs])
        NG = 2048 if (6 * D) % 2048 == 0 else 1536
        wts = [k.sbuf("adaw%d" % i, [128, NG], F32) for i in range(3)]
        ao = k.sbuf("adao", [3, 6 * D], F32)
        n = 0
        for g in range(6 * D // NG):
            pss = [nps() for _ in range(NG // 512)]
            for kc in range(KC):
                wt = wts[n % 3]; n += 1
                k.dma("sp", lambda e: e.dma_start(out=wt[:, :], in_=I["w_ada"][kc * 128:(kc + 1) * 128, g * NG:(g + 1) * NG]), reads=[I["w_ada"]], writes=[wt])
                for j in range(NG // 512):
                    k.op("pe", lambda: A.matmul(pss[j][0:3, :], lhsT=cs[:, :, kc], rhs=wt[:, j * 512:(j + 1) * 512], start=(kc == 0), stop=(kc == KC - 1)),
                         reads=[cs, wt], writes=[pss[j]], pe_acc=(kc > 0))
            for j in range(NG // 512):
                k.op("act", lambda: S.copy(out=ao[:, g * NG + j * 512:g * NG + (j + 1) * 512], in_=pss[j][0:3, :]), reads=[pss[j]], writes=[ao])
        ADA2 = ADA[:, :].rearrange("r (j d) -> (r j) d", j=6)
        aoj = [k.sbuf("adaoj%d" % i, [3, D], F32) for i in range(2)]
        for j in range(6):
            t_ = aoj[j % 2]
            k.op("act", lambda: S.copy(out=t_[:, :], in_=ao[:, j * D:(j + 1) * D]), reads=[ao], writes=[t_])
            k.dma("pool", lambda e: e.indirect_dma_start(out=ADA2, out_offset=bass.IndirectOffsetOnAxis(ap=cidx[0:3, 8 + j:9 + j], axis=0),
                                                         in_=t_[:, :], in_offset=None), reads=[t_, cidx], writes=[])
    k.all_core_barrier()

    with k.scope():
        pid = nc.partition_id()
        ADA4 = ADA[:, :].rearrange("(l v) (j d) -> v l j d", v=3, j=6)
        a_t = k.sbuf("ma", [6, D], F32); b_t = k.sbuf("mb", [6, D], F32); g_t = k.sbuf("mg", [6, D], F32); m_t = k.sbuf("mm", [6, D], F32)
        for l in range(L):
            for v in range(2):
                if v == 0:
                    k.dma("sp", lambda e: e.dma_start(out=a_t[:, :], in_=ADA4[pid, l, :, :]), reads=[ADA], writes=[a_t])
                else:
                    k.dma("sp", lambda e: e.dma_start(out=a_t[:, :], in_=ADA4[2, l, :, :]), reads=[ADA], writes=[a_t])
                k.dma("sp", lambda e: e.dma_start(out=b_t[:, :], in_=I["b_ada"][l, :].rearrange("(j d) -> j d", j=6)), reads=[I["b_ada"]], writes=[b_t])
                k.op("dve", lambda: V.memset(g_t[:, :], 1.0), writes=[g_t])
                k.dma("sp", lambda e: e.dma_start(out=g_t[1:2, :], in_=I["g1"][l:l + 1, :]), reads=[I["g1"]], writes=[g_t])
                k.dma("sp", lambda e: e.dma_start(out=g_t[4:5, :], in_=I["g2"][l:l + 1, :]), reads=[I["g2"]], writes=[g_t])
                k.op("dve", lambda: V.tensor_tensor(out=a_t[:, :], in0=a_t[:, :], in1=b_t[:, :], op=ALU.add), reads=[a_t, b_t], writes=[a_t])
                k.op("dve", lambda: V.scalar_tensor_tensor(out=m_t[:, :], in0=a_t[:, :], scalar=1.0, in1=g_t[:, :], op0=ALU.add, op1=ALU.mult),
                     reads=[a_t, g_t], writes=[m_t])
                r0 = (l * 2 + v) * 6
                k.dma("sp", lambda e: e.dma_start(out=MOD[r0:r0 + 6, :], in_=a_t[:, :]), reads=[a_t], writes=[MOD])
                k.dma("sp", lambda e: e.dma_start(out=MOD[r0 + 1:r0 + 2, :], in_=m_t[1:2, :]), reads=[m_t], writes=[MOD])
                k.dma("sp", lambda e: e.dma_start(out=MOD[r0 + 4:r0 + 5, :], in_=m_t[4:5, :]), reads=[m_t], writes=[MOD])
    _dbg(k, c, "MOD", lambda: MOD.ap(), [L * 12, D])
    _dbg(k, c, "ADA", lambda: ADA.ap(), [L * 3, 6 * D])

    def bc_load(dst, row):
        k.dma("sp", lambda e: e.dma_start(out=dst[:, :], in_=MOD[row:row + 1, :].partition_broadcast(128)), reads=[MOD], writes=[dst])

    def rstd_of(ss, n, tmp):
        k.op("dve", lambda: V.tensor_scalar(out=ss[:, :], in0=ss[:, :], scalar1=1.0 / n, scalar2=EPS, op0=ALU.mult, op1=ALU.add), reads=[ss], writes=[ss])
        k.op("act", lambda: S.activation(out=tmp[:, :], in_=ss[:, :], func=AF.Sqrt), reads=[ss], writes=[tmp])
        k.op("dve", lambda: V.reciprocal(out=ss[:, :], in_=tmp[:, :]), reads=[tmp], writes=[ss])

    def norm_mod_T(xt, hb, junk, Abc, Sbc, ss, tmp, hT, ti, ntok):
        k.op("act", lambda: S.activation(out=junk[:, :], in_=xt[:, :], func=AF.Square, accum_out=ss[:, 0:1]), reads=[xt], writes=[junk, ss])
        rstd_of(ss, D, tmp)
        k.op("dve", lambda: V.scalar_tensor_tensor(out=xt[:, :], in0=xt[:, :], scalar=ss[:, 0:1], in1=Abc[:, :], op0=ALU.mult, op1=ALU.mult),
             reads=[xt, ss, Abc], writes=[xt])
        k.op("dve", lambda: V.tensor_tensor(out=hb[:, :], in0=xt[:, :], in1=Sbc[:, :], op=ALU.add), reads=[xt, Sbc], writes=[hb])
        transpose_into(hb, hT, ti, KC)

    def transpose_into(hb, hT, ti, nkc):
        for k0 in range(0, nkc, 8):
            n8 = min(8, nkc - k0)
            p = nps()
            pb = p[:, :].bitcast(BF16)
            for j in range(n8):
                k.op("pe", lambda: A.transpose(out=pb[:, j * 128:(j + 1) * 128], in_=hb[:, (k0 + j) * 128:(k0 + j + 1) * 128], identity=ident_b[:, :]),
                     reads=[hb, ident_b], writes=[p])
            eng = "act" if (k0 // 8) % 2 else "dve"
            src = lambda: pb[:, 0:n8 * 128].rearrange("p (a b) -> p a b", a=n8)
            dst = lambda: hT[:, k0:k0 + n8, ti * 128:(ti + 1) * 128]
            if eng == "act":
                k.op("act", lambda: S.copy(out=dst(), in_=src()), reads=[p], writes=[hT])
            else:
                k.op("dve", lambda: V.tensor_copy(out=dst(), in_=src()), reads=[p], writes=[hT])

    def phase_in(l):
        with k.scope():
            Abc = k.sbuf("Abc", [128, D], F32); Sbc = k.sbuf("Sbc", [128, D], F32)
            xts = [k.sbuf("xt%d" % i, [128, D], F32) for i in range(2)]
            hb = k.sbuf("hb", [128, D], BF16); junk = k.sbuf("junk", [128, D], BF16)
            ss = k.sbuf("ss", [128, 1], F32); tmp = k.sbuf("tmp", [128, 1], F32)
            hT = k.sbuf("hT", [128, KC, 512], BF16)
            GW_ = 384
            wts = [k.sbuf("wt%d" % i, [128, KC, GW_], BF16) for i in range(2)]
            ots = [k.sbuf("ot%d" % i, [128, 512], F32) for i in range(4)]
            groups = []
            for (c0, w) in c.ntiles:
                if groups and groups[-1][1] + w <= GW_:
                    groups[-1][1] += w; groups[-1][2].append((c0, w))
                else:
                    groups.append([c0, w, [(c0, w)]])
            cur_v = None
            nw = 0; no = 0; nx = 0
            for (tok0, ntok, is_ctx) in c.blocks:
                v = 1 if is_ctx else 0
                if v != cur_v:
                    bc_load(Abc, (l * 2 + v) * 6 + 1); bc_load(Sbc, (l * 2 + v) * 6 + 0); cur_v = v
                for ti in range(ntok // 128):
                    xt = xts[nx % 2]; nx += 1
                    k.dma("sp", lambda e: e.dma_start(out=xt[:, :], in_=X[tok0 + ti * 128:tok0 + (ti + 1) * 128, :]), reads=[X], writes=[xt])
                    norm_mod_T(xt, hb, junk, Abc, Sbc, ss, tmp, hT, ti, ntok)
                for (g0, gw, tiles) in groups:
                    wt = wts[nw % 2]; nw += 1
                    for kq in range(0, KC, 8):
                        k8 = min(8, KC - kq)
                        k.dma("sp", lambda e: e.dma_start(out=wt[:, kq:kq + k8, 0:gw], in_=WINB[l * D + kq * 128:l * D + (kq + k8) * 128, g0:g0 + gw].rearrange("(kc p) n -> p kc n", p=128)),
                              reads=[WINB], writes=[wt])
                    for (c0, w) in tiles:
                        lo = c0 - g0
                        p = nps()
                        for kc in range(KC):
                            k.op("pe", lambda: A.matmul(p[0:w, 0:ntok], lhsT=wt[:, kc, lo:lo + w], rhs=hT[:, kc, 0:ntok], start=(kc == 0), stop=(kc == KC - 1)),
                                 reads=[wt, hT], writes=[p], pe_acc=(kc > 0))
                        ot = ots[no % 4]; no += 1
                        if no % 2:
                            k.op("act", lambda: S.copy(out=ot[0:w, 0:ntok], in_=p[0:w, 0:ntok]), reads=[p], writes=[ot])
                        else:
                            k.op("dve", lambda: V.tensor_copy(out=ot[0:w, 0:ntok], in_=p[0:w, 0:ntok]), reads=[p], writes=[ot])
                        k.dma("sp", lambda e: e.dma_start(out=PT[c0:c0 + w, tok0:tok0 + ntok], in_=ot[0:w, 0:ntok]), reads=[ot], writes=[PT])


    def colload(dst, src_ap_fn, srcbuf):
        k.dma("sp", lambda e: e.dma_start(out=dst, in_=src_ap_fn()), reads=[srcbuf], writes=[])

    SEGS = [(0, Tc)] + [(Tc + i * min(2048, T), min(2048, T)) for i in range(T // min(2048, T))]

    def gelu_tanh(dst, x, t1, n):
        pass

    def phase_lru(l):
        NCH = c.LW // 128
        with k.scope():
            par = k.sbuf("lpar", [128, 16, NCH], F32)
            for kk in range(4):
                k.dma("sp", lambda e: e.dma_start(out=par[:, kk, :], in_=I["conv_w"][l, kk, :].rearrange("(c p) -> p c", p=128)), reads=[I["conv_w"]], writes=[par])
            k.dma("sp", lambda e: e.dma_start(out=par[:, 4, :], in_=I["conv_b"][l, :].rearrange("(c p) -> p c", p=128)), reads=[I["conv_b"]], writes=[par])
            for d in range(2):
                k.dma("sp", lambda e: e.dma_start(out=par[:, 5 + d, :], in_=I["lru_lam"][l, d, :].rearrange("(c p) -> p c", p=128)), reads=[I["lru_lam"]], writes=[par])
                k.dma("sp", lambda e: e.dma_start(out=par[:, 7 + d, :], in_=I["lru_br"][l, d, :].rearrange("(c p) -> p c", p=128)), reads=[I["lru_br"]], writes=[par])
                k.dma("sp", lambda e: e.dma_start(out=par[:, 9 + d, :], in_=I["lru_bi"][l, d, :].rearrange("(c p) -> p c", p=128)), reads=[I["lru_bi"]], writes=[par])
            k.op("act", lambda: S.activation(out=par[:, 11:13, :], in_=par[:, 5:7, :], func=AF.Exp, scale=-1.0), reads=[par], writes=[par])
            k.op("dve", lambda: V.tensor_scalar(out=par[:, 11:13, :], in0=par[:, 11:13, :], scalar1=1.0, scalar2=None, op0=ALU.add), reads=[par], writes=[par])
            k.op("act", lambda: S.activation(out=par[:, 11:13, :], in_=par[:, 11:13, :], func=AF.Ln), reads=[par], writes=[par])
            k.op("dve", lambda: V.tensor_scalar(out=par[:, 13:15, :], in0=par[:, 11:13, :], scalar1=-16.0, scalar2=None, op0=ALU.mult), reads=[par], writes=[par])
            k.op("dve", lambda: V.tensor_scalar(out=par[:, 11:13, :], in0=par[:, 11:13, :], scalar1=-8.0, scalar2=None, op0=ALU.mult), reads=[par], writes=[par])
            XB = k.sbuf("lX", [128, TT], F32); U = k.sbuf("lU", [128, TT], F32)
            SG = max(sz for _, sz in SEGS)
            ub = k.sbuf("lub", [128, SG], BF16); r_ = k.sbuf("lr", [128, SG], F32); i_ = k.sbuf("li", [128, SG], F32)
            t_ = k.sbuf("lt", [128, SG], F32); h_ = k.sbuf("lh", [128, SG], F32); yb = k.sbuf("lyb", [128, SG], BF16)
            carry = k.sbuf("lcarry", [128, 1], F32)
            wf = k.sbuf("lwf", [128, 4, 128], F32); wb = k.sbuf("lwb", [128, 4, 128], BF16)
            for ch in range(NCH):
                pc = lambda j: par[:, j, ch:ch + 1]
                k.dma("sp", lambda e: e.dma_start(out=XB[:, :], in_=PT[c.o_lx + ch * 128:c.o_lx + (ch + 1) * 128, :]), reads=[PT], writes=[XB])
                for d in range(2):
                    k.dma("sp", lambda e: e.dma_start(out=wf[:, d, :], in_=I["lru_wr"][l, d, ch, :, :]), reads=[I["lru_wr"]], writes=[wf])
                    k.dma("sp", lambda e: e.dma_start(out=wf[:, 2 + d, :], in_=I["lru_wi"][l, d, ch, :, :]), reads=[I["lru_wi"]], writes=[wf])
                k.op("dve", lambda: V.tensor_copy(out=wb[:, :, :], in_=wf[:, :, :]), reads=[wf], writes=[wb])
                k.op("act", lambda: S.activation(out=U[:, :], in_=XB[:, :], func=AF.Identity, bias=pc(4), scale=pc(2)), reads=[XB, par], writes=[U])
                for (lo, hi) in ((0, Tc), (Tc, TT)):
                    for kk, sh in ((0, -2), (1, -1), (3, 1)):
                        a0, a1 = max(lo, lo - sh), min(hi, hi - sh)
                        k.op("dve", lambda: V.scalar_tensor_tensor(out=U[:, a0:a1], in0=XB[:, a0 + sh:a1 + sh], scalar=pc(kk), in1=U[:, a0:a1], op0=ALU.mult, op1=ALU.add),
                             reads=[XB, U, par], writes=[U])
                for d in range(2):
                    order = SEGS if d == 0 else [SEGS[0]] + SEGS[1:][::-1]
                    for si, (s0, sz) in enumerate(order):
                        k.op("dve", lambda: V.tensor_copy(out=ub[:, 0:sz], in_=U[:, s0:s0 + sz]), reads=[U], writes=[ub])
                        for p0 in range(0, sz, 512):
                            pw = min(512, sz - p0)
                            pr, pi = nps(), nps()
                            k.op("pe", lambda: A.matmul(pr[:, 0:pw], lhsT=wb[:, d, :], rhs=ub[:, p0:p0 + pw], start=True, stop=True), reads=[wb, ub], writes=[pr])
                            k.op("pe", lambda: A.matmul(pi[:, 0:pw], lhsT=wb[:, 2 + d, :], rhs=ub[:, p0:p0 + pw], start=True, stop=True), reads=[wb, ub], writes=[pi])
                            k.op("act", lambda: S.activation(out=r_[:, p0:p0 + pw], in_=pr[:, 0:pw], func=AF.Sigmoid, bias=pc(7 + d)), reads=[pr, par], writes=[r_])
                            k.op("act", lambda: S.activation(out=i_[:, p0:p0 + pw], in_=pi[:, 0:pw], func=AF.Sigmoid, bias=pc(9 + d)), reads=[pi, par], writes=[i_])
                        k.op("act", lambda: S.activation(out=t_[:, 0:sz], in_=r_[:, 0:sz], func=AF.Exp, scale=pc(13 + d)), reads=[r_, par], writes=[t_])
                        k.op("act", lambda: S.activation(out=r_[:, 0:sz], in_=r_[:, 0:sz], func=AF.Exp, scale=pc(11 + d)), reads=[r_, par], writes=[r_])
                        k.op("dve", lambda: V.tensor_scalar(out=t_[:, 0:sz], in0=t_[:, 0:sz], scalar1=-1.0, scalar2=1.0, op0=ALU.mult, op1=ALU.add), reads=[t_], writes=[t_])
                        k.op("act", lambda: S.activation(out=t_[:, 0:sz], in_=t_[:, 0:sz], func=AF.Sqrt), reads=[t_], writes=[t_])
                        k.op("dve", lambda: V.tensor_tensor(out=i_[:, 0:sz], in0=i_[:, 0:sz], in1=U[:, s0:s0 + sz], op=ALU.mult), reads=[i_, U], writes=[i_])
                        k.op("dve", lambda: V.tensor_tensor(out=i_[:, 0:sz], in0=i_[:, 0:sz], in1=t_[:, 0:sz], op=ALU.mult), reads=[i_, t_], writes=[i_])
                        init = 0.0 if si == 0 else carry[:, 0:1]
                        if d == 0:
                            k.op("dve", lambda: V.tensor_tensor_scan(out=h_[:, 0:sz], data0=r_[:, 0:sz], data1=i_[:, 0:sz], initial=init, op0=ALU.mult, op1=ALU.add),
                                 reads=[r_, i_, carry], writes=[h_])
                            k.op("dve", lambda: V.tensor_copy(out=carry[:, :], in_=h_[:, sz - 1:sz]), reads=[h_], writes=[carry])
                            k.op("act", lambda: S.copy(out=XB[:, s0:s0 + sz], in_=h_[:, 0:sz]), reads=[h_], writes=[XB])
                        else:
                            k.op("dve", lambda: V.tensor_tensor_scan(out=h_[:, sz - 1::-1] if False else h_[:, 0:sz][:, ::-1], data0=r_[:, 0:sz][:, ::-1], data1=i_[:, 0:sz][:, ::-1],
                                                                   initial=init, op0=ALU.mult, op1=ALU.add), reads=[r_, i_, carry], writes=[h_])
                            k.op("dve", lambda: V.tensor_copy(out=carry[:, :], in_=h_[:, 0:1]), reads=[h_], writes=[carry])
                            k.op("dve", lambda: V.tensor_tensor(out=XB[:, s0:s0 + sz], in0=XB[:, s0:s0 + sz], in1=h_[:, 0:sz], op=ALU.add), reads=[h_, XB], writes=[XB])
                for (s0, sz) in SEGS:
                    k.dma("sp", lambda e: e.dma_start(out=r_[:, 0:sz], in_=PT[c.o_lg + ch * 128:c.o_lg + (ch + 1) * 128, s0:s0 + sz]), reads=[PT], writes=[r_])
                    k.op("act", lambda: S.activation(out=t_[:, 0:sz], in_=r_[:, 0:sz], func=AF.Square), reads=[r_], writes=[t_])
                    k.op("dve", lambda: V.tensor_scalar(out=t_[:, 0:sz], in0=t_[:, 0:sz], scalar1=0.044715, scalar2=1.0, op0=ALU.mult, op1=ALU.add), reads=[t_], writes=[t_])
                    k.op("dve", lambda: V.tensor_tensor(out=t_[:, 0:sz], in0=t_[:, 0:sz], in1=r_[:, 0:sz], op=ALU.mult), reads=[t_, r_], writes=[t_])
                    k.op("act", lambda: S.activation(out=t_[:, 0:sz], in_=t_[:, 0:sz], func=AF.Sigmoid, scale=1.5957691216057308), reads=[t_], writes=[t_])
                    k.op("dve", lambda: V.tensor_tensor(out=t_[:, 0:sz], in0=t_[:, 0:sz], in1=r_[:, 0:sz], op=ALU.mult), reads=[t_, r_], writes=[t_])
                    k.op("dve", lambda: V.tensor_tensor(out=yb[:, 0:sz], in0=t_[:, 0:sz], in1=XB[:, s0:s0 + sz], op=ALU.mult), reads=[t_, XB], writes=[yb])
                    k.dma("sp", lambda e: e.dma_start(out=YT[ch * 128:(ch + 1) * 128, s0:s0 + sz], in_=yb[:, 0:sz]), reads=[yb], writes=[YT])


    ones_f = k.sbuf("ones_f", [128, 128], F32)
    k.op("dve", lambda: V.memset(ones_f[:, :], 1.0), writes=[ones_f])

    def phase_hg(l):
        with k.scope():
            SG = max(sz for _, sz in SEGS)
            NTs = SG // 128
            tri = k.sbuf("htri", [128, 2, 128], F32)
            k.op("dve", lambda: V.memset(tri[:, :, :], 0.0), writes=[tri])
            CH = 32
            NB = 128 // CH
            for d in range(2):
                for hb_ in range(NB):
                    k.dma("sp", lambda e: e.dma_start(out=tri[hb_ * CH:(hb_ + 1) * CH, d, hb_ * CH:(hb_ + 1) * CH], in_=I["tri"][d, 0:CH, 0:CH]), reads=[I["tri"]], writes=[tri])
            M = k.sbuf("hM", [128, 2, SG], F32)
            k.op("dve", lambda: V.memset(M[:, :, :], 1.0), writes=[M])
            k.op("dve", lambda: V.memset(M[:, 0, :].rearrange("p (c t) -> p c t", t=CH)[:, :, 0:1], 0.0), writes=[M])
            k.op("dve", lambda: V.memset(M[:, 1, :].rearrange("p (c t) -> p c t", t=CH)[:, :, CH - 1:CH], 0.0), writes=[M])
            lbt = k.sbuf("hlb", [128, 2, L, c.HH], F32); lbc = k.sbuf("hlbc", [128, 2, 2, c.HH], F32)
            gcol = k.sbuf("hgcol", [128, c.HH], F32)
            k.dma("sp", lambda e: e.dma_start(out=gcol[:, :], in_=I["hg_g"][l, :].rearrange("(h p) -> p h", p=128)), reads=[I["hg_g"]], writes=[gcol])
            for d in range(2):
                for ll in range(L):
                    k.dma("sp", lambda e: e.dma_start(out=lbt[:, d, ll, :], in_=I["hg_lb"][d, ll, :].rearrange("(h p) -> p h", p=128)), reads=[I["hg_lb"]], writes=[lbt])
            if l == 0:
                k.op("dve", lambda: V.memset(lbc[:, :, 0, :], 0.0), writes=[lbc])
            else:
                k.op("dve", lambda: V.tensor_tensor(out=lbc[:, :, 0, :], in0=lbt[:, :, 1, :], in1=lbt[:, :, 0, :], op=ALU.subtract), reads=[lbt], writes=[lbc])
                k.op("act", lambda: S.activation(out=lbc[:, :, 0, :], in_=lbc[:, :, 0, :], func=AF.Sigmoid), reads=[lbc], writes=[lbc])
            k.op("dve", lambda: V.tensor_scalar(out=lbc[:, :, 1, :], in0=lbc[:, :, 0, :], scalar1=-1.0, scalar2=1.0, op0=ALU.mult, op1=ALU.add), reads=[lbc], writes=[lbc])
            OS = k.sbuf("hOS", [128, TT], F32)
            q_ = k.sbuf("hq", [128, SG], F32); fr = k.sbuf("hfr", [128, SG], F32); g_ = k.sbuf("hg_", [128, SG], F32); kk_ = k.sbuf("hkk", [128, SG], F32)
            vT = k.sbuf("hvT", [128, SG], F32)
            qt = k.sbuf("hqt", [128, SG], BF16); kt = k.sbuf("hkt", [128, SG], BF16); kh = k.sbuf("hkh", [128, SG], BF16); vb = k.sbuf("hvb", [128, SG], BF16)
            vtok = k.sbuf("hvtok", [128, NTs, 128], BF16); khc = k.sbuf("hkhc", [CH, NTs * NB, 128], BF16); vc = k.sbuf("hvc", [CH, NTs * NB, 128], BF16)
            egl = k.sbuf("hegl", [128, SG // CH], F32)
            Sf = k.sbuf("hSf", [128, 128], F32); Sb = k.sbuf("hSb", [128, 128], BF16)
            ams = [k.sbuf("ham%d" % i, [128, 128], BF16) for i in range(2)]
            sq = k.sbuf("hsq", [128, 512], F32); rs = k.sbuf("hrs", [128, 512], F32); yb = k.sbuf("hyb", [128, 512], BF16)
            for h in range(c.HH):
                for d in range(2):
                    order = SEGS if d == 0 else [SEGS[0]] + SEGS[1:][::-1]
                    k.op("dve", lambda: V.memset(Sf[:, :], 0.0), writes=[Sf])
                    k.op("dve", lambda: V.memset(Sb[:, :], 0.0), writes=[Sb])
                    fo = c.o_hff if d == 0 else c.o_hfb
                    for (s0, sz) in order:
                        nch = sz // CH
                        k.dma("sp", lambda e: e.dma_start(out=q_[:, 0:sz], in_=PT[c.o_hq + h * 128:c.o_hq + (h + 1) * 128, s0:s0 + sz]), reads=[PT], writes=[q_])
                        k.dma("sp", lambda e: e.dma_start(out=fr[:, 0:sz], in_=PT[fo + h * 128:fo + (h + 1) * 128, s0:s0 + sz]), reads=[PT], writes=[fr])
                        k.dma("sp", lambda e: e.dma_start(out=vT[:, 0:sz], in_=PT[c.o_hi + h * 128:c.o_hi + (h + 1) * 128, s0:s0 + sz]), reads=[PT], writes=[vT])
                        k.op("act", lambda: S.activation(out=fr[:, 0:sz], in_=fr[:, 0:sz], func=AF.Sigmoid), reads=[fr], writes=[fr])
                        k.op("dve", lambda: V.tensor_scalar(out=fr[:, 0:sz], in0=fr[:, 0:sz], scalar1=lbc[:, d, 1, h:h + 1], scalar2=lbc[:, d, 0, h:h + 1], op0=ALU.mult, op1=ALU.add),
                             reads=[fr, lbc], writes=[fr])
                        k.op("dve", lambda: V.tensor_scalar(out=kk_[:, 0:sz], in0=fr[:, 0:sz], scalar1=-1.0, scalar2=1.0, op0=ALU.mult, op1=ALU.add), reads=[fr], writes=[kk_])
                        k.op("act", lambda: S.activation(out=fr[:, 0:sz], in_=fr[:, 0:sz], func=AF.Ln), reads=[fr], writes=[fr])
                        if d == 0:
                            k.op("dve", lambda: V.tensor_tensor_scan(out=g_[:, 0:sz], data0=M[:, 0, 0:sz], data1=fr[:, 0:sz], initial=0.0, op0=ALU.mult, op1=ALU.add),
                                 reads=[M, fr], writes=[g_])
                        else:
                            k.op("dve", lambda: V.tensor_tensor_scan(out=g_[:, 0:sz][:, ::-1], data0=M[:, 1, 0:sz][:, ::-1], data1=fr[:, 0:sz][:, ::-1], initial=0.0,
                                                                   op0=ALU.mult, op1=ALU.add), reads=[M, fr], writes=[g_])
                        gl = lambda: g_[:, 0:sz].rearrange("p (c t) -> p c t", t=CH)[:, :, (CH - 1 if d == 0 else 0)]
                        k.op("act", lambda: S.activation(out=egl[:, 0:nch], in_=gl(), func=AF.Exp), reads=[g_], writes=[egl])
                        k.op("act", lambda: S.activation(out=fr[:, 0:sz], in_=g_[:, 0:sz], func=AF.Exp), reads=[g_], writes=[fr])
                        k.op("dve", lambda: V.tensor_tensor(out=qt[:, 0:sz], in0=q_[:, 0:sz], in1=fr[:, 0:sz], op=ALU.mult), reads=[q_, fr], writes=[qt])
                        k.op("act", lambda: S.activation(out=fr[:, 0:sz], in_=g_[:, 0:sz], func=AF.Exp, scale=-1.0), reads=[g_], writes=[fr])
                        k.op("dve", lambda: V.tensor_tensor(out=kk_[:, 0:sz], in0=kk_[:, 0:sz], in1=fr[:, 0:sz], op=ALU.mult), reads=[kk_, fr], writes=[kk_])
                        k.op("act", lambda: S.copy(out=kt[:, 0:sz], in_=kk_[:, 0:sz]), reads=[kk_], writes=[kt])
                        k.op("dve", lambda: V.tensor_tensor(out=kh[:, 0:sz].rearrange("p (c t) -> p c t", t=CH), in0=kk_[:, 0:sz].rearrange("p (c t) -> p c t", t=CH),
                                                            in1=egl[:, 0:nch].unsqueeze(2).to_broadcast([128, nch, CH]), op=ALU.mult), reads=[kk_, egl], writes=[kh])
                        k.op("act", lambda: S.copy(out=vb[:, 0:sz], in_=vT[:, 0:sz]), reads=[vT], writes=[vb])
                        if h == 0 and d == 0 and s0 == 0:
                            _dump(k, c, "g", g_, lambda: g_[:, 0:sz], [128, sz]); _dump(k, c, "qt", qt, lambda: qt[:, 0:sz], [128, sz], BF16)
                            _dump(k, c, "kt", kt, lambda: kt[:, 0:sz], [128, sz], BF16); _dump(k, c, "kh", kh, lambda: kh[:, 0:sz], [128, sz], BF16)
                            _dump(k, c, "egl", egl, lambda: egl[:, 0:nch], [128, nch])
                        if c.hgcut == 1:
                            return
                        nt = sz // 128
                        for src, dst in ((vb, vc), (kh, khc)):
                            for t0 in range(0, nt * NB, 8):
                                n8 = min(8, nt * NB - t0)
                                p = nps(); pb = p[:, :].bitcast(BF16)
                                for j in range(n8):
                                    k.op("pe", lambda: A.transpose(out=pb[0:CH, j * 128:(j + 1) * 128], in_=src[:, (t0 + j) * CH:(t0 + j + 1) * CH], identity=ident_b[:, :]),
                                         reads=[src, ident_b], writes=[p])
                                k.op("act", lambda: S.copy(out=dst[:, t0:t0 + n8, :], in_=pb[0:CH, 0:n8 * 128].rearrange("p (a b) -> p a b", a=n8)), reads=[p], writes=[dst])
                        for src, dst in ((vb, vtok),):
                            for t0 in range(0, nt, 8):
                                n8 = min(8, nt - t0)
                                p = nps(); pb = p[:, :].bitcast(BF16)
                                for j in range(n8):
                                    k.op("pe", lambda: A.transpose(out=pb[:, j * 128:(j + 1) * 128], in_=src[:, (t0 + j) * 128:(t0 + j + 1) * 128], identity=ident_b[:, :]),
                                         reads=[src, ident_b], writes=[p])
                                k.op("dve", lambda: V.tensor_copy(out=dst[:, t0:t0 + n8, :], in_=pb[:, 0:n8 * 128].rearrange("p (a b) -> p a b", a=n8)), reads=[p], writes=[dst])
                        if c.hgcut == 2:
                            return
                        blocks = list(range(0, sz, 512))
                        if d == 1:
                            blocks = blocks[::-1]
                        na = 0
                        for b0 in blocks:
                            bw = min(512, sz - b0)
                            po = npo()
                            tiles = list(range(b0 // 128, (b0 + bw) // 128))
                            if d == 1:
                                tiles = tiles[::-1]
                            for ti in tiles:
                                lo = ti * 128 - b0
                                pa = nps()
                                k.op("pe", lambda: A.matmul(pa[:, 0:128], lhsT=kt[:, ti * 128:(ti + 1) * 128], rhs=qt[:, ti * 128:(ti + 1) * 128], start=True, stop=True),
                                     reads=[kt, qt], writes=[pa])
                                am = ams[na % 2]; na += 1
                                k.op("dve", lambda: V.tensor_tensor(out=am[:, :], in0=pa[:, 0:128], in1=tri[:, d, :], op=ALU.mult), reads=[pa, tri], writes=[am])
                                k.op("pe", lambda: A.matmul(po[:, lo:lo + 128], lhsT=vtok[:, ti, :], rhs=am[:, :], start=True, stop=False), reads=[vtok, am], writes=[po])
                                if c.hgcut == 3:
                                    return
                                chs = list(range(NB)) if d == 0 else list(range(NB))[::-1]
                                for ci, cc in enumerate(chs):
                                    cidx_ = ti * NB + cc
                                    k.op("pe", lambda: A.matmul(po[:, lo + cc * CH:lo + (cc + 1) * CH], lhsT=Sb[:, :], rhs=qt[:, cidx_ * CH:(cidx_ + 1) * CH], start=False, stop=True),
                                         reads=[Sb, qt], writes=[po], pe_acc=True)
                                    if c.hgcut == 4:
                                        return
                                    pst = nps()
                                    k.op("pe", lambda: A.matmul(pst[:, 0:128], lhsT=khc[:, cidx_, :], rhs=vc[:, cidx_, :], start=True, stop=True),
                                         reads=[khc, vc], writes=[pst])
                                    k.op("dve", lambda: V.scalar_tensor_tensor(out=Sf[:, :], in0=Sf[:, :], scalar=egl[:, cidx_:cidx_ + 1], in1=pst[:, 0:128], op0=ALU.mult, op1=ALU.add),
                                         reads=[Sf, egl, pst], writes=[Sf])
                                    k.op("act", lambda: S.copy(out=Sb[:, :], in_=Sf[:, :]), reads=[Sf], writes=[Sb])
                            if d == 0:
                                k.op("dve", lambda: V.tensor_copy(out=OS[:, s0 + b0:s0 + b0 + bw], in_=po[:, 0:bw]), reads=[po], writes=[OS])
                            else:
                                k.op("dve", lambda: V.tensor_tensor(out=OS[:, s0 + b0:s0 + b0 + bw], in0=OS[:, s0 + b0:s0 + b0 + bw], in1=po[:, 0:bw], op=ALU.add), reads=[po, OS], writes=[OS])
                if h == 0:
                    _dump(k, c, "OS", OS, lambda: OS[:, :], [128, TT])
                for b0 in range(0, TT, 512):
                    bw = min(512, TT - b0)
                    k.op("act", lambda: S.activation(out=sq[:, 0:bw], in_=OS[:, b0:b0 + bw], func=AF.Square), reads=[OS], writes=[sq])
                    p = nps()
                    k.op("pe", lambda: A.matmul(p[:, 0:bw], lhsT=ones_f[:, :], rhs=sq[:, 0:bw], start=True, stop=True), reads=[ones_f, sq], writes=[p])
                    k.op("dve", lambda: V.tensor_scalar(out=rs[:, 0:bw], in0=p[:, 0:bw], scalar1=1.0 / 128, scalar2=EPS, op0=ALU.mult, op1=ALU.add), reads=[p], writes=[rs])
                    k.op("act", lambda: S.activation(out=rs[:, 0:bw], in_=rs[:, 0:bw], func=AF.Sqrt), reads=[rs], writes=[rs])
                    k.op("dve", lambda: V.reciprocal(out=rs[:, 0:bw], in_=rs[:, 0:bw]), reads=[rs], writes=[rs])
                    k.op("dve", lambda: V.scalar_tensor_tensor(out=rs[:, 0:bw], in0=rs[:, 0:bw], scalar=gcol[:, h:h + 1], in1=OS[:, b0:b0 + bw], op0=ALU.mult, op1=ALU.mult),
                         reads=[rs, gcol, OS], writes=[rs])
                    k.dma("sp", lambda e: e.dma_start(out=sq[:, 0:bw], in_=PT[c.o_hg + h * 128:c.o_hg + (h + 1) * 128, b0:b0 + bw]), reads=[PT], writes=[sq])
                    k.op("act", lambda: S.activation(out=sq[:, 0:bw], in_=sq[:, 0:bw], func=AF.Silu), reads=[sq], writes=[sq])
                    k.op("dve", lambda: V.tensor_tensor(out=yb[:, 0:bw], in0=rs[:, 0:bw], in1=sq[:, 0:bw], op=ALU.mult), reads=[rs, sq], writes=[yb])
                    k.dma("sp", lambda e: e.dma_start(out=YT[c.LW + h * 128:c.LW + (h + 1) * 128, b0:b0 + bw], in_=yb[:, 0:bw]), reads=[yb], writes=[YT])


    QT = k.dram("QT", [c.MH * 192, TT], BF16)
    KT = k.dram("KT", [c.MH * 128, TT], BF16)
    KRT = k.dram("KRT", [64, TT], BF16)
    VV = k.dram("VV", [TT, c.MH * 128], BF16)
    rt_f = k.sbuf("rt_f", [128, 128], F32)
    k.dma("sp", lambda e: e.dma_start(out=rt_f[:, :], in_=I["rope_rt"][:, :]), reads=[I["rope_rt"]], writes=[rt_f])

    def rope_apply(p, nrow, ntok, cs, xs, t1, outb):
        k.op("act", lambda: S.copy(out=xs[0:nrow, 0:ntok], in_=p[0:nrow, 0:ntok]), reads=[p], writes=[xs])
        p2 = nps()
        k.op("pe", lambda: A.matmul(p2[0:nrow, 0:ntok], lhsT=rt_f[0:nrow, 0:nrow], rhs=xs[0:nrow, 0:ntok], start=True, stop=True), reads=[rt_f, xs], writes=[p2])
        k.op("dve", lambda: V.tensor_tensor(out=t1[0:nrow, 0:ntok], in0=xs[0:nrow, 0:ntok], in1=cs[0:nrow, 0, 0:ntok], op=ALU.mult), reads=[xs, cs], writes=[t1])
        k.op("dve", lambda: V.tensor_tensor(out=xs[0:nrow, 0:ntok], in0=p2[0:nrow, 0:ntok], in1=cs[0:nrow, 1, 0:ntok], op=ALU.mult), reads=[p2, cs], writes=[xs])
        k.op("dve", lambda: V.tensor_tensor(out=outb[0:nrow, 0:ntok], in0=t1[0:nrow, 0:ntok], in1=xs[0:nrow, 0:ntok], op=ALU.add), reads=[t1, xs], writes=[outb])

    def phase_mla_proj(l):
        QC, VC = c.QR // 128, c.KVR // 128
        with k.scope():
            wuq = k.sbuf("wuq", [128, QC, c.MH * 192], BF16); wukv = k.sbuf("wukv", [128, VC, c.MH * 256], BF16)
            wtmp = k.sbuf("wtmp", [128, max(c.MH * 192, c.MH * 256)], F32)
            for kc in range(QC):
                k.dma("sp", lambda e: e.dma_start(out=wtmp[:, 0:c.MH * 192], in_=I["w_uq"][l, kc * 128:(kc + 1) * 128, :]), reads=[I["w_uq"]], writes=[wtmp])
                k.op("dve", lambda: V.tensor_copy(out=wuq[:, kc, :], in_=wtmp[:, 0:c.MH * 192]), reads=[wtmp], writes=[wuq])
            for kc in range(VC):
                k.dma("sp", lambda e: e.dma_start(out=wtmp[:, 0:c.MH * 256], in_=I["w_ukv"][l, kc * 128:(kc + 1) * 128, :]), reads=[I["w_ukv"]], writes=[wtmp])
                k.op("dve", lambda: V.tensor_copy(out=wukv[:, kc, :], in_=wtmp[:, 0:c.MH * 256]), reads=[wtmp], writes=[wukv])
            gq = k.sbuf("gq", [128, QC], F32); gkv = k.sbuf("gkv", [128, VC], F32)
            k.dma("sp", lambda e: e.dma_start(out=gq[:, :], in_=I["q_g"][l, :].rearrange("(c p) -> p c", p=128)), reads=[I["q_g"]], writes=[gq])
            k.dma("sp", lambda e: e.dma_start(out=gkv[:, :], in_=I["kv_g"][l, :].rearrange("(c p) -> p c", p=128)), reads=[I["kv_g"]], writes=[gkv])
            xin = k.sbuf("mxin", [128, QC, 512], F32); xn = k.sbuf("mxn", [128, QC, 512], BF16)
            sq = k.sbuf("msq", [128, 512], F32); rs = k.sbuf("mrs", [128, 512], F32)
            cs = k.sbuf("mcs", [128, 2, 512], F32); xs = k.sbuf("mxs", [128, 512], F32); t1 = k.sbuf("mt1", [128, 512], F32)
            obs = [k.sbuf("mob%d" % i, [128, 512], BF16) for i in range(3)]
            no = [0]

            def normed(row0, nchunk, gcol, ntok, tok0):
                k.dma("sp", lambda e: e.dma_start(out=xin[:, 0:nchunk, 0:ntok], in_=PT[row0:row0 + nchunk * 128, tok0:tok0 + ntok].rearrange("(c p) t -> p c t", p=128)),
                      reads=[PT], writes=[xin])
                p = nps()
                for cc in range(nchunk):
                    k.op("act", lambda: S.activation(out=sq[:, 0:ntok], in_=xin[:, cc, 0:ntok], func=AF.Square), reads=[xin], writes=[sq])
                    k.op("pe", lambda: A.matmul(p[:, 0:ntok], lhsT=ones_f[:, :], rhs=sq[:, 0:ntok], start=(cc == 0), stop=(cc == nchunk - 1)), reads=[ones_f, sq], writes=[p], pe_acc=(cc > 0))
                k.op("dve", lambda: V.tensor_scalar(out=rs[:, 0:ntok], in0=p[:, 0:ntok], scalar1=1.0 / (nchunk * 128), scalar2=EPS, op0=ALU.mult, op1=ALU.add), reads=[p], writes=[rs])
                k.op("act", lambda: S.activation(out=rs[:, 0:ntok], in_=rs[:, 0:ntok], func=AF.Sqrt), reads=[rs], writes=[rs])
                k.op("dve", lambda: V.reciprocal(out=rs[:, 0:ntok], in_=rs[:, 0:ntok]), reads=[rs], writes=[rs])
                for cc in range(nchunk):
                    k.op("dve", lambda: V.scalar_tensor_tensor(out=xn[:, cc, 0:ntok], in0=xin[:, cc, 0:ntok], scalar=gcol[:, cc:cc + 1], in1=rs[:, 0:ntok], op0=ALU.mult, op1=ALU.mult),
                         reads=[xin, gcol, rs], writes=[xn])

            def nob():
                no[0] += 1
                return obs[no[0] % 3]

            for (tok0, ntok, is_ctx) in c.blocks:
                if not is_ctx:
                    k.dma("sp", lambda e: e.dma_start(out=cs[:, :, 0:ntok], in_=I["rope_cs"][:, :, tok0 - Tc:tok0 - Tc + ntok].rearrange("a p t -> p a t")), reads=[I["rope_cs"]], writes=[cs])
                normed(c.o_cq, QC, gq, ntok, tok0)
                for h in range(c.MH):
                    for (c0, w, rope) in ((h * 192, 128, False), (h * 192 + 128, 64, True)):
                        p = nps()
                        for kc in range(QC):
                            k.op("pe", lambda: A.matmul(p[0:w, 0:ntok], lhsT=wuq[:, kc, c0:c0 + w], rhs=xn[:, kc, 0:ntok], start=(kc == 0), stop=(kc == QC - 1)),
                                 reads=[wuq, xn], writes=[p], pe_acc=(kc > 0))
                        ob = nob()
                        if rope and not is_ctx:
                            rope_apply(p, w, ntok, cs, xs, t1, ob)
                        else:
                            k.op("act", lambda: S.copy(out=ob[0:w, 0:ntok], in_=p[0:w, 0:ntok]), reads=[p], writes=[ob])
                        k.dma("sp", lambda e: e.dma_start(out=QT[c0:c0 + w, tok0:tok0 + ntok], in_=ob[0:w, 0:ntok]), reads=[ob], writes=[QT])
                normed(c.o_ckv, VC, gkv, ntok, tok0)
                for h in range(c.MH):
                    p = nps()
                    for kc in range(VC):
                        k.op("pe", lambda: A.matmul(p[:, 0:ntok], lhsT=wukv[:, kc, h * 256:h * 256 + 128], rhs=xn[:, kc, 0:ntok], start=(kc == 0), stop=(kc == VC - 1)),
                             reads=[wukv, xn], writes=[p], pe_acc=(kc > 0))
                    ob = nob()
                    k.op("act", lambda: S.copy(out=ob[:, 0:ntok], in_=p[:, 0:ntok]), reads=[p], writes=[ob])
                    k.dma("sp", lambda e: e.dma_start(out=KT[h * 128:(h + 1) * 128, tok0:tok0 + ntok], in_=ob[:, 0:ntok]), reads=[ob], writes=[KT])
                for ti in range(ntok // 128):
                    for h0 in range(0, c.MH, 4):
                        nh = min(4, c.MH - h0)
                        p = nps()
                        for hh in range(nh):
                            for kc in range(VC):
                                k.op("pe", lambda: A.matmul(p[:, hh * 128:(hh + 1) * 128], lhsT=xn[:, kc, ti * 128:(ti + 1) * 128], rhs=wukv[:, kc, (h0 + hh) * 256 + 128:(h0 + hh) * 256 + 256],
                                                            start=(kc == 0), stop=(kc == VC - 1)), reads=[wukv, xn], writes=[p], pe_acc=(kc > 0 or hh > 0))
                        ob = nob()
                        k.op("dve", lambda: V.tensor_copy(out=ob[:, 0:nh * 128], in_=p[:, 0:nh * 128]), reads=[p], writes=[ob])
                        k.dma("sp", lambda e: e.dma_start(out=VV[tok0 + ti * 128:tok0 + (ti + 1) * 128, h0 * 128:(h0 + nh) * 128], in_=ob[:, 0:nh * 128]), reads=[ob], writes=[VV])
                k.dma("sp", lambda e: e.dma_start(out=xs[0:64, 0:ntok], in_=PT[c.o_kr:c.o_kr + 64, tok0:tok0 + ntok]), reads=[PT], writes=[xs])
                ob = nob()
                if is_ctx:
                    k.op("act", lambda: S.copy(out=ob[0:64, 0:ntok], in_=xs[0:64, 0:ntok]), reads=[xs], writes=[ob])
                else:
                    p2 = nps()
                    k.op("pe", lambda: A.matmul(p2[0:64, 0:ntok], lhsT=rt_f[0:64, 0:64], rhs=xs[0:64, 0:ntok], start=True, stop=True), reads=[rt_f, xs], writes=[p2])
                    k.op("dve", lambda: V.tensor_tensor(out=t1[0:64, 0:ntok], in0=xs[0:64, 0:ntok], in1=cs[0:64, 0, 0:ntok], op=ALU.mult), reads=[xs, cs], writes=[t1])
                    k.op("dve", lambda: V.tensor_tensor(out=sq[0:64, 0:ntok], in0=p2[0:64, 0:ntok], in1=cs[0:64, 1, 0:ntok], op=ALU.mult), reads=[p2, cs], writes=[sq])
                    k.op("dve", lambda: V.tensor_tensor(out=ob[0:64, 0:ntok], in0=t1[0:64, 0:ntok], in1=sq[0:64, 0:ntok], op=ALU.add), reads=[t1, sq], writes=[ob])
                k.dma("sp", lambda e: e.dma_start(out=KRT[:, tok0:tok0 + ntok], in_=ob[0:64, 0:ntok]), reads=[ob], writes=[KRT])

    def attention_core(q_tiles, nq0, q0_fn, q1_fn, key_parts, v_fn, scale, out_fn, mask_fn=None, sink_col=None, dv=128, st=None):
        pass

    def phase_mla_attn(l, need_ctx):
        NT = TT // 128
        scale = 192.0 ** -0.5
        with k.scope():
            kt0 = k.sbuf("akt0", [128, TT], BF16); kt1 = k.sbuf("akt1", [64, TT], BF16); vtok = k.sbuf("avtok", [128, NT, 128], BF16)
            Ssb = k.sbuf("aS", [128, TT], F32); Pb = k.sbuf("aP", [128, TT], BF16); PTr = k.sbuf("aPT", [128, NT, 128], BF16)
            q0s = [k.sbuf("aq0%d" % i, [128, 128], BF16) for i in range(2)]; q1s = [k.sbuf("aq1%d" % i, [64, 128], BF16) for i in range(2)]
            mx = k.sbuf("amx", [128, 1], F32); rsum = k.sbuf("ars", [128, 1], F32); ob = k.sbuf("aob", [128, 128], BF16); oT = k.sbuf("aoT", [128, 512], BF16)
            k.dma("sp", lambda e: e.dma_start(out=kt1[:, :], in_=KRT[:, :]), reads=[KRT], writes=[kt1])
            nq = 0
            for h in range(c.MH):
                k.dma("sp", lambda e: e.dma_start(out=kt0[:, :], in_=KT[h * 128:(h + 1) * 128, :]), reads=[KT], writes=[kt0])
                for tq in range(0, NT, 8):
                    tn = min(8, NT - tq)
                    k.dma("sp", lambda e: e.dma_start(out=vtok[:, tq:tq + tn, :], in_=VV[tq * 128:(tq + tn) * 128, h * 128:(h + 1) * 128].rearrange("(t p) v -> p t v", p=128)), reads=[VV], writes=[vtok])
                qtiles = ([(i * 128, 0, Tc) for i in range(Tc // 128)] if need_ctx else []) + [(Tc + i * 128, 0, TT) for i in range(T // 128)]
                for qi, (qs, k0, k1) in enumerate(qtiles):
                    q0, q1 = q0s[nq % 2], q1s[nq % 2]; nq += 1
                    k.dma("sp", lambda e: e.dma_start(out=q0[:, :], in_=QT[h * 192:h * 192 + 128, qs:qs + 128]), reads=[QT], writes=[q0])
                    k.dma("sp", lambda e: e.dma_start(out=q1[:, :], in_=QT[h * 192 + 128:h * 192 + 192, qs:qs + 128]), reads=[QT], writes=[q1])
                    nk = k1 - k0
                    for kb in range(k0, k1, 512):
                        kw = min(512, k1 - kb)
                        p = nps()
                        k.op("pe", lambda: A.matmul(p[:, 0:kw], lhsT=q0[:, :], rhs=kt0[:, kb:kb + kw], start=True, stop=False), reads=[q0, kt0], writes=[p])
                        k.op("pe", lambda: A.matmul(p[:, 0:kw], lhsT=q1[:, :], rhs=kt1[:, kb:kb + kw], start=False, stop=True), reads=[q1, kt1], writes=[p], pe_acc=True)
                        if (kb // 512) % 2:
                            k.op("act", lambda: S.activation(out=Ssb[:, kb:kb + kw], in_=p[:, 0:kw], func=AF.Copy, scale=scale), reads=[p], writes=[Ssb])
                        else:
                            k.op("dve", lambda: V.tensor_scalar(out=Ssb[:, kb:kb + kw], in0=p[:, 0:kw], scalar1=scale, scalar2=None, op0=ALU.mult), reads=[p], writes=[Ssb])
                    k.op("dve", lambda: V.tensor_reduce(out=mx[:, :], in_=Ssb[:, k0:k1], axis=AX.X, op=ALU.max), reads=[Ssb], writes=[mx])
                    k.op("dve", lambda: V.tensor_scalar(out=mx[:, :], in0=mx[:, :], scalar1=-1.0, scalar2=None, op0=ALU.mult), reads=[mx], writes=[mx])
                    k.op("act", lambda: S.activation(out=Pb[:, k0:k1], in_=Ssb[:, k0:k1], func=AF.Exp, bias=mx[:, 0:1], accum_out=rsum[:, 0:1]), reads=[Ssb, mx], writes=[Pb, rsum])
                    kts = list(range(k0 // 128, k1 // 128))
                    for t0 in range(0, len(kts), 8):
                        n8 = min(8, len(kts) - t0)
                        p = nps(); pb = p[:, :].bitcast(BF16)
                        for j in range(n8):
                            kt_ = kts[t0 + j]
                            k.op("pe", lambda: A.transpose(out=pb[:, j * 128:(j + 1) * 128], in_=Pb[:, kt_ * 128:(kt_ + 1) * 128], identity=ident_b[:, :]), reads=[Pb, ident_b], writes=[p])
                        tt0 = kts[t0]
                        if (t0 // 8) % 2:
                            k.op("act", lambda: S.copy(out=PTr[:, tt0:tt0 + n8, :], in_=pb[:, 0:n8 * 128].rearrange("p (a b) -> p a b", a=n8)), reads=[p], writes=[PTr])
                        else:
                            k.op("dve", lambda: V.tensor_copy(out=PTr[:, tt0:tt0 + n8, :], in_=pb[:, 0:n8 * 128].rearrange("p (a b) -> p a b", a=n8)), reads=[p], writes=[PTr])
                    po = npo()
                    for j, kt_ in enumerate(kts):
                        k.op("pe", lambda: A.matmul(po[:, 0:128], lhsT=PTr[:, kt_, :], rhs=vtok[:, kt_, :], start=(j == 0), stop=(j == len(kts) - 1)), reads=[PTr, vtok], writes=[po], pe_acc=(j > 0))
                    k.op("dve", lambda: V.reciprocal(out=rsum[:, :], in_=rsum[:, :]), reads=[rsum], writes=[rsum])
                    k.op("dve", lambda: V.tensor_scalar(out=ob[:, :], in0=po[:, 0:128], scalar1=rsum[:, 0:1], scalar2=None, op0=ALU.mult), reads=[po, rsum], writes=[ob])
                    p = nps(); pb = p[:, :].bitcast(BF16)
                    k.op("pe", lambda: A.transpose(out=pb[:, 0:128], in_=ob[:, :], identity=ident_b[:, :]), reads=[ob, ident_b], writes=[p])
                    k.op("act", lambda: S.copy(out=oT[:, 0:128], in_=pb[:, 0:128]), reads=[p], writes=[oT])
                    r0 = c.LW + c.HK + h * 128
                    k.dma("sp", lambda e: e.dma_start(out=YT[r0:r0 + 128, qs:qs + 128], in_=oT[:, 0:128]), reads=[oT], writes=[YT])

    def phase_mla(l, need_ctx=True):
        phase_mla_proj(l)
        phase_mla_attn(l, need_ctx)


    def phase_win(l, need_ctx=True):
        NT = TT // 128
        scale = 64.0 ** -0.5
        G = c.WH // c.WKV
        with k.scope():
            wm = k.sbuf("wwm", [128, 384], F32)
            k.dma("sp", lambda e: e.dma_start(out=wm[:, :], in_=I["wmask"][:, :]), reads=[I["wmask"]], writes=[wm])
            sinkc = k.sbuf("wsink", [128, c.WH], F32)
            k.dma("sp", lambda e: e.dma_start(out=sinkc[:, :], in_=I["sink"][l:l + 1, :].partition_broadcast(128)), reads=[I["sink"]], writes=[sinkc])
            cs = k.sbuf("wcs", [128, 2, 512], F32); xs = k.sbuf("wxs", [128, 512], F32); t1 = k.sbuf("wt1", [128, 512], F32); t2 = k.sbuf("wt2", [128, 512], F32)
            xin = k.sbuf("wxin", [64, 512], F32)
            kb = k.sbuf("wkb", [64, TT], BF16); vb = k.sbuf("wvb", [64, TT], BF16); vtok = k.sbuf("wvtok", [128, NT, 64], BF16); qb = k.sbuf("wqb", [64, TT], BF16)
            Ssb = k.sbuf("wS", [128, 384 + Tc], F32); Pb = k.sbuf("wP", [128, 384 + Tc], BF16); PTr = k.sbuf("wPT", [128, (384 + Tc) // 128, 128], BF16)
            mx = k.sbuf("wmx", [128, 1], F32); rsum = k.sbuf("wrs", [128, 1], F32); es = k.sbuf("wes", [128, 1], F32)
            ob = k.sbuf("wob", [128, 64], BF16); oT = k.sbuf("woT", [64, 128], BF16)

            def load_rope(row0, dst, do_rope=True):
                for (tok0, ntok, is_ctx) in c.blocks:
                    k.dma("sp", lambda e: e.dma_start(out=xin[:, 0:ntok], in_=PT[row0:row0 + 64, tok0:tok0 + ntok]), reads=[PT], writes=[xin])
                    if is_ctx or not do_rope:
                        k.op("act", lambda: S.copy(out=dst[:, tok0:tok0 + ntok], in_=xin[:, 0:ntok]), reads=[xin], writes=[dst])
                    else:
                        k.dma("sp", lambda e: e.dma_start(out=cs[:, :, 0:ntok], in_=I["rope_cs"][:, :, tok0 - Tc:tok0 - Tc + ntok].rearrange("a p t -> p a t")), reads=[I["rope_cs"]], writes=[cs])
                        p2 = nps()
                        k.op("pe", lambda: A.matmul(p2[0:64, 0:ntok], lhsT=rt_f[0:64, 0:64], rhs=xin[:, 0:ntok], start=True, stop=True), reads=[rt_f, xin], writes=[p2])
                        k.op("dve", lambda: V.tensor_tensor(out=t1[0:64, 0:ntok], in0=xin[:, 0:ntok], in1=cs[0:64, 0, 0:ntok], op=ALU.mult), reads=[xin, cs], writes=[t1])
                        k.op("dve", lambda: V.tensor_tensor(out=t2[0:64, 0:ntok], in0=p2[0:64, 0:ntok], in1=cs[0:64, 1, 0:ntok], op=ALU.mult), reads=[p2, cs], writes=[t2])
                        k.op("dve", lambda: V.tensor_tensor(out=dst[:, tok0:tok0 + ntok], in0=t1[0:64, 0:ntok], in1=t2[0:64, 0:ntok], op=ALU.add), reads=[t1, t2], writes=[dst])

            for kv in range(c.WKV):
                load_rope(c.o_wk + kv * 64, kb)
                load_rope(c.o_wv + kv * 64, vb, do_rope=False)
                for t0 in range(0, NT, 8):
                    n8 = min(8, NT - t0)
                    p = nps(); pb = p[:, :].bitcast(BF16)
                    for j in range(n8):
                        k.op("pe", lambda: A.transpose(out=pb[:, j * 64:(j + 1) * 64], in_=vb[:, (t0 + j) * 128:(t0 + j + 1) * 128], identity=ident_b[0:64, 0:64]), reads=[vb, ident_b], writes=[p])
                    k.op("dve", lambda: V.tensor_copy(out=vtok[:, t0:t0 + n8, :], in_=pb[:, 0:n8 * 64].rearrange("p (a b) -> p a b", a=n8)), reads=[p], writes=[vtok])
                for g in range(G):
                    h = kv * G + g
                    load_rope(c.o_wq + h * 64, qb)
                    qtiles = []
                    if need_ctx:
                        qtiles += [(i * 128, [(0, Tc, None)]) for i in range(Tc // 128)]
                    for b in range(T // 128):
                        lo, hi = max(0, b * 128 - 128), min(T, b * 128 + 256)
                        qtiles.append((Tc + b * 128, [(Tc + lo, hi - lo, lo - (b * 128 - 128)), (0, Tc, None)]))
                    for (qs, parts) in qtiles:
                        off = 0
                        for (ks, kl, moff) in parts:
                            p = nps()
                            k.op("pe", lambda: A.matmul(p[:, 0:kl], lhsT=qb[:, qs:qs + 128], rhs=kb[:, ks:ks + kl], start=True, stop=True), reads=[qb, kb], writes=[p])
                            if moff is not None:
                                k.op("dve", lambda: V.scalar_tensor_tensor(out=Ssb[:, off:off + kl], in0=p[:, 0:kl], scalar=scale, in1=wm[:, moff:moff + kl], op0=ALU.mult, op1=ALU.add),
                                     reads=[p, wm], writes=[Ssb])
                            else:
                                k.op("act", lambda: S.activation(out=Ssb[:, off:off + kl], in_=p[:, 0:kl], func=AF.Copy, scale=scale), reads=[p], writes=[Ssb])
                            off += kl
                        nk = off
                        k.op("dve", lambda: V.tensor_reduce(out=mx[:, :], in_=Ssb[:, 0:nk], axis=AX.X, op=ALU.max), reads=[Ssb], writes=[mx])
                        k.op("dve", lambda: V.tensor_tensor(out=mx[:, :], in0=mx[:, :], in1=sinkc[:, h:h + 1], op=ALU.max), reads=[mx, sinkc], writes=[mx])
                        k.op("dve", lambda: V.tensor_scalar(out=mx[:, :], in0=mx[:, :], scalar1=-1.0, scalar2=None, op0=ALU.mult), reads=[mx], writes=[mx])
                        k.op("act", lambda: S.activation(out=Pb[:, 0:nk], in_=Ssb[:, 0:nk], func=AF.Exp, bias=mx[:, 0:1], accum_out=rsum[:, 0:1]), reads=[Ssb, mx], writes=[Pb, rsum])
                        k.op("act", lambda: S.activation(out=es[:, :], in_=sinkc[:, h:h + 1], func=AF.Exp, bias=mx[:, 0:1]), reads=[sinkc, mx], writes=[es])
                        k.op("dve", lambda: V.tensor_tensor(out=rsum[:, :], in0=rsum[:, :], in1=es[:, :], op=ALU.add), reads=[rsum, es], writes=[rsum])
                        k.op("dve", lambda: V.reciprocal(out=rsum[:, :], in_=rsum[:, :]), reads=[rsum], writes=[rsum])
                        nkt = nk // 128
                        p = nps(); pb = p[:, :].bitcast(BF16)
                        for j in range(nkt):
                            k.op("pe", lambda: A.transpose(out=pb[:, j * 128:(j + 1) * 128], in_=Pb[:, j * 128:(j + 1) * 128], identity=ident_b[:, :]), reads=[Pb, ident_b], writes=[p])
                        k.op("act", lambda: S.copy(out=PTr[:, 0:nkt, :], in_=pb[:, 0:nkt * 128].rearrange("p (a b) -> p a b", a=nkt)), reads=[p], writes=[PTr])
                        po = npo()
                        vt_ids = []
                        for (ks, kl, moff) in parts:
                            vt_ids += list(range(ks // 128, (ks + kl) // 128))
                        for j, vt in enumerate(vt_ids):
                            k.op("pe", lambda: A.matmul(po[:, 0:64], lhsT=PTr[:, j, :], rhs=vtok[:, vt, :], start=(j == 0), stop=(j == nkt - 1)), reads=[PTr, vtok], writes=[po], pe_acc=(j > 0))
                        k.op("dve", lambda: V.tensor_scalar(out=ob[:, :], in0=po[:, 0:64], scalar1=rsum[:, 0:1], scalar2=None, op0=ALU.mult), reads=[po, rsum], writes=[ob])
                        p = nps(); pb = p[:, :].bitcast(BF16)
                        k.op("pe", lambda: A.transpose(out=pb[0:64, 0:128], in_=ob[:, :], identity=ident_b[:, :]), reads=[ob, ident_b], writes=[p])
                        k.op("act", lambda: S.copy(out=oT[:, :], in_=pb[0:64, 0:128]), reads=[p], writes=[oT])
                        r0 = c.LW + c.HK + c.MH * 128 + h * 64
                        k.dma("sp", lambda e: e.dma_start(out=YT[r0:r0 + 64, qs:qs + 128], in_=oT[:, :]), reads=[oT], writes=[YT])


    H2 = k.dram("H2", [TT, D], BF16)
    AFFT = k.dram("AFFT", [c.NE, TT], F32)
    MO = k.dram("MO", [TT, D], F32)

    def phase_out(l, need_ctx):
        DC = c.DMIX // 128
        with k.scope():
            ga = k.sbuf("oga", [128, D], F32); A2 = k.sbuf("oA2", [128, D], F32); S2 = k.sbuf("oS2", [128, D], F32)
            xts = [k.sbuf("oxt%d" % i, [128, D], F32) for i in range(2)]
            yT = k.sbuf("oyT", [128, DC, 256], BF16)
            wos = [k.sbuf("owo%d" % i, [128, DC, 256], BF16) for i in range(2)]
            tmp = k.sbuf("otmp", [128, 256], F32)
            hb = k.sbuf("ohb", [128, D], BF16); hT = k.sbuf("ohT", [128, KC, 128], F32)
            ss = k.sbuf("oss", [128, 1], F32); st = k.sbuf("ost", [128, 1], F32)
            wr = k.sbuf("owr", [128, KC, c.NE], F32)
            for kq in range(0, KC, 8):
                k8 = min(8, KC - kq)
                k.dma("sp", lambda e: e.dma_start(out=wr[:, kq:kq + k8, :], in_=I["w_router"][l, kq * 128:(kq + k8) * 128, :].rearrange("(kc p) n -> p kc n", p=128)), reads=[I["w_router"]], writes=[wr])
            lg = k.sbuf("olg", [128, c.NE], F32); lgT = k.sbuf("olgT", [c.NE, 128], F32)
            cur_v = None
            nwo = 0
            blocks = []
            for (tok0, ntok, is_ctx) in c.blocks:
                if is_ctx and not need_ctx:
                    continue
                for b0 in range(0, ntok, 256):
                    blocks.append((tok0 + b0, min(256, ntok - b0), is_ctx))
            for (tok0, ntok, is_ctx) in blocks:
                v = 1 if is_ctx else 0
                if v != cur_v:
                    bc_load(ga, (l * 2 + v) * 6 + 2); bc_load(A2, (l * 2 + v) * 6 + 4); bc_load(S2, (l * 2 + v) * 6 + 3); cur_v = v
                nt = ntok // 128
                for kq in range(0, DC, 8):
                    k8 = min(8, DC - kq)
                    k.dma("sp", lambda e: e.dma_start(out=yT[:, kq:kq + k8, 0:ntok], in_=YT[kq * 128:(kq + k8) * 128, tok0:tok0 + ntok].rearrange("(kc p) t -> p kc t", p=128)), reads=[YT], writes=[yT])
                for ti in range(nt):
                    k.dma("sp", lambda e: e.dma_start(out=xts[ti][:, :], in_=X[tok0 + ti * 128:tok0 + (ti + 1) * 128, :]), reads=[X], writes=[xts[ti]])
                for n0 in range(0, D, 256):
                    wo = wos[nwo % 2]; nwo += 1
                    for kq in range(0, DC, 8):
                        k8 = min(8, DC - kq)
                        k.dma("sp", lambda e: e.dma_start(out=wo[:, kq:kq + k8, :], in_=WOUTB[l * c.DMIX + kq * 128:l * c.DMIX + (kq + k8) * 128, n0:n0 + 256].rearrange("(kc p) n -> p kc n", p=128)), reads=[WOUTB], writes=[wo])
                    for ti in range(nt):
                        p = nps()
                        for kc in range(DC):
                            k.op("pe", lambda: A.matmul(p[:, 0:256], lhsT=yT[:, kc, ti * 128:(ti + 1) * 128], rhs=wo[:, kc, :], start=(kc == 0), stop=(kc == DC - 1)),
                                 reads=[yT, wo], writes=[p], pe_acc=(kc > 0))
                        k.op("dve", lambda: V.tensor_tensor(out=tmp[:, :], in0=p[:, 0:256], in1=ga[:, n0:n0 + 256], op=ALU.mult), reads=[p, ga], writes=[tmp])
                        k.op("pool", lambda: nc.gpsimd.tensor_tensor(out=xts[ti][:, n0:n0 + 256], in0=xts[ti][:, n0:n0 + 256], in1=tmp[:, :], op=ALU.add), reads=[tmp, xts[ti]], writes=[xts[ti]])
                for ti in range(nt):
                    xt = xts[ti]
                    r0 = tok0 + ti * 128
                    k.dma("sp", lambda e: e.dma_start(out=X[r0:r0 + 128, :], in_=xt[:, :]), reads=[xt], writes=[X])
                    k.op("act", lambda: S.activation(out=hb[:, :], in_=xt[:, :], func=AF.Square, accum_out=ss[:, 0:1]), reads=[xt], writes=[hb, ss])
                    rstd_of(ss, D, st)
                    k.op("dve", lambda: V.scalar_tensor_tensor(out=xt[:, :], in0=xt[:, :], scalar=ss[:, 0:1], in1=A2[:, :], op0=ALU.mult, op1=ALU.mult), reads=[xt, ss, A2], writes=[xt])
                    k.op("dve", lambda: V.tensor_tensor(out=xt[:, :], in0=xt[:, :], in1=S2[:, :], op=ALU.add), reads=[xt, S2], writes=[xt])
                    k.op("act", lambda: S.copy(out=hb[:, :], in_=xt[:, :]), reads=[xt], writes=[hb])
                    k.dma("sp", lambda e: e.dma_start(out=H2[r0:r0 + 128, :], in_=hb[:, :]), reads=[hb], writes=[H2])
                    for k0 in range(0, KC, 4):
                        n4 = min(4, KC - k0)
                        p = nps()
                        for j in range(n4):
                            k.op("pe", lambda: A.transpose(out=p[:, j * 128:(j + 1) * 128], in_=xt[:, (k0 + j) * 128:(k0 + j + 1) * 128], identity=ident_f[:, :]), reads=[xt, ident_f], writes=[p])
                        k.op("act", lambda: S.copy(out=hT[:, k0:k0 + n4, :], in_=p[:, 0:n4 * 128].rearrange("p (a b) -> p a b", a=n4)), reads=[p], writes=[hT])
                    p = nps()
                    for kc in range(KC):
                        k.op("pe", lambda: A.matmul(p[:, 0:c.NE], lhsT=hT[:, kc, :], rhs=wr[:, kc, :], start=(kc == 0), stop=(kc == KC - 1)), reads=[hT, wr], writes=[p], pe_acc=(kc > 0))
                    k.op("dve", lambda: V.tensor_reduce(out=ss[:, :], in_=p[:, 0:c.NE], axis=AX.X, op=ALU.max), reads=[p], writes=[ss])
                    k.op("dve", lambda: V.tensor_scalar(out=ss[:, :], in0=ss[:, :], scalar1=-1.0, scalar2=None, op0=ALU.mult), reads=[ss], writes=[ss])
                    k.op("act", lambda: S.activation(out=lg[:, :], in_=p[:, 0:c.NE], func=AF.Exp, bias=ss[:, 0:1], accum_out=st[:, 0:1]), reads=[p, ss], writes=[lg, st])
                    k.op("dve", lambda: V.reciprocal(out=st[:, :], in_=st[:, :]), reads=[st], writes=[st])
                    k.op("dve", lambda: V.tensor_scalar(out=lg[:, :], in0=lg[:, :], scalar1=st[:, 0:1], scalar2=None, op0=ALU.mult), reads=[lg, st], writes=[lg])
                    p = nps()
                    k.op("pe", lambda: A.transpose(out=p[0:c.NE, 0:128], in_=lg[:, :], identity=ident_f[:, :]), reads=[lg, ident_f], writes=[p])
                    k.op("act", lambda: S.copy(out=lgT[:, :], in_=p[0:c.NE, 0:128]), reads=[p], writes=[lgT])
                    k.dma("sp", lambda e: e.dma_start(out=AFFT[:, r0:r0 + 128], in_=lgT[:, :]), reads=[lgT], writes=[AFFT])

    def regions(need_ctx):
        r = []
        if need_ctx:
            r.append((0, 0, Tc, c.capc))
        r.append((1, Tc, T, c.cap))
        return r

    def phase_moe(l, need_ctx):
        FC = c.FF // 128
        with k.scope():
            z = k.sbuf("mz", [128, D], F32)
            k.op("dve", lambda: V.memset(z[:, :], 0.0), writes=[z])
            for t0 in range(0 if need_ctx else Tc, TT, 128):
                k.dma("sp", lambda e: e.dma_start(out=MO[t0:t0 + 128, :], in_=z[:, :]), reads=[z], writes=[MO])
        for (ri, t0, n, cap) in regions(need_ctx):
            ns = min(128, cap)
            NG_ = cap // ns
            with k.scope():
                idxT = k.sbuf("eidx", [128, c.NE * NG_], I32); gT = k.sbuf("egT", [128, c.NE * NG_], F32); idxS = k.sbuf("eidxS", [128, c.NE * NG_], I32)
                nsp = max(1, D // getattr(c, "msplit", 2048))
                Dh = D // nsp
                MO2 = MO[:, :].rearrange("t (s d) -> (t s) d", s=nsp)
                with k.scope():
                    wk = k.sbuf("twk", [c.NE, n], F32); vals = k.sbuf("tvals", [c.NE, cap], F32); idx = k.sbuf("tidx", [c.NE, cap], U32); idxf = k.sbuf("tidxf", [c.NE, cap], F32)
                    k.dma("sp", lambda e: e.dma_start(out=wk[:, :], in_=AFFT[:, t0:t0 + n]), reads=[AFFT], writes=[wk])
                    for r in range((cap + 7) // 8):
                        k.op("dve", lambda: V.max(out=vals[:, r * 8:(r + 1) * 8], in_=wk[:, :]), reads=[wk], writes=[vals])
                        k.op("dve", lambda: V.max_index(out=idx[:, r * 8:(r + 1) * 8], in_max=vals[:, r * 8:(r + 1) * 8], in_values=wk[:, :]), reads=[wk, vals], writes=[idx])
                        k.op("dve", lambda: V.match_replace(out=wk[:, :], in_to_replace=vals[:, r * 8:(r + 1) * 8], in_values=wk[:, :], imm_value=-1.0), reads=[wk, vals], writes=[wk])
                    k.op("dve", lambda: V.tensor_copy(out=idxf[:, :], in_=idx[:, :]), reads=[idx], writes=[idxf])
                    idxf2 = k.sbuf("tidxf2", [c.NE, cap], F32)
                    k.op("dve", lambda: V.tensor_scalar(out=idxf2[:, :], in0=idxf[:, :], scalar1=float(nsp), scalar2=None, op0=ALU.mult), reads=[idxf], writes=[idxf2])
                    for j in range(NG_):
                        for (src, dst) in ((idxf, idxT), (vals, gT), (idxf2, idxS)):
                            p = nps()
                            k.op("pe", lambda: A.transpose(out=p[0:ns, 0:c.NE], in_=src[:, j * ns:(j + 1) * ns], identity=ident_f[0:c.NE, 0:c.NE]), reads=[src, ident_f], writes=[p])
                            k.op("dve", lambda: V.tensor_copy(out=dst[0:ns, :].rearrange("p (e j) -> p e j", e=c.NE)[:, :, j], in_=p[0:ns, 0:c.NE]), reads=[p], writes=[dst])
                hid = k.sbuf("ehid", [128, FC, cap], BF16)
                for e_ in range(c.NE):
                    with k.scope():
                        xgT = k.sbuf("exgT", [128, KC, cap], BF16)
                        xgs = [k.sbuf("exg%d" % i, [128, D], BF16) for i in range(2)]
                        for j in range(NG_):
                            xg = xgs[j % 2]
                            col = e_ * NG_ + j
                            k.dma("pool", lambda e: e.indirect_dma_start(out=xg[0:ns, :], out_offset=None, in_=H2[:, :], in_offset=bass.IndirectOffsetOnAxis(ap=idxT[0:ns, col:col + 1], axis=0),
                                                                         element_offset=t0 * D), reads=[H2, idxT], writes=[xg])
                            for k0 in range(0, KC, 8):
                                n8 = min(8, KC - k0)
                                p = nps(); pb = p[:, :].bitcast(BF16)
                                for jj in range(n8):
                                    k.op("pe", lambda: A.transpose(out=pb[:, jj * 128:jj * 128 + ns], in_=xg[0:ns, (k0 + jj) * 128:(k0 + jj + 1) * 128], identity=ident_b[0:ns, 0:ns]),
                                         reads=[xg, ident_b], writes=[p])
                                k.op("act" if (k0 // 8) % 2 else "dve",
                                     (lambda: S.copy(out=xgT[:, k0:k0 + n8, j * ns:(j + 1) * ns], in_=pb[:, 0:n8 * 128].rearrange("p (a b) -> p a b", a=n8)[:, :, 0:ns])) if (k0 // 8) % 2 else
                                     (lambda: V.tensor_copy(out=xgT[:, k0:k0 + n8, j * ns:(j + 1) * ns], in_=pb[:, 0:n8 * 128].rearrange("p (a b) -> p a b", a=n8)[:, :, 0:ns])),
                                     reads=[p], writes=[xgT])
                        wgs = [k.sbuf("ewg%d" % i, [128, KC, 256], BF16) for i in range(2)]
                        wus = [k.sbuf("ewu%d" % i, [128, KC, 256], BF16) for i in range(2)]
                        tg = k.sbuf("etg", [128, 512], F32)
                        WGh, WUh, WDh = WGB[e_ // NH], WUB[e_ // NH], WDB[e_ // NH]
                        wrow = (l * NH + e_ % NH) * D
                        nw_ = 0
                        for f0 in range(0, c.FF, 256):
                            fw = min(256, c.FF - f0)
                            wg, wu = wgs[nw_ % 2], wus[nw_ % 2]; nw_ += 1
                            for kq in range(0, KC, 8):
                                k8 = min(8, KC - kq)
                                k.dma("sp", lambda e: e.dma_start(out=wg[:, kq:kq + k8, 0:fw], in_=WGh[wrow + kq * 128:wrow + (kq + k8) * 128, f0:f0 + fw].rearrange("(kc p) n -> p kc n", p=128)), reads=[WGh], writes=[wg])
                                k.dma("sp", lambda e: e.dma_start(out=wu[:, kq:kq + k8, 0:fw], in_=WUh[wrow + kq * 128:wrow + (kq + k8) * 128, f0:f0 + fw].rearrange("(kc p) n -> p kc n", p=128)), reads=[WUh], writes=[wu])
                            for fi in range(fw // 128):
                                fc = (f0 // 128) + fi
                                for s0 in range(0, cap, 512):
                                    sw = min(512, cap - s0)
                                    pg, pu = nps(), nps()
                                    for kc in range(KC):
                                        k.op("pe", lambda: A.matmul(pg[:, 0:sw], lhsT=wg[:, kc, fi * 128:(fi + 1) * 128], rhs=xgT[:, kc, s0:s0 + sw], start=(kc == 0), stop=(kc == KC - 1)),
                                             reads=[wg, xgT], writes=[pg], pe_acc=(kc > 0))
                                    for kc in range(KC):
                                        k.op("pe", lambda: A.matmul(pu[:, 0:sw], lhsT=wu[:, kc, fi * 128:(fi + 1) * 128], rhs=xgT[:, kc, s0:s0 + sw], start=(kc == 0), stop=(kc == KC - 1)),
                                             reads=[wu, xgT], writes=[pu], pe_acc=(kc > 0))
                                    k.op("act", lambda: S.activation(out=tg[:, 0:sw], in_=pg[:, 0:sw], func=AF.Silu), reads=[pg], writes=[tg])
                                    k.op("dve", lambda: V.tensor_tensor(out=hid[:, fc, s0:s0 + sw], in0=tg[:, 0:sw], in1=pu[:, 0:sw], op=ALU.mult), reads=[tg, pu], writes=[hid])
                    with k.scope():
                        wd = k.sbuf("ewd", [128, FC, D], BF16)
                        yos = [[k.sbuf("eyo%d_%d" % (i, h_), [128, Dh], F32) for h_ in range(nsp)] for i in range(2)]
                        WDh = WDB[e_ // NH]
                        drow = (l * NH + e_ % NH) * c.FF
                        k.dma("sp", lambda e: e.dma_start(out=wd[:, :, :], in_=WDh[drow:drow + c.FF, :].rearrange("(fc p) n -> p fc n", p=128)), reads=[WDh], writes=[wd])
                        for j in range(NG_):
                            yo = yos[j % 2]
                            col = e_ * NG_ + j
                            NST = min(512, Dh)
                            for n0 in range(0, D, NST):
                                nw2 = min(NST, D - n0)
                                p = nps()
                                for fc in range(FC):
                                    k.op("pe", lambda: A.matmul(p[0:ns, 0:nw2], lhsT=hid[:, fc, j * ns:(j + 1) * ns], rhs=wd[:, fc, n0:n0 + nw2], start=(fc == 0), stop=(fc == FC - 1)),
                                         reads=[hid, wd], writes=[p], pe_acc=(fc > 0))
                                yh = yo[n0 // Dh]
                                m0 = n0 % Dh
                                if (n0 // NST) % 2:
                                    k.op("act", lambda: S.activation(out=yh[0:ns, m0:m0 + nw2], in_=p[0:ns, 0:nw2], func=AF.Copy, scale=gT[0:ns, col:col + 1]), reads=[p, gT], writes=[yh])
                                else:
                                    k.op("dve", lambda: V.tensor_scalar(out=yh[0:ns, m0:m0 + nw2], in0=p[0:ns, 0:nw2], scalar1=gT[0:ns, col:col + 1], scalar2=None, op0=ALU.mult), reads=[p, gT], writes=[yh])
                            for h_ in range(nsp):
                                yh = yo[h_]
                                k.dma("pool", lambda e: e.indirect_dma_start(out=MO2, out_offset=bass.IndirectOffsetOnAxis(ap=idxS[0:ns, col:col + 1], axis=0), in_=yh[0:ns, :], in_offset=None,
                                                                             element_offset=t0 * D + h_ * Dh, compute_op=ALU.add), reads=[yh, idxS], writes=[MO])

    def phase_final(l, need_ctx, last):
        with k.scope():
            ga = k.sbuf("fga", [128, D], F32); gfb = k.sbuf("fgf", [128, D], F32)
            xts = [k.sbuf("fxt%d" % i, [128, D], F32) for i in range(2)]; mts = [k.sbuf("fmt%d" % i, [128, D], F32) for i in range(2)]
            junk = k.sbuf("fjunk", [128, D], BF16); ss = k.sbuf("fss", [128, 1], F32); st = k.sbuf("fst", [128, 1], F32)
            if last:
                k.dma("sp", lambda e: e.dma_start(out=gfb[:, :], in_=I["gf"][0:1, :].partition_broadcast(128)), reads=[I["gf"]], writes=[gfb])
            cur_v = None
            n = 0
            for t0 in range(0 if need_ctx else Tc, TT, 128):
                v = 1 if t0 < Tc else 0
                if v != cur_v:
                    bc_load(ga, (l * 2 + v) * 6 + 5); cur_v = v
                xt, mt = xts[n % 2], mts[n % 2]; n += 1
                k.dma("sp", lambda e: e.dma_start(out=xt[:, :], in_=X[t0:t0 + 128, :]), reads=[X], writes=[xt])
                k.dma("sp", lambda e: e.dma_start(out=mt[:, :], in_=MO[t0:t0 + 128, :]), reads=[MO], writes=[mt])
                k.op("dve", lambda: V.tensor_tensor(out=mt[:, :], in0=mt[:, :], in1=ga[:, :], op=ALU.mult), reads=[mt, ga], writes=[mt])
                k.op("pool", lambda: nc.gpsimd.tensor_tensor(out=xt[:, :], in0=xt[:, :], in1=mt[:, :], op=ALU.add), reads=[xt, mt], writes=[xt])
                if not last:
                    k.dma("sp", lambda e: e.dma_start(out=X[t0:t0 + 128, :], in_=xt[:, :]), reads=[xt], writes=[X])
                else:
                    k.op("act", lambda: S.activation(out=junk[:, :], in_=xt[:, :], func=AF.Square, accum_out=ss[:, 0:1]), reads=[xt], writes=[junk, ss])
                    rstd_of(ss, D, st)
                    k.op("dve", lambda: V.scalar_tensor_tensor(out=xt[:, :], in0=xt[:, :], scalar=ss[:, 0:1], in1=gfb[:, :], op0=ALU.mult, op1=ALU.mult), reads=[xt, ss, gfb], writes=[xt])
                    k.dma("sp", lambda e: e.dma_start(out=OUT[t0 - Tc:t0 - Tc + 128, :], in_=xt[:, :]), reads=[xt], writes=[OUT])

    def layer(l):
        need_ctx = l < L - 1
        phase_in(l)
        phase_lru(l)
        phase_hg(l)
        phase_mla(l, need_ctx)
        phase_win(l, need_ctx)
        phase_out(l, need_ctx)
        phase_moe(l, need_ctx)
        phase_final(l, need_ctx, l == L - 1)

    B = dict(OUT=OUT, WINB=WINB, WOUTB=WOUTB, MOD=MOD, X=X, PT=PT, YT=YT, ident_f=ident_f, ident_b=ident_b, nps=nps, PS=PS,
             phase_in=phase_in)
    if c.stop_after in ("alloc", "stage", "in", "lru", "hg", "mlap", "mla", "win", "out", "moe"):
        seq = ["alloc", "stage", "in", "lru", "hg", "mlap", "mla", "win", "out", "moe"]
        n_ = seq.index(c.stop_after)
        if n_ >= 2: phase_in(0)
        if n_ >= 3: phase_lru(0)
        if n_ >= 4: phase_hg(0)
        if n_ >= 5: phase_mla_proj(0)
        if n_ >= 6: phase_mla_attn(0, True)
        if n_ >= 7: phase_win(0, True)
        if n_ >= 8: phase_out(0, True)
        if n_ >= 9: phase_moe(0, True)
        k.barrier()
        for r0 in range(0, T, 512):
            k.dma("sp", lambda e: e.dma_start(out=OUT[r0:r0 + 512, :], in_=X[Tc + r0:Tc + r0 + 512, :]), reads=[X], writes=[OUT])
        return nc, k, I, B
    if c.stop_after == "in0":
        phase_in(0)
        _dbg(k, c, "PT", lambda: PT.ap(), [DIN, TT])
        return nc, k, I, B
    if c.stop_after == "":
        for rep in range(getattr(c, "repeat", 1)):
            for l in range(L):
                if rep > 0 and l == L - 1:
                    continue
                layer(l)
        return nc, k, I, B
    if c.stop_after.startswith("mix"):
        PTin = inp("PT_in", [DIN, TT])
        k.dma("sp", lambda e: e.dma_start(out=PT.ap(), in_=PTin.ap()), reads=[PTin], writes=[PT])
        k.barrier()
        for ph in c.stop_after.split("_")[1:]:
            dict(lru=phase_lru, hg=phase_hg, mla=phase_mla, win=phase_win)[ph](0)
        _dbg(k, c, "YT", lambda: YT.ap(), [c.DMIX, TT], BF16)
        return nc, k, I, B
    return nc, k, I, B


def prep(cfg, inp):
    c = cfg
    f32 = np.float32
    T, D, KC = c.T, c.D, c.KC
    vecs = np.stack([inp["c"][0], inp["c"][1], inp["c_ctx"]]).astype(f32)
    cv = np.ascontiguousarray(vecs.reshape(3, KC, 128).transpose(2, 0, 1))
    pos = np.arange(T)
    row, col = pos // c.GW, pos % c.GW
    inv = (10000.0 ** (-np.arange(16, dtype=f32) / 16)).astype(f32)
    d = np.arange(64)
    p = np.where(d[:, None] < 32, row[None, :], col[None, :]).astype(f32)
    ang = p * inv[d % 16][:, None]
    cs = np.stack([np.cos(ang), np.sin(ang)]).astype(f32)
    rope_cs = np.ascontiguousarray(np.concatenate([cs, cs], axis=1))
    R32 = np.zeros((32, 32), f32)
    for i in range(16):
        R32[i, i + 16] = -1.0
        R32[i + 16, i] = 1.0
    R128 = np.kron(np.eye(4, dtype=f32), R32)
    qi = np.arange(128)[:, None]
    offs = np.arange(384)[None, :] - 128
    wmask = np.where(np.abs(offs - qi) <= 128, 0.0, -1e30).astype(f32)
    s_, t_ = np.arange(64)[:, None], np.arange(64)[None, :]
    tri = np.stack([np.tile((s_ <= t_).astype(f32), (1, 8)), np.tile((s_ >= t_).astype(f32), (1, 8))])
    maps = []
    for core in range(2):
        l = core
        pidx = np.arange(128)
        coreidx = np.zeros((128, 16), np.int32)
        for j in range(6):
            coreidx[:, 8 + j] = (pidx + 3 * core) * 6 + j
        coreidx[:, 0] = pidx + core * D
        coreidx[:, 1] = pidx + core * c.DMIX
        coreidx[:, 2] = pidx + core * (c.NE // 2) * D
        coreidx[:, 3] = pidx + core * (c.NE // 2) * c.FF
        coreidx[:, 4] = pidx + 3 * core
        m = dict(
            x=inp["x"][core], ctx=inp["ctx"][core], cv=cv,
            w_ada=inp["w_ada"][l], b_ada=inp["b_ada"], g1=inp["norm1_g"], g2=inp["norm2_g"], gf=inp["final_norm_g"].reshape(1, D),
            w_in=inp["w_in"][l], w_out=inp["w_out"][l],
            w_gate=inp["w_gate"][l].reshape(c.NE * D, c.FF), w_up=inp["w_up"][l].reshape(c.NE * D, c.FF),
            w_down=inp["w_down"][l].reshape(c.NE * c.FF, D), w_router=inp["w_router"],
            w_uq=inp["mla_w_uq"], w_ukv=inp["mla_w_ukv"], conv_w=inp["lru_conv_w"], conv_b=inp["lru_conv_b"],
            lru_wr=inp["lru_wr"], lru_wi=inp["lru_wi"], lru_br=inp["lru_br"], lru_bi=inp["lru_bi"], lru_lam=inp["lru_lambda"],
            hg_lb=inp["hg_lb_logits"], hg_g=inp["hg_norm_g"], q_g=inp["mla_q_norm_g"], kv_g=inp["mla_kv_norm_g"], sink=inp["win_sink"],
            coreidx=coreidx, ident_f=np.eye(128, dtype=f32), rope_cs=rope_cs, rope_rt=np.ascontiguousarray(R128.T), wmask=wmask, tri=tri,
        )
        maps.append({k_: np.ascontiguousarray(v_) for k_, v_ in m.items()})
    return maps


def kernel(**inputs):
    from concourse.bass_utils import run_bass_kernel_spmd
    cfg = Cfg(**FULL, debug=False, stop_after="", hgcut=0)
    inp = {k_: np.asarray(v_) for k_, v_ in inputs.items()}
    nc, k, I, B = build(cfg)
    k.close()
    maps = prep(cfg, inp)
    res = run_bass_kernel_spmd(nc, maps, core_ids=[0, 1])
    return np.stack([np.asarray(res.results[c_]["out"]) for c_ in range(2)]).astype(np.float32)
```
